# Optimizing a Trainium2 kernel written in Bass

```python
import jax, jax.numpy as jnp
from jax import lax
import numpy as np

D_MODEL = 1024
BATCH = 32
SEQ = 2048
DEPTH = 1

D_MIX = D_MODEL
D_HGRN = D_MIX // 2
N_HGRN_HEADS = 4
HGRN_HEAD_DIM = D_HGRN // N_HGRN_HEADS
D_CONV = D_MIX - D_HGRN
N_CONV_GROUPS = 8
CONV_WIDTH = 31
D_FF = 2816
CHUNK = 64
ALPHA = (2.0 * DEPTH) ** 0.25
BETA = (8.0 * DEPTH) ** -0.25
LN_EPS = 1e-5
RMS_EPS = 1e-6
D_IN = 4 * D_HGRN + 2 * D_CONV

kernel_name = 'hgrn2_conformer_conv_hybrid_deepnorm'


def layer_norm(x, g, b):
    xf = x.astype(jnp.float32)
    mu = jnp.mean(xf, axis=-1, keepdims=True)
    var = jnp.mean(jnp.square(xf - mu), axis=-1, keepdims=True)
    y = (xf - mu) * lax.rsqrt(var + LN_EPS) * g.astype(jnp.float32) + b.astype(jnp.float32)
    return y.astype(x.dtype)


def swiglu(x, w_gate, w_up, w_down):
    return (jax.nn.silu(x @ w_gate) * (x @ w_up)) @ w_down


def hgrn2_chunkwise(q, k, v, log_f):
    B, T, H, dk = q.shape
    dv = v.shape[-1]
    n = T // CHUNK

    def to_chunks(a):
        a = a.astype(jnp.float32)
        return a.reshape(B, n, CHUNK, H, a.shape[-1]).transpose(1, 0, 3, 2, 4)

    qc, kc, vc, gc = to_chunks(q), to_chunks(k), to_chunks(v), to_chunks(log_f)
    causal = jnp.tril(jnp.ones((CHUNK, CHUNK), dtype=bool))[:, :, None]

    def step(S, inp):
        qi, ki, vi, gi = inp
        b = jnp.cumsum(gi, axis=-2)
        diff = b[..., :, None, :] - b[..., None, :, :]
        decay = jnp.exp(jnp.where(causal, diff, -jnp.inf))
        scores = jnp.einsum('bhtk,bhsk,bhtsk->bhts', qi, ki, decay)
        o_intra = jnp.einsum('bhts,bhsv->bhtv', scores, vi)
        o_inter = jnp.einsum('bhtk,bhkv->bhtv', qi * jnp.exp(b), S)
        b_last = b[..., -1:, :]
        S_new = S * jnp.exp(b_last[..., 0, :])[..., None] + jnp.einsum(
            'bhsk,bhsv->bhkv', ki * jnp.exp(b_last - b), vi)
        return S_new, o_intra + o_inter

    S0 = jnp.zeros((B, H, dk, dv), jnp.float32)
    _, o = lax.scan(step, S0, (qc, kc, vc, gc))
    return o.transpose(1, 0, 3, 2, 4).reshape(B, T, H * dv)


def setup_inputs(seed: int = 0) -> dict:
    key = jax.random.key(seed)
    ks = jax.random.split(key, 24)
    L = DEPTH

    def normal(k, shape, scale):
        return scale * jax.random.normal(k, shape, jnp.float32)

    return {
        'x': normal(ks[0], (BATCH, SEQ, D_MODEL), 1.0),
        'ffn1_w_gate': normal(ks[1], (L, D_MODEL, D_FF), D_MODEL ** -0.5),
        'ffn1_w_up': normal(ks[2], (L, D_MODEL, D_FF), D_MODEL ** -0.5),
        'ffn1_w_down': normal(ks[3], (L, D_FF, D_MODEL), BETA * D_FF ** -0.5),
        'ln1_g': 1.0 + normal(ks[4], (L, D_MODEL), 0.02),
        'ln1_b': normal(ks[5], (L, D_MODEL), 0.02),
        'w_in': normal(ks[6], (L, D_MODEL, D_IN), D_MODEL ** -0.5),
        'lb_logits': normal(ks[7], (L + 1, D_HGRN), 0.1),
        'hgrn_norm_g': 1.0 + normal(ks[8], (L, D_HGRN), 0.02),
        'conv_w': normal(ks[9], (L, CONV_WIDTH, 1, D_CONV), CONV_WIDTH ** -0.5),
        'conv_b': normal(ks[10], (L, D_CONV), 0.02),
        'conv_ln_g': 1.0 + normal(ks[11], (L, D_CONV), 0.02),
        'conv_ln_b': normal(ks[12], (L, D_CONV), 0.02),
        'w_out': normal(ks[13], (L, D_MIX, D_MODEL), BETA * D_MIX ** -0.5),
        'ln2_g': 1.0 + normal(ks[14], (L, D_MODEL), 0.02),
        'ln2_b': normal(ks[15], (L, D_MODEL), 0.02),
        'ffn2_w_gate': normal(ks[16], (L, D_MODEL, D_FF), D_MODEL ** -0.5),
        'ffn2_w_up': normal(ks[17], (L, D_MODEL, D_FF), D_MODEL ** -0.5),
        'ffn2_w_down': normal(ks[18], (L, D_FF, D_MODEL), BETA * D_FF ** -0.5),
        'ln3_g': 1.0 + normal(ks[19], (L, D_MODEL), 0.02),
        'ln3_b': normal(ks[20], (L, D_MODEL), 0.02),
    }


def reference(x, ffn1_w_gate, ffn1_w_up, ffn1_w_down, ln1_g, ln1_b, w_in, lb_logits,
              hgrn_norm_g, conv_w, conv_b, conv_ln_g, conv_ln_b, w_out, ln2_g, ln2_b,
              ffn2_w_gate, ffn2_w_up, ffn2_w_down, ln3_g, ln3_b):
    B, T, _ = x.shape
    lower_bounds = jnp.cumsum(jax.nn.softmax(lb_logits.astype(jnp.float32), axis=0), axis=0)

    for l in range(DEPTH):
        x = layer_norm(ALPHA * x + 0.5 * swiglu(x, ffn1_w_gate[l], ffn1_w_up[l], ffn1_w_down[l]),
                       ln1_g[l], ln1_b[l])

        proj = x @ w_in[l]
        q, f_raw, i_val, g_out, conv_val, conv_gate = jnp.split(
            proj, [D_HGRN, 2 * D_HGRN, 3 * D_HGRN, 4 * D_HGRN, 4 * D_HGRN + D_CONV], axis=-1)

        lb = lower_bounds[l]
        f = lb + (1.0 - lb) * jax.nn.sigmoid(f_raw.astype(jnp.float32))
        k = 1.0 - f
        log_f = jnp.log(f)

        def heads(a):
            return a.reshape(B, T, N_HGRN_HEADS, HGRN_HEAD_DIM)

        o = hgrn2_chunkwise(heads(q), heads(k), heads(i_val), heads(log_f))
        oh = o.reshape(B, T, N_HGRN_HEADS, HGRN_HEAD_DIM)
        oh = oh * lax.rsqrt(jnp.mean(jnp.square(oh), axis=-1, keepdims=True) + RMS_EPS)
        o_a = (oh.reshape(B, T, D_HGRN) * hgrn_norm_g[l].astype(jnp.float32)
               * jax.nn.sigmoid(g_out.astype(jnp.float32))).astype(x.dtype)

        u = conv_val * jax.nn.sigmoid(conv_gate)
        c = lax.conv_general_dilated(
            u, conv_w[l], window_strides=(1,), padding=[(CONV_WIDTH - 1, 0)],
            dimension_numbers=('NWC', 'WIO', 'NWC'), feature_group_count=D_CONV) + conv_b[l]
        o_b = jax.nn.silu(layer_norm(c, conv_ln_g[l], conv_ln_b[l]))

        mix = jnp.concatenate([o_a, o_b], axis=-1) @ w_out[l]
        x = layer_norm(ALPHA * x + mix, ln2_g[l], ln2_b[l])

        x = layer_norm(ALPHA * x + 0.5 * swiglu(x, ffn2_w_gate[l], ffn2_w_up[l], ffn2_w_down[l]),
                       ln3_g[l], ln3_b[l])
    return x
```

```python
import numpy as np
from contextlib import ExitStack
import concourse.bass as bass
import concourse.mybir as mybir
from concourse.bass_utils import run_bass_kernel_spmd

F32 = mybir.dt.float32
BF16 = mybir.dt.bfloat16
I32 = mybir.dt.int32
AF = mybir.ActivationFunctionType
ALU = mybir.AluOpType

D = 1024
KC = 8
DFF = 2816
FC = 22
DIN = 3072
NH = 4
CW = 31
HALO = CW - 1
ALPHA = 2.0 ** 0.25
LN_EPS = 1e-5
RMS_EPS = 1e-6
NT = 512
NB = NT // 128
RING_ELEMS = 22 * 1024
NSCR = 14
MAGIC = 0x5F3759DF

ENGINES = ("tensor", "vector", "scalar", "gpsimd", "sync")


class Buf:
    __slots__ = ("name", "writer", "readers", "aliases", "dead")

    def __init__(self, name, aliases=()):
        self.name = name
        self.writer = None
        self.readers = []
        self.aliases = list(aliases)
        self.dead = False


class Op:
    __slots__ = ("eng", "fn", "dma", "deps", "signal", "sem", "val", "waits", "pre")

    def __init__(self, eng, fn, dma):
        self.eng = eng
        self.fn = fn
        self.dma = dma
        self.deps = []
        self.signal = False
        self.sem = None
        self.val = 0
        self.waits = []


def OPC(name, *args, **kwargs):
    return lambda e: getattr(e, name)(*args, **kwargs)


class Prog:
    def __init__(self, nc):
        self.nc = nc
        self.ops = []

    def op(self, eng, fn, reads=(), writes=(), dma=None):
        o = Op(eng, fn, dma)
        deps = {}
        for b in reads:
            assert not b.dead, "read of dead buffer " + b.name
            for bb in [b] + b.aliases:
                if bb.writer is not None:
                    deps[id(bb.writer)] = bb.writer
        for b in writes:
            assert not b.dead, "write of dead buffer " + b.name
            for bb in [b] + b.aliases:
                if bb.writer is not None:
                    deps[id(bb.writer)] = bb.writer
                for r in bb.readers:
                    deps[id(r)] = r
        for d in deps.values():
            if d is o:
                continue
            if d.eng == "tensor" and eng == "tensor" and d.dma is None and dma is None:
                continue
            o.deps.append(d)
            d.signal = True
        for b in reads:
            b.readers.append(o)
        for b in writes:
            b.readers = []
            for bb in b.aliases:
                bb.readers = []
                bb.writer = None
            b.aliases = []
            b.writer = o
        self.ops.append(o)
        return o

    def emit(self, final_waits=()):
        nc = self.nc
        with ExitStack() as es:
            sems = {}

            def get_sem(key):
                if key not in sems:
                    sems[key] = es.enter_context(nc.semaphore("s_" + key))
                return sems[key]

            counts = {}
            KDMA = 8
            for o in final_waits:
                o.signal = True
            for o in self.ops:
                o.pre = None
                if o.dma:
                    n = counts.get("d_" + o.dma, 0)
                    counts["d_" + o.dma] = n + 1
                    o.signal = True
                    o.sem = get_sem("d_%s_%d" % (o.dma, n % KDMA))
                    o.val = 16 * (n // KDMA + 1)
                    if n >= KDMA:
                        o.pre = (o.sem, 16 * (n // KDMA))
                    continue
                if not o.signal:
                    continue
                key = "e_" + o.eng
                counts[key] = counts.get(key, 0) + 1
                o.sem = get_sem(key)
                o.val = counts[key]
            self.counts = counts
            known = {e: {} for e in ENGINES}
            per_eng = {e: [] for e in ENGINES}
            for o in self.ops:
                need = {}
                if o.pre is not None:
                    need[id(o.pre[0])] = o.pre
                for d in o.deps:
                    k = id(d.sem)
                    if k not in need or need[k][1] < d.val:
                        need[k] = (d.sem, d.val)
                kn = known[o.eng]
                for k, (s, v) in need.items():
                    if kn.get(k, 0) >= v:
                        continue
                    kn[k] = v
                    o.waits.append((s, v))
                per_eng[o.eng].append(o)
            with nc.Block() as block:
                def mk(engname):
                    def body(eng):
                        for o in per_eng[engname]:
                            for (s, v) in o.waits:
                                eng.wait_ge(s, v)
                            ins = o.fn(eng)
                            if o.signal:
                                ins.then_inc(o.sem, 16 if o.dma else 1)
                        if engname == "sync":
                            for o in final_waits:
                                eng.wait_ge(o.sem, o.val)
                    return body
                for e in ENGINES:
                    if per_eng[e] or e == "sync":
                        getattr(block, e)(mk(e))


def build_nc(nseq, T):
    ntok = nseq * T
    tiles_per_seq = T // NT
    ntiles = nseq * tiles_per_seq
    nc = bass.Bass("TRN2", target_bir_lowering=False)

    def din(name, shape):
        return nc.dram_tensor(name, list(shape), F32, kind="ExternalInput").ap()

    x_d = din("x", [ntok, D])
    wg_d = [din("ffn1_w_gate", [D, DFF]), din("ffn2_w_gate", [D, DFF])]
    wu_d = [din("ffn1_w_up", [D, DFF]), din("ffn2_w_up", [D, DFF])]
    wd_d = [din("ffn1_w_down", [DFF, D]), din("ffn2_w_down", [DFF, D])]
    win_d = din("w_in", [D, DIN])
    wout_d = din("w_out", [D, D])
    lng_d = [din("ln1_g", [1, D]), din("ln2_g", [1, D]), din("ln3_g", [1, D])]
    lnb_d = [din("ln1_b", [1, D]), din("ln2_b", [1, D]), din("ln3_b", [1, D])]
    lb_d = din("lb_logits", [2, 512])
    hg_d = din("hgrn_norm_g", [1, 512])
    cw_d = din("conv_w", [CW, 512])
    cb_d = din("conv_b", [1, 512])
    clg_d = din("conv_ln_g", [1, 512])
    clb_d = din("conv_ln_b", [1, 512])
    out_d = nc.dram_tensor("out", [ntok, D], F32, kind="ExternalOutput").ap()

    P = Prog(nc)
    fin = []
    with ExitStack() as es:
        def sb(name, shape, dt):
            return es.enter_context(nc.sbuf_tensor(name, list(shape), dt))

        X = [sb("X%d" % i, [128, NB, D], F32) for i in range(2)]
        Xb = [[Buf("X%d_%d" % (i, b)) for b in range(NB)] for i in range(2)]
        XT = sb("XT", [128, KC, NT], BF16)
        XTb = [Buf("XT%d" % b) for b in range(NB)]
        HT = sb("HT", [128, FC, NT], BF16)
        HTb = [Buf("HT%d" % f) for f in range(FC)]
        HTf = HT[:].rearrange("p f t -> p (f t)")
        BC = HTf[:, 0:4096].bitcast(F32).rearrange("p (h t) -> p h t", h=NH)
        CS = HTf[:, 4096:8192].bitcast(F32).rearrange("p (h t) -> p h t", h=NH)
        THG = HTf[:, 8192:10240].rearrange("p (h t) -> p h t", h=NH)
        BCb = [Buf("BC%d" % h, aliases=HTb) for h in range(NH)]
        CSb = [Buf("CS%d" % h, aliases=HTb) for h in range(NH)]
        THGb = [Buf("THG%d" % h, aliases=HTb) for h in range(NH)]
        mix_alias = BCb + CSb + THGb

        KT = sb("KT", [128, NH, NT], BF16)
        KTb = [Buf("KT%d" % h) for h in range(NH)]
        QT = sb("QT", [128, NH, NT], BF16)
        QTb = [Buf("QT%d" % h) for h in range(NH)]
        V = sb("V", [128, NB, 512], BF16)
        Vb = [Buf("V%d" % b) for b in range(NB)]
        KTOK = sb("KTOK", [128, NB, 512], BF16)
        KTOKb = [Buf("KTOK%d" % b) for b in range(NB)]
        AT = [sb("AT%d" % i, [128, NH, 128], BF16) for i in range(NB)]
        ATb = [Buf("AT%d" % i) for i in range(NB)]
        S = sb("S", [128, NH, 128], F32)
        Sb_ = Buf("S")
        SBS = sb("SBS", [128, NT // 64 + 1, NH, 128], BF16)
        SBSb = [Buf("SBS%d" % i) for i in range(NT // 64 + 1)]
        UT = sb("UT", [128, NH, HALO + NT], BF16)
        UTb = [Buf("UT%d" % c) for c in range(NH)]
        CB = V
        CBb = [Buf("CB%d" % c) for c in range(NH)]
        CQ = KTOK
        CQb = [Buf("CQ%d" % c) for c in range(NH)]
        OS = sb("OS", [128, NB, 512], F32)
        OSb = [Buf("OS%d" % b) for b in range(NB)]
        CT = sb("CT", [128, KC, NT], BF16)
        CTb = [[Buf("CT%d_%d" % (c, b)) for b in range(NB)] for c in range(KC)]
        GB = sb("GB", [128, 2, 2, D], F32)
        GBb = [Buf("GB%d" % i) for i in range(2)]
        SCR = sb("SCR", [128, NSCR, 512], F32)
        SCRb = [Buf("SCR%d" % i) for i in range(NSCR)]
        RING = sb("RING", [128, RING_ELEMS], BF16)
        IDF = sb("IDF", [128, 128], F32)
        IDB = sb("IDB", [128, 128], BF16)
        ONEB = sb("ONEB", [128, 128], BF16)
        ONEF = sb("ONEF", [128, 64], F32)
        MASK = sb("MASK", [128, 128], F32)
        PR = sb("PR", [37, 512], F32)
        PT = sb("PT", [128, NH, 37], F32)
        PC = sb("PC", [128, NH, 8], F32)
        W05 = sb("W05", [128, NH, CW], F32)
        STT = sb("STT", [128, 2, NB, 2, 6], F32)
        SM = sb("SM", [128, 2, NB, 8], F32)
        SM2 = sb("SM2", [128, 4, 16], F32)
        cB = Buf("consts")
        STTb = [[Buf("STT%d_%d" % (i, b)) for b in range(NB)] for i in range(2)]
        SMb = [[Buf("SM%d_%d" % (i, b)) for b in range(NB)] for i in range(2)]
        SM2b = [Buf("SM2_%d" % i) for i in range(4)]

        psum = [es.enter_context(nc.psum_tensor("PS%d" % i, [128, 512], F32)) for i in range(8)]
        psb = [Buf("PS%d" % i) for i in range(8)]
        st = {"ps": 0, "scr": 0, "sm2": 0, "at": 0}

        ps_first = {}

        def ps_alloc():
            i = st["ps"] % 8
            st["ps"] += 1
            ps_first[id(psb[i])] = True
            return psum[i], psb[i]

        def MM(pb, out, lhsT, rhs, reads):
            first = ps_first[id(pb)]
            ps_first[id(pb)] = False
            P.op("tensor", OPC("matmul", out, lhsT=lhsT, rhs=rhs, start=first, stop=True, skip_group_check=True),
                 reads=reads, writes=[pb])

        class Scr:
            pass

        def scr_alloc():
            i = st["scr"] % NSCR
            st["scr"] += 1
            old = SCRb[i]
            nb = Buf("SCR%d_%d" % (i, st["scr"]), aliases=[old] + old.aliases)
            old.dead = True
            SCRb[i] = nb
            return SCR[:, i, :], nb

        def as_bf(ap, n):
            return ap.bitcast(BF16)[:, 0:n]

        ring = {"head": 0, "live": []}

        def ring_alloc(n):
            start = ring["head"]
            if start + n > RING_ELEMS:
                start = 0
            end = start + n
            aliases = []
            keep = []
            for (s0, e0, b0, rel) in ring["live"]:
                if s0 < end and start < e0:
                    if not rel[0]:
                        return None
                    aliases.append(b0)
                else:
                    keep.append((s0, e0, b0, rel))
            for b0 in aliases:
                b0.dead = True
            al = []
            for b0 in aliases:
                al.append(b0)
                al.extend(b0.aliases)
            nb = Buf("W@%d" % start, aliases=al)
            rel = [False]
            keep.append((start, end, nb, rel))
            ring["live"] = keep
            ring["head"] = end
            return start, nb, rel

        class WTile:
            def __init__(self, src_ap, shape):
                self.src = src_ap
                self.shape = shape
                self.ap = None
                self.buf = None
                self.rel = None

        wstream = []
        wcur = {"i": 0}

        def wfill():
            while wcur["i"] < len(wstream):
                wt = wstream[wcur["i"]]
                a, b = wt.shape
                r = ring_alloc(a * b)
                if r is None:
                    return
                start, nb, rel = r
                wt.ap = RING[:, start:start + a * b].rearrange("p (a b) -> p a b", a=a)
                wt.buf = nb
                wt.rel = rel
                P.op("gpsimd", OPC("dma_start", out=wt.ap, in_=wt.src),
                     writes=[nb], dma="w")
                wcur["i"] += 1

        def wneed(wt):
            if wt.buf is None:
                wfill()
            assert wt.buf is not None, "ring too small / stream order"
            return wt

        def wrelease(wt):
            wt.rel[0] = True
            wfill()

        def ffn_tiles(l):
            gu = []
            for fg in range(FC // 2):
                cs = slice(fg * 256, (fg + 1) * 256)
                g = WTile(wg_d[l].rearrange("(k p) f -> p k f", p=128)[:, :, cs], (KC, 256))
                u = WTile(wu_d[l].rearrange("(k p) f -> p k f", p=128)[:, :, cs], (KC, 256))
                gu.append((g, u))
            dn = []
            for half in range(2):
                parts = []
                for q in range(2):
                    src = wd_d[l].rearrange("(f p) d -> p f d", p=128)[:, q * 11:(q + 1) * 11, half * 512:(half + 1) * 512]
                    parts.append(WTile(src, (11, 512)))
                dn.append(parts)
            return gu, dn

        def mix_tiles():
            wi = []
            for g in range(6):
                wi.append(WTile(win_d.rearrange("(k p) c -> p k c", p=128)[:, :, g * 512:(g + 1) * 512], (KC, 512)))
            wo = []
            for half in range(2):
                wo.append(WTile(wout_d.rearrange("(k p) c -> p k c", p=128)[:, :, half * 512:(half + 1) * 512], (KC, 512)))
            return wi, wo

        plan = []
        for t in range(ntiles):
            plan.append((ffn_tiles(0), mix_tiles(), ffn_tiles(1)))

        def push_ffn(ft):
            gu, dn = ft
            for g, u in gu:
                wstream.extend([g, u])
            for parts in dn:
                wstream.extend(parts)

        def push_mix(mt):
            wi, wo = mt
            wstream.extend([wi[2], wi[1], wi[3], wi[5], wi[4], wi[0]])
            wstream.extend(wo)

        for pair in range(ntiles // 2):
            a, b = plan[2 * pair], plan[2 * pair + 1]
            push_ffn(a[0]); push_ffn(b[0])
            push_mix(a[1]); push_mix(b[1])
            push_ffn(a[2]); push_ffn(b[2])

        P.op("gpsimd", OPC("memset", IDF[:], 1.0), writes=[cB])
        P.op("gpsimd", OPC("affine_select", out=IDF[:], in_=IDF[:], pattern=[[-1, 128]],
                                                 compare_op=ALU.is_equal, fill=0.0, base=0,
                                                 channel_multiplier=1), reads=[cB], writes=[cB])
        P.op("gpsimd", OPC("memset", MASK[:], 1.0), writes=[cB])
        P.op("gpsimd", OPC("affine_select", out=MASK[:], in_=MASK[:], pattern=[[1, 128]],
                                                 compare_op=ALU.is_ge, fill=0.0, base=0,
                                                 channel_multiplier=-1), reads=[cB], writes=[cB])
        P.op("gpsimd", OPC("memset", MASK[0:64, 64:128], 0.0), reads=[cB], writes=[cB])
        P.op("gpsimd", OPC("memset", ONEB[:], 1.0), writes=[cB])
        P.op("gpsimd", OPC("memset", ONEF[:], 1.0), writes=[cB])
        P.op("vector", OPC("tensor_copy", out=IDB[:], in_=IDF[:]), reads=[cB], writes=[cB])
        rows = [(lb_d, 0, 2), (hg_d, 2, 1), (cb_d, 3, 1), (clg_d, 4, 1), (clb_d, 5, 1), (cw_d, 6, CW)]
        for (src, r0, n) in rows:
            P.op("sync", OPC("dma_start", out=PR[r0:r0 + n, :], in_=src),
                 writes=[cB], dma="c")
        pt_ps, pt_b = ps_alloc()
        for c in range(NH):
            MM(pt_b, pt_ps[:, c * 37:(c + 1) * 37], PR[0:37, c * 128:(c + 1) * 128], IDF[0:37, 0:37], [cB])
        P.op("vector", OPC("tensor_copy", out=PT[:].rearrange("p h r -> p (h r)"), in_=pt_ps[:, 0:NH * 37]),
             reads=[pt_b], writes=[cB])
        P.op("vector", OPC("tensor_tensor", out=PC[:, :, 3], in0=PT[:, :, 0], in1=PT[:, :, 1], op=ALU.subtract),
             reads=[cB], writes=[cB])
        P.op("scalar", OPC("activation", out=PC[:, :, 4], in_=PC[:, :, 3], func=AF.Tanh, scale=0.5),
             reads=[cB], writes=[cB])
        P.op("vector", OPC("tensor_scalar", out=PC[:, :, 0], in0=PC[:, :, 4], scalar1=0.25, scalar2=0.75,
                                                 op0=ALU.mult, op1=ALU.add), reads=[cB], writes=[cB])
        P.op("vector", OPC("tensor_scalar", out=PC[:, :, 1], in0=PC[:, :, 4], scalar1=-0.25, scalar2=0.25,
                                                 op0=ALU.mult, op1=ALU.add), reads=[cB], writes=[cB])
        P.op("vector", OPC("tensor_scalar", out=PC[:, :, 2], in0=PT[:, :, 2], scalar1=0.5, scalar2=None,
                                                 op0=ALU.mult), reads=[cB], writes=[cB])
        P.op("vector", OPC("tensor_scalar", out=W05[:], in0=PT[:, :, 6:6 + CW], scalar1=0.5, scalar2=None,
                                                 op0=ALU.mult), reads=[cB], writes=[cB])

        def newton_rsqrt(eng, y, v, tmp, bufs, n_iter=2):
            P.op(eng, OPC("tensor_scalar", out=y.bitcast(I32), in0=v.bitcast(I32), scalar1=1, scalar2=None,
                                                op0=ALU.arith_shift_right), reads=bufs, writes=bufs)
            P.op(eng, OPC("tensor_scalar", out=y.bitcast(I32), in0=y.bitcast(I32), scalar1=-1, scalar2=MAGIC,
                                                op0=ALU.mult, op1=ALU.add), reads=bufs, writes=bufs)
            for _ in range(n_iter):
                P.op(eng, OPC("tensor_tensor", out=tmp, in0=y, in1=y, op=ALU.mult), reads=bufs, writes=bufs)
                P.op(eng, OPC("tensor_tensor", out=tmp, in0=tmp, in1=v, op=ALU.mult), reads=bufs, writes=bufs)
                P.op(eng, OPC("tensor_scalar", out=tmp, in0=tmp, scalar1=-0.5, scalar2=1.5,
                                                    op0=ALU.mult, op1=ALU.add), reads=bufs, writes=bufs)
                P.op(eng, OPC("tensor_tensor", out=y, in0=y, in1=tmp, op=ALU.mult), reads=bufs, writes=bufs)

        def load_x(tile, xi):
            t0 = tile * NT
            for b in range(NB):
                P.op("sync", OPC("dma_start", out=X[xi][:, b, :], in_=x_d[t0 + b * 128:t0 + (b + 1) * 128, :]),
                     writes=[Xb[xi][b]], dma="x")

        def load_gb(li, slot):
            P.op("sync", OPC("dma_start", out=GB[:, slot, 0, :], in_=lng_d[li].partition_broadcast(128)),
                 writes=[GBb[slot]], dma="g")
            P.op("sync", OPC("dma_start", out=GB[:, slot, 1, :], in_=lnb_d[li].partition_broadcast(128)),
                 writes=[GBb[slot]], dma="g")

        def transposes(xi):
            for b in range(NB):
                xb_ap, xb_b = scr_alloc()
                xb16 = as_bf(xb_ap, D)
                P.op("scalar", OPC("copy", out=xb16, in_=X[xi][:, b, :]),
                     reads=[Xb[xi][b]], writes=[xb_b])
                ps, pb = ps_alloc()
                psT = ps[:].bitcast(BF16)
                for k in range(KC):
                    P.op("tensor", OPC("transpose", psT[:, k * 128:(k + 1) * 128],
                                                                                 xb16[:, k * 128:(k + 1) * 128], IDB[:]),
                         reads=[xb_b, cB], writes=[pb])
                P.op("scalar", OPC("copy", out=XT[:, :, b * 128:(b + 1) * 128], in_=psT.rearrange("p (k t) -> p k t", k=KC)),
                     reads=[pb], writes=[XTb[b]])

        def ln_finish(xi, b, slot, eps, last, tile):
            sm = SM[:, xi, b, :]
            bufs = [SMb[xi][b]]
            P.op("vector", OPC("bn_aggr", out=sm[:, 0:2], in_=STT[:, xi, b, :, :].rearrange("p g s -> p (g s)")),
                 reads=[STTb[xi][b]], writes=bufs)
            P.op("vector", OPC("tensor_scalar", out=sm[:, 2:3], in0=sm[:, 1:2], scalar1=eps, scalar2=None,
                                                     op0=ALU.add), reads=bufs, writes=bufs)
            newton_rsqrt("vector", sm[:, 3:4], sm[:, 2:3], sm[:, 4:5], bufs)
            P.op("vector", OPC("scalar_tensor_tensor", out=sm[:, 5:6], in0=sm[:, 0:1], scalar=-1.0, in1=sm[:, 3:4],
                                                            op0=ALU.mult, op1=ALU.mult), reads=bufs, writes=bufs)
            xblk = X[xi][:, b, :]
            P.op("scalar", OPC("activation", out=xblk, in_=xblk, func=AF.Identity, scale=sm[:, 3:4], bias=sm[:, 5:6]),
                 reads=[Xb[xi][b]] + bufs, writes=[Xb[xi][b]])
            P.op("gpsimd", OPC("tensor_tensor", out=xblk, in0=xblk, in1=GB[:, slot, 0, :], op=ALU.mult),
                 reads=[Xb[xi][b], GBb[slot]], writes=[Xb[xi][b]])
            P.op("gpsimd", OPC("tensor_tensor", out=xblk, in0=xblk, in1=GB[:, slot, 1, :], op=ALU.add),
                 reads=[Xb[xi][b], GBb[slot]], writes=[Xb[xi][b]])
            if last:
                t0 = tile * NT
                fin.append(P.op("sync", OPC("dma_start", out=out_d[t0 + b * 128:t0 + (b + 1) * 128, :], in_=xblk),
                                reads=[Xb[xi][b]], dma="o"))

        def resid_ln(xi, pdict, res_scale, slot, eps, last, tile, wrel):
            for half in range(2):
                for b in range(NB):
                    ps, pb = pdict(half, b)
                    xs = X[xi][:, b, half * 512:(half + 1) * 512]
                    P.op("vector", OPC("scalar_tensor_tensor", out=xs, in0=xs, scalar=res_scale, in1=ps[:],
                                                                                   op0=ALU.mult, op1=ALU.add),
                         reads=[Xb[xi][b], pb], writes=[Xb[xi][b]])
                    P.op("vector", OPC("bn_stats", out=STT[:, xi, b, half, :], in_=xs),
                         reads=[Xb[xi][b]], writes=[STTb[xi][b]])
                    if half == 1:
                        ln_finish(xi, b, slot, eps, last, tile)
                wrel(half)

        def ffn(xi, tiles, slot, last, tile):
            gu, dn = tiles
            for f in range(FC):
                HTb[f].aliases = [m for m in mix_alias]
            first = [True]
            for fg in range(FC // 2):
                g, u = gu[fg]
                wneed(g)
                wneed(u)
                for fi in range(2):
                    f = fg * 2 + fi
                    pg, pgb = ps_alloc()
                    pu, pub = ps_alloc()
                    for (wt, ps, pb) in ((g, pg, pgb), (u, pu, pub)):
                        for k in range(KC):
                            MM(pb, ps[:], wt.ap[:, k, fi * 128:(fi + 1) * 128], XT[:, k, :], [wt.buf] + XTb)
                    sg, sgb = scr_alloc()
                    P.op("scalar", OPC("activation", out=sg, in_=pg[:], func=AF.Silu),
                         reads=[pgb], writes=[sgb])
                    P.op("vector", OPC("tensor_tensor", out=HT[:, f, :], in0=sg, in1=pu[:], op=ALU.mult),
                         reads=[sgb, pub], writes=[HTb[f]])
                    if first[0]:
                        for m in mix_alias:
                            m.writer = None
                            m.readers = []
                        first[0] = False
                    HTb[f].aliases = []
                wrelease(g)
                wrelease(u)
            yield

            def pd(half, b):
                for part in dn[half]:
                    wneed(part)
                ps, pb = ps_alloc()
                for f in range(FC):
                    part = dn[half][f // 11]
                    MM(pb, ps[:], HT[:, f, b * 128:(b + 1) * 128], part.ap[:, f % 11, :], [HTb[f], part.buf])
                return ps, pb

            def wrel(half):
                for part in dn[half]:
                    wrelease(part)

            resid_ln(xi, pd, 2.0 * ALPHA, slot, 4.0 * LN_EPS, last, tile, wrel)

        def mixer(xi, tiles, slot, seq_start, par):
            wi, wo = tiles
            for m in mix_alias:
                m.aliases = [h for h in HTb]
            wf, wq, wv, wgo, wcv, wcg = wi[1], wi[0], wi[2], wi[3], wi[4], wi[5]
            NCH = NT // 64

            def rslot(ci):
                return ci if ci > 0 else (0 if par == 0 else NCH)

            def wslot(ci):
                return ci + 1 if ci < NCH - 1 else (NCH if par == 0 else 0)

            def proj_fm(wt, c):
                ps, pb = ps_alloc()
                for k in range(KC):
                    MM(pb, ps[:], wt.ap[:, k, c * 128:(c + 1) * 128], XT[:, k, :], [wt.buf] + XTb)
                return ps, pb

            if seq_start:
                P.op("gpsimd", OPC("memset", S[:], 0.0), writes=[Sb_])
                P.op("gpsimd", OPC("memset", SBS[:, rslot(0), :, :], 0.0), writes=[SBSb[rslot(0)]])
                for c in range(NH):
                    P.op("gpsimd", OPC("memset", UT[:, c, 0:HALO], 0.0), writes=[UTb[c]])

            for b in range(NB):
                Vb[b].aliases = list(CBb)
                KTOKb[b].aliases = list(CQb)
            wneed(wv)
            for b in range(NB):
                ps, pb = ps_alloc()
                for k in range(KC):
                    MM(pb, ps[:], XT[:, k, b * 128:(b + 1) * 128], wv.ap[:, k, :], [wv.buf, XTb[b]])
                P.op("scalar", OPC("copy", out=V[:, b, :], in_=ps[:]), reads=[pb], writes=[Vb[b]])
            wrelease(wv)
            wneed(wf)
            KH = []
            for h in range(NH):
                ps, pb = proj_fm(wf, h)
                fa, fb = scr_alloc()
                P.op("scalar", OPC("activation", out=fa, in_=ps[:], func=AF.Tanh, scale=0.5), reads=[pb], writes=[fb])
                P.op("vector", OPC("tensor_scalar", out=fa, in0=fa, scalar1=PC[:, h, 1:2], scalar2=PC[:, h, 0:1],
                                   op0=ALU.mult, op1=ALU.add), reads=[fb, cB], writes=[fb])
                for c in range(NCH):
                    cs = slice(c * 64, (c + 1) * 64)
                    P.op("vector", OPC("tensor_tensor_scan", out=BC[:, h, cs], data0=fa[:, cs], data1=ONEF[:, 0:64], initial=1.0,
                                       op0=ALU.mult, op1=ALU.mult), reads=[fb, cB], writes=[BCb[h]])
                rb, rbb = scr_alloc()
                P.op("vector", OPC("reciprocal", out=rb, in_=BC[:, h, :]), reads=[BCb[h]], writes=[rbb])
                P.op("vector", OPC("tensor_scalar", out=fa, in0=fa, scalar1=-1.0, scalar2=1.0, op0=ALU.mult, op1=ALU.add),
                     reads=[fb], writes=[fb])
                P.op("vector", OPC("tensor_tensor", out=KT[:, h, :], in0=fa, in1=rb, op=ALU.mult), reads=[fb, rbb], writes=[KTb[h]])
                P.op("gpsimd", OPC("tensor_tensor", out=rb.rearrange("p (c t) -> p c t", t=64), in0=rb.rearrange("p (c t) -> p c t", t=64),
                                   in1=BC[:, h, :].rearrange("p (c t) -> p c t", t=64)[:, :, 63:64].to_broadcast([128, NCH, 64]),
                                   op=ALU.mult), reads=[rbb, BCb[h]], writes=[rbb])
                kh, khb = scr_alloc()
                kh16 = as_bf(kh, NT)
                P.op("gpsimd", OPC("tensor_tensor", out=kh16, in0=fa, in1=rb, op=ALU.mult), reads=[fb, rbb], writes=[khb])
                KH.append((kh16, khb))
            wrelease(wf)
            wneed(wgo)
            for h in range(NH):
                ps, pb = proj_fm(wgo, h)
                P.op("scalar", OPC("activation", out=THG[:, h, :], in_=ps[:], func=AF.Tanh, scale=0.5), reads=[pb], writes=[THGb[h]])
            wrelease(wgo)
            THC = []
            wneed(wcg)
            for c in range(NH):
                ps, pb = proj_fm(wcg, c)
                ta, tb = scr_alloc()
                P.op("scalar", OPC("activation", out=ta, in_=ps[:], func=AF.Tanh, scale=0.5), reads=[pb], writes=[tb])
                THC.append((ta, tb))
            wrelease(wcg)
            wneed(wcv)
            for c in range(NH):
                ps, pb = proj_fm(wcv, c)
                ta, tb = THC[c]
                P.op("vector", OPC("scalar_tensor_tensor", out=UT[:, c, HALO:HALO + NT], in0=ta, scalar=1.0, in1=ps[:],
                                   op0=ALU.add, op1=ALU.mult), reads=[tb, pb], writes=[UTb[c]])
            wrelease(wcv)
            wneed(wq)
            for h in range(NH):
                ps, pb = proj_fm(wq, h)
                P.op("vector", OPC("tensor_tensor", out=QT[:, h, :], in0=ps[:], in1=BC[:, h, :], op=ALU.mult),
                     reads=[pb, BCb[h]], writes=[QTb[h]])
            wrelease(wq)
            for b in range(NB):
                ps, pb = ps_alloc()
                psT = ps[:].bitcast(BF16)
                for h in range(NH):
                    kh16, khb = KH[h]
                    P.op("tensor", OPC("transpose", psT[:, h * 128:(h + 1) * 128], kh16[:, b * 128:(b + 1) * 128], IDB[:]),
                         reads=[khb, cB], writes=[pb])
                P.op("scalar", OPC("copy", out=KTOK[:, b, :], in_=psT[:, 0:512]), reads=[pb], writes=[KTOKb[b]])
            yield

            def conv_mm(c):
                cps, cpb = ps_alloc()
                dg16 = dgb = None
                for j in range(CW):
                    if j % 8 == 0:
                        dg, dgb = scr_alloc()
                        dg16 = as_bf(dg, 1024)
                        n = min(8, CW - j)
                        P.op("gpsimd", OPC("tensor_tensor", out=dg16[:, 0:n * 128].rearrange("p (j t) -> p j t", j=n),
                                           in0=IDF[:].rearrange("p (o t) -> p o t", o=1).to_broadcast([128, n, 128]),
                                           in1=W05[:, c, j:j + n].rearrange("p (j o) -> p j o", o=1).to_broadcast([128, n, 128]),
                                           op=ALU.mult), reads=[cB], writes=[dgb])
                    jj = j % 8
                    MM(cpb, cps[:], dg16[:, jj * 128:(jj + 1) * 128], UT[:, c, j:j + NT], [dgb, UTb[c]])
                P.op("scalar", OPC("activation", out=CS[:, c, :], in_=cps[:], func=AF.Identity, bias=PT[:, c, 3:4]),
                     reads=[cpb, cB], writes=[CSb[c]])

            def conv_fin(c):
                P.op("scalar", OPC("copy", out=CB[:, c, :], in_=CS[:, c, :]), reads=[CSb[c]], writes=[CBb[c]])
                P.op("scalar", OPC("activation", out=CQ[:, c, :], in_=CS[:, c, :], func=AF.Square), reads=[CSb[c]], writes=[CQb[c]])
                P.op("gpsimd", OPC("tensor_copy", out=UT[:, c, 0:HALO], in_=UT[:, c, NT:NT + HALO]), reads=[UTb[c]], writes=[UTb[c]])

            def stage_a(b):
                bs = slice(b * 128, (b + 1) * 128)
                sc, scb = ps_alloc()
                for h in range(NH):
                    MM(scb, sc[:, h * 128:(h + 1) * 128], KT[:, h, bs], QT[:, h, bs], [KTb[h], QTb[h]])
                P.op("vector", OPC("tensor_tensor", out=AT[b][:], in0=sc[:].rearrange("p (h t) -> p h t", h=NH),
                                   in1=MASK[:].rearrange("p (o t) -> p o t", o=1).to_broadcast([128, NH, 128]), op=ALU.mult),
                     reads=[scb, cB], writes=[ATb[b]])
                for c2 in range(2):
                    p_, pb_ = ps_alloc()
                    rs = slice(c2 * 64, (c2 + 1) * 64)
                    for h in range(NH):
                        MM(pb_, p_[:, h * 128:(h + 1) * 128], KTOK[rs, b, h * 128:(h + 1) * 128], V[rs, b, h * 128:(h + 1) * 128],
                           [KTOKb[b], Vb[b]])
                    ci = b * 2 + c2
                    for h in range(NH):
                        P.op("vector", OPC("scalar_tensor_tensor", out=S[:, h, :], in0=S[:, h, :], scalar=BC[:, h, ci * 64 + 63:ci * 64 + 64],
                                           in1=p_[:, h * 128:(h + 1) * 128], op0=ALU.mult, op1=ALU.add),
                             reads=[Sb_, BCb[h], pb_], writes=[Sb_])
                    nxt = wslot(ci)
                    P.op("scalar", OPC("copy", out=SBS[:, nxt, :, :], in_=S[:]), reads=[Sb_], writes=[SBSb[nxt]])

            for b in range(NB):
                stage_a(b)
            conv_mm(0)
            conv_mm(1)

            OQ = {}

            def rms_stats(b):
                oq16, oqb = OQ[b]
                ss, ssb = ps_alloc()
                for h in range(NH):
                    MM(ssb, ss[:, h:h + 1], oq16[:, h * 128:(h + 1) * 128], ONEB[:, 0:1], [oqb, cB])
                sm = SM2[:, b, :]
                smb = [SM2b[b]]
                P.op("vector", OPC("tensor_scalar", out=sm[:, 0:4], in0=ss[:, 0:4], scalar1=1.0 / 128.0, scalar2=RMS_EPS,
                                   op0=ALU.mult, op1=ALU.add), reads=[ssb], writes=smb)
                newton_rsqrt("vector", sm[:, 4:8], sm[:, 0:4], sm[:, 8:12], smb)

            for b in range(NB):
                o_ps, o_b = ps_alloc()
                for h in range(NH):
                    MM(o_b, o_ps[:, h * 128:(h + 1) * 128], V[:, b, h * 128:(h + 1) * 128], AT[b][:, h, :], [Vb[b], ATb[b]])
                for c2 in range(2):
                    ci = b * 2 + c2
                    ts_ = slice(b * 128 + c2 * 64, b * 128 + (c2 + 1) * 64)
                    for h in range(NH):
                        MM(o_b, o_ps[:, h * 128 + c2 * 64:h * 128 + (c2 + 1) * 64], SBS[:, rslot(ci), h, :], QT[:, h, ts_],
                           [SBSb[rslot(ci)], QTb[h]])
                P.op("scalar", OPC("copy", out=OS[:, b, :], in_=o_ps[:]), reads=[o_b], writes=[OSb[b]])
                oq, oqb = scr_alloc()
                oq16 = as_bf(oq, 512)
                P.op("scalar", OPC("activation", out=oq16, in_=o_ps[:], func=AF.Square), reads=[o_b], writes=[oqb])
                OQ[b] = (oq16, oqb)
                if b > 0:
                    rms_stats(b - 1)
            rms_stats(NB - 1)

            def rms_apply(b):
                bs = slice(b * 128, (b + 1) * 128)
                sm = SM2[:, b, :]
                lt, ltb = scr_alloc()
                ltv = lt.rearrange("p (h t) -> p h t", h=NH)
                P.op("vector", OPC("tensor_copy", out=ltv, in_=sm[:, 4:8].rearrange("p (h o) -> p h o", o=1).to_broadcast([128, NH, 128])),
                     reads=[SM2b[b]], writes=[ltb])
                rb_ps, rb_b = ps_alloc()
                for h in range(NH):
                    MM(rb_b, rb_ps[:, h * 128:(h + 1) * 128], ltv[:, h, :], IDF[:], [ltb, cB])
                os_ = OS[:, b, :]
                for h in range(NH):
                    P.op("vector", OPC("scalar_tensor_tensor", out=os_[:, h * 128:(h + 1) * 128], in0=os_[:, h * 128:(h + 1) * 128],
                                       scalar=PC[:, h, 2:3], in1=rb_ps[:, h * 128:(h + 1) * 128], op0=ALU.mult, op1=ALU.mult),
                         reads=[OSb[b], rb_b, cB], writes=[OSb[b]])
                P.op("vector", OPC("scalar_tensor_tensor", out=CT[:, 0:NH, bs], in0=THG[:, :, bs], scalar=1.0,
                                   in1=os_.rearrange("p (h t) -> p h t", h=NH), op0=ALU.add, op1=ALU.mult),
                     reads=THGb + [OSb[b]], writes=[CTb[c][b] for c in range(NH)])

            for c in range(NH):
                CBb[c].aliases = list(Vb)
                CQb[c].aliases = list(KTOKb)
            conv_fin(0)
            conv_fin(1)
            for c in (2, 3):
                conv_mm(c)
                conv_fin(c)
                rms_apply(2 * (c - 2))
                rms_apply(2 * (c - 2) + 1)

            stp, stb = ps_alloc()
            for b in range(NB):
                bs = slice(b * 128, (b + 1) * 128)
                for (q, src, srcb) in ((0, CB, CBb), (1, CQ, CQb)):
                    for c in range(NH):
                        MM(stb, stp[:, b * 2 + q:b * 2 + q + 1], src[:, c, bs], ONEB[:, 0:1], [srcb[c], cB])
            cm, cmb = scr_alloc()
            P.op("vector", OPC("tensor_scalar", out=cm[:, 0:2 * NB], in0=stp[:, 0:2 * NB], scalar1=1.0 / 512.0, scalar2=None, op0=ALU.mult),
                 reads=[stb], writes=[cmb])
            cmv = cm[:, 0:2 * NB].rearrange("p (b q) -> p b q", q=2)
            mean = cmv[:, :, 0]
            ex2 = cmv[:, :, 1]
            P.op("vector", OPC("tensor_tensor", out=cm[:, 16:16 + NB], in0=mean, in1=mean, op=ALU.mult), reads=[cmb], writes=[cmb])
            P.op("vector", OPC("tensor_tensor", out=cm[:, 8:8 + NB], in0=ex2, in1=cm[:, 16:16 + NB], op=ALU.subtract), reads=[cmb], writes=[cmb])
            P.op("vector", OPC("tensor_scalar", out=cm[:, 8:8 + NB], in0=cm[:, 8:8 + NB], scalar1=LN_EPS, scalar2=None, op0=ALU.add),
                 reads=[cmb], writes=[cmb])
            newton_rsqrt("vector", cm[:, 12:12 + NB], cm[:, 8:8 + NB], cm[:, 16:16 + NB], [cmb])
            P.op("vector", OPC("scalar_tensor_tensor", out=cm[:, 20:20 + NB], in0=mean, scalar=-1.0, in1=cm[:, 12:12 + NB],
                               op0=ALU.mult, op1=ALU.mult), reads=[cmb], writes=[cmb])
            ra_ps, ra_b = ps_alloc()
            rn_ps, rn_b = ps_alloc()
            for (col0, dst, dstb) in ((12, ra_ps, ra_b), (20, rn_ps, rn_b)):
                lt, ltb = scr_alloc()
                ltv = lt.rearrange("p (b t) -> p b t", b=NB)
                P.op("vector", OPC("tensor_copy", out=ltv, in_=cm[:, col0:col0 + NB].rearrange("p (b o) -> p b o", o=1).to_broadcast([128, NB, 128])),
                     reads=[cmb], writes=[ltb])
                for b in range(NB):
                    MM(dstb, dst[:, b * 128:(b + 1) * 128], ltv[:, b, :], IDF[:], [ltb, cB])
            for c in range(NH):
                P.op("vector", OPC("tensor_tensor", out=CS[:, c, :], in0=CS[:, c, :], in1=ra_ps[:], op=ALU.mult),
                     reads=[CSb[c], ra_b], writes=[CSb[c]])
                P.op("vector", OPC("tensor_tensor", out=CS[:, c, :], in0=CS[:, c, :], in1=rn_ps[:], op=ALU.add),
                     reads=[CSb[c], rn_b], writes=[CSb[c]])
                P.op("scalar", OPC("activation", out=CT[:, NH + c, :], in_=CS[:, c, :], func=AF.Silu, scale=PT[:, c, 4:5], bias=PT[:, c, 5:6]),
                     reads=[CSb[c], cB], writes=[CTb[NH + c][b] for b in range(NB)])

            def pd(half, b):
                wneed(wo[half])
                ps, pb = ps_alloc()
                for k in range(KC):
                    MM(pb, ps[:], CT[:, k, b * 128:(b + 1) * 128], wo[half].ap[:, k, :], [CTb[k][b], wo[half].buf])
                return ps, pb

            def wrel(half):
                wrelease(wo[half])

            resid_ln(xi, pd, ALPHA, slot, LN_EPS, False, 0, wrel)

        assert ntiles % 2 == 0
        wfill()
        load_x(0, 0)
        load_x(1, 1)
        sa, sb_ = 0, 1
        load_gb(0, sa)
        transposes(0)
        for pair in range(ntiles // 2):
            tA, tB = 2 * pair, 2 * pair + 1
            fA, mA, gA = plan[tA]
            fB, mB, gB = plan[tB]
            load_gb(1, sb_)
            g = ffn(0, fA, sa, False, tA); next(g)
            transposes(1)
            next(g, None)
            g = ffn(1, fB, sa, False, tB); next(g)
            transposes(0)
            next(g, None)
            load_gb(2, sa)
            g = mixer(0, mA, sb_, (tA % tiles_per_seq) == 0, 0); next(g)
            transposes(1)
            next(g, None)
            g = mixer(1, mB, sb_, (tB % tiles_per_seq) == 0, 1); next(g)
            transposes(0)
            next(g, None)
            load_gb(0, sb_)
            g = ffn(0, gA, sa, True, tA); next(g)
            transposes(1)
            next(g, None)
            if tA + 2 < ntiles:
                load_x(tA + 2, 0)
            g = ffn(1, gB, sa, True, tB); next(g)
            if tA + 2 < ntiles:
                transposes(0)
            next(g, None)
            if tB + 2 < ntiles:
                load_x(tB + 2, 1)
            sa, sb_ = sb_, sa
        print('sbuf bytes remaining/partition:', nc.sbuf_bytes_remaining)
        P.emit(final_waits=fin)
    return nc


_NC_CACHE = {}


def _get_nc(nseq, T):
    key = (nseq, T)
    if key not in _NC_CACHE:
        _NC_CACHE[key] = build_nc(nseq, T)
    return _NC_CACHE[key]


def kernel(**inputs):
    x = np.asarray(inputs["x"], dtype=np.float32)
    B, T, _ = x.shape
    n = 8
    nseq = B // n
    nc = _get_nc(nseq, T)
    shared = {}
    for k in ("ffn1_w_gate", "ffn1_w_up", "ffn1_w_down", "ffn2_w_gate", "ffn2_w_up", "ffn2_w_down", "w_in", "w_out"):
        a = np.asarray(inputs[k], dtype=np.float32)
        shared[k] = np.ascontiguousarray(a.reshape(a.shape[-2], a.shape[-1]))
    for k in ("ln1_g", "ln1_b", "ln2_g", "ln2_b", "ln3_g", "ln3_b", "hgrn_norm_g", "conv_b", "conv_ln_g", "conv_ln_b"):
        a = np.asarray(inputs[k], dtype=np.float32)
        shared[k] = np.ascontiguousarray(a.reshape(1, a.shape[-1]))
    shared["lb_logits"] = np.ascontiguousarray(np.asarray(inputs["lb_logits"], dtype=np.float32).reshape(2, 512))
    shared["conv_w"] = np.ascontiguousarray(np.asarray(inputs["conv_w"], dtype=np.float32).reshape(CW, 512))
    in_maps = []
    for c in range(n):
        m = dict(shared)
        m["x"] = np.ascontiguousarray(x[c * nseq:(c + 1) * nseq].reshape(nseq * T, D))
        in_maps.append(m)
    res = run_bass_kernel_spmd(nc, in_maps, core_ids=list(range(n)))
    out = np.concatenate([np.asarray(r["out"]).reshape(nseq, T, D) for r in res.results], axis=0)
    return out.astype(np.float32, copy=False)
```

```python
import numpy as np
from contextlib import ExitStack
import concourse.bass as bass
import concourse.mybir as mybir
from concourse.bass_utils import run_bass_kernel_spmd

F32 = mybir.dt.float32
BF16 = mybir.dt.bfloat16
I32 = mybir.dt.int32
AF = mybir.ActivationFunctionType
ALU = mybir.AluOpType

D = 1024
KC = 8
DFF = 2816
FC = 22
DIN = 3072
NH = 4
CW = 31
HALO = CW - 1
ALPHA = 2.0 ** 0.25
LN_EPS = 1e-5
RMS_EPS = 1e-6
NT = 512
NB = NT // 128
RING_ELEMS = 22 * 1024
NSCR = 14
MAGIC = 0x5F3759DF

ENGINES = ("tensor", "vector", "scalar", "gpsimd", "sync")


class Buf:
    __slots__ = ("name", "writer", "readers", "aliases", "dead")

    def __init__(self, name, aliases=()):
        self.name = name
        self.writer = None
        self.readers = []
        self.aliases = list(aliases)
        self.dead = False


class Op:
    __slots__ = ("eng", "fn", "dma", "deps", "signal", "sem", "val", "waits", "pre")

    def __init__(self, eng, fn, dma):
        self.eng = eng
        self.fn = fn
        self.dma = dma
        self.deps = []
        self.signal = False
        self.sem = None
        self.val = 0
        self.waits = []


def OPC(name, *args, **kwargs):
    return lambda e: getattr(e, name)(*args, **kwargs)


class Prog:
    def __init__(self, nc):
        self.nc = nc
        self.ops = []

    def op(self, eng, fn, reads=(), writes=(), dma=None):
        o = Op(eng, fn, dma)
        deps = {}
        for b in reads:
            assert not b.dead, "read of dead buffer " + b.name
            for bb in [b] + b.aliases:
                if bb.writer is not None:
                    deps[id(bb.writer)] = bb.writer
        for b in writes:
            assert not b.dead, "write of dead buffer " + b.name
            for bb in [b] + b.aliases:
                if bb.writer is not None:
                    deps[id(bb.writer)] = bb.writer
                for r in bb.readers:
                    deps[id(r)] = r
        for d in deps.values():
            if d is o:
                continue
            if d.eng == "tensor" and eng == "tensor" and d.dma is None and dma is None:
                continue
            o.deps.append(d)
            d.signal = True
        for b in reads:
            b.readers.append(o)
        for b in writes:
            b.readers = []
            for bb in b.aliases:
                bb.readers = []
                bb.writer = None
            b.aliases = []
            b.writer = o
        self.ops.append(o)
        return o

    def emit(self, final_waits=()):
        nc = self.nc
        with ExitStack() as es:
            sems = {}

            def get_sem(key):
                if key not in sems:
                    sems[key] = es.enter_context(nc.semaphore("s_" + key))
                return sems[key]

            counts = {}
            KDMA = 8
            for o in final_waits:
                o.signal = True
            for o in self.ops:
                o.pre = None
                if o.dma:
                    n = counts.get("d_" + o.dma, 0)
                    counts["d_" + o.dma] = n + 1
                    o.signal = True
                    o.sem = get_sem("d_%s_%d" % (o.dma, n % KDMA))
                    o.val = 16 * (n // KDMA + 1)
                    if n >= KDMA:
                        o.pre = (o.sem, 16 * (n // KDMA))
                    continue
                if not o.signal:
                    continue
                key = "e_" + o.eng
                counts[key] = counts.get(key, 0) + 1
                o.sem = get_sem(key)
                o.val = counts[key]
            self.counts = counts
            known = {e: {} for e in ENGINES}
            per_eng = {e: [] for e in ENGINES}
            for o in self.ops:
                need = {}
                if o.pre is not None:
                    need[id(o.pre[0])] = o.pre
                for d in o.deps:
                    k = id(d.sem)
                    if k not in need or need[k][1] < d.val:
                        need[k] = (d.sem, d.val)
                kn = known[o.eng]
                for k, (s, v) in need.items():
                    if kn.get(k, 0) >= v:
                        continue
                    kn[k] = v
                    o.waits.append((s, v))
                per_eng[o.eng].append(o)
            with nc.Block() as block:
                def mk(engname):
                    def body(eng):
                        for o in per_eng[engname]:
                            for (s, v) in o.waits:
                                eng.wait_ge(s, v)
                            ins = o.fn(eng)
                            if o.signal:
                                ins.then_inc(o.sem, 16 if o.dma else 1)
                        if engname == "sync":
                            for o in final_waits:
                                eng.wait_ge(o.sem, o.val)
                    return body
                for e in ENGINES:
                    if per_eng[e] or e == "sync":
                        getattr(block, e)(mk(e))


def build_nc(nseq, T):
    ntok = nseq * T
    tiles_per_seq = T // NT
    ntiles = nseq * tiles_per_seq
    nc = bass.Bass("TRN2", target_bir_lowering=False)

    def din(name, shape):
        return nc.dram_tensor(name, list(shape), F32, kind="ExternalInput").ap()

    x_d = din("x", [ntok, D])
    wg_d = [din("ffn1_w_gate", [D, DFF]), din("ffn2_w_gate", [D, DFF])]
    wu_d = [din("ffn1_w_up", [D, DFF]), din("ffn2_w_up", [D, DFF])]
    wd_d = [din("ffn1_w_down", [DFF, D]), din("ffn2_w_down", [DFF, D])]
    win_d = din("w_in", [D, DIN])
    wout_d = din("w_out", [D, D])
    lng_d = [din("ln1_g", [1, D]), din("ln2_g", [1, D]), din("ln3_g", [1, D])]
    lnb_d = [din("ln1_b", [1, D]), din("ln2_b", [1, D]), din("ln3_b", [1, D])]
    lb_d = din("lb_logits", [2, 512])
    hg_d = din("hgrn_norm_g", [1, 512])
    cw_d = din("conv_w", [CW, 512])
    cb_d = din("conv_b", [1, 512])
    clg_d = din("conv_ln_g", [1, 512])
    clb_d = din("conv_ln_b", [1, 512])
    out_d = nc.dram_tensor("out", [ntok, D], F32, kind="ExternalOutput").ap()

    P = Prog(nc)
    fin = []
    with ExitStack() as es:
        def sb(name, shape, dt):
            return es.enter_context(nc.sbuf_tensor(name, list(shape), dt))

        X = [sb("X%d" % i, [128, NB, D], F32) for i in range(2)]
        Xb = [[Buf("X%d_%d" % (i, b)) for b in range(NB)] for i in range(2)]
        XT = sb("XT", [128, KC, NT], BF16)
        XTb = [Buf("XT%d" % b) for b in range(NB)]
        HT = sb("HT", [128, FC, NT], BF16)
        HTb = [Buf("HT%d" % f) for f in range(FC)]
        HTf = HT[:].rearrange("p f t -> p (f t)")
        BC = HTf[:, 0:4096].bitcast(F32).rearrange("p (h t) -> p h t", h=NH)
        CS = HTf[:, 4096:8192].bitcast(F32).rearrange("p (h t) -> p h t", h=NH)
        THG = HTf[:, 8192:10240].rearrange("p (h t) -> p h t", h=NH)
        BCb = [Buf("BC%d" % h, aliases=HTb) for h in range(NH)]
        CSb = [Buf("CS%d" % h, aliases=HTb) for h in range(NH)]
        THGb = [Buf("THG%d" % h, aliases=HTb) for h in range(NH)]
        mix_alias = BCb + CSb + THGb

        KT = sb("KT", [128, NH, NT], BF16)
        KTb = [Buf("KT%d" % h) for h in range(NH)]
        QT = sb("QT", [128, NH, NT], BF16)
        QTb = [Buf("QT%d" % h) for h in range(NH)]
        V = sb("V", [128, NB, 512], BF16)
        Vb = [Buf("V%d" % b) for b in range(NB)]
        KTOK = sb("KTOK", [128, NB, 512], BF16)
        KTOKb = [Buf("KTOK%d" % b) for b in range(NB)]
        AT = [sb("AT%d" % i, [128, NH, 128], BF16) for i in range(NB)]
        ATb = [Buf("AT%d" % i) for i in range(NB)]
        S = sb("S", [128, NH, 128], F32)
        Sb_ = Buf("S")
        SBS = sb("SBS", [128, NT // 64 + 1, NH, 128], BF16)
        SBSb = [Buf("SBS%d" % i) for i in range(NT // 64 + 1)]
        UT = sb("UT", [128, NH, HALO + NT], BF16)
        UTb = [Buf("UT%d" % c) for c in range(NH)]
        CB = V
        CBb = [Buf("CB%d" % c) for c in range(NH)]
        CQ = KTOK
        CQb = [Buf("CQ%d" % c) for c in range(NH)]
        OS = sb("OS", [128, NB, 512], F32)
        OSb = [Buf("OS%d" % b) for b in range(NB)]
        CT = sb("CT", [128, KC, NT], BF16)
        CTb = [[Buf("CT%d_%d" % (c, b)) for b in range(NB)] for c in range(KC)]
        GB = sb("GB", [128, 2, 2, D], F32)
        GBb = [Buf("GB%d" % i) for i in range(2)]
        SCR = sb("SCR", [128, NSCR, 512], F32)
        SCRb = [Buf("SCR%d" % i) for i in range(NSCR)]
        RING = sb("RING", [128, RING_ELEMS], BF16)
        IDF = sb("IDF", [128, 128], F32)
        IDB = sb("IDB", [128, 128], BF16)
        ONEB = sb("ONEB", [128, 128], BF16)
        ONEF = sb("ONEF", [128, 64], F32)
        MASK = sb("MASK", [128, 128], F32)
        PR = sb("PR", [37, 512], F32)
        PT = sb("PT", [128, NH, 37], F32)
        PC = sb("PC", [128, NH, 8], F32)
        W05 = sb("W05", [128, NH, CW], F32)
        STT = sb("STT", [128, 2, NB, 2, 6], F32)
        SM = sb("SM", [128, 2, NB, 8], F32)
        SM2 = sb("SM2", [128, 4, 16], F32)
        cB = Buf("consts")
        STTb = [[Buf("STT%d_%d" % (i, b)) for b in range(NB)] for i in range(2)]
        SMb = [[Buf("SM%d_%d" % (i, b)) for b in range(NB)] for i in range(2)]
        SM2b = [Buf("SM2_%d" % i) for i in range(4)]

        psum = [es.enter_context(nc.psum_tensor("PS%d" % i, [128, 512], F32)) for i in range(8)]
        psb = [Buf("PS%d" % i) for i in range(8)]
        st = {"ps": 0, "scr": 0, "sm2": 0, "at": 0}

        ps_first = {}

        def ps_alloc():
            i = st["ps"] % 8
            st["ps"] += 1
            ps_first[id(psb[i])] = True
            return psum[i], psb[i]

        def MM(pb, out, lhsT, rhs, reads):
            first = ps_first[id(pb)]
            ps_first[id(pb)] = False
            P.op("tensor", OPC("matmul", out, lhsT=lhsT, rhs=rhs, start=first, stop=True, skip_group_check=True),
                 reads=reads, writes=[pb])

        class Scr:
            pass

        def scr_alloc():
            i = st["scr"] % NSCR
            st["scr"] += 1
            old = SCRb[i]
            nb = Buf("SCR%d_%d" % (i, st["scr"]), aliases=[old] + old.aliases)
            old.dead = True
            SCRb[i] = nb
            return SCR[:, i, :], nb

        def as_bf(ap, n):
            return ap.bitcast(BF16)[:, 0:n]

        ring = {"head": 0, "live": []}

        def ring_alloc(n):
            start = ring["head"]
            if start + n > RING_ELEMS:
                start = 0
            end = start + n
            aliases = []
            keep = []
            for (s0, e0, b0, rel) in ring["live"]:
                if s0 < end and start < e0:
                    if not rel[0]:
                        return None
                    aliases.append(b0)
                else:
                    keep.append((s0, e0, b0, rel))
            for b0 in aliases:
                b0.dead = True
            al = []
            for b0 in aliases:
                al.append(b0)
                al.extend(b0.aliases)
            nb = Buf("W@%d" % start, aliases=al)
            rel = [False]
            keep.append((start, end, nb, rel))
            ring["live"] = keep
            ring["head"] = end
            return start, nb, rel

        class WTile:
            def __init__(self, src_ap, shape):
                self.src = src_ap
                self.shape = shape
                self.ap = None
                self.buf = None
                self.rel = None

        wstream = []
        wcur = {"i": 0}

        def wfill():
            while wcur["i"] < len(wstream):
                wt = wstream[wcur["i"]]
                a, b = wt.shape
                r = ring_alloc(a * b)
                if r is None:
                    return
                start, nb, rel = r
                wt.ap = RING[:, start:start + a * b].rearrange("p (a b) -> p a b", a=a)
                wt.buf = nb
                wt.rel = rel
                P.op("gpsimd", OPC("dma_start", out=wt.ap, in_=wt.src),
                     writes=[nb], dma="w")
                wcur["i"] += 1

        def wneed(wt):
            if wt.buf is None:
                wfill()
            assert wt.buf is not None, "ring too small / stream order"
            return wt

        def wrelease(wt):
            wt.rel[0] = True
            wfill()

        def ffn_tiles(l):
            gu = []
            for fg in range(FC // 2):
                cs = slice(fg * 256, (fg + 1) * 256)
                g = WTile(wg_d[l].rearrange("(k p) f -> p k f", p=128)[:, :, cs], (KC, 256))
                u = WTile(wu_d[l].rearrange("(k p) f -> p k f", p=128)[:, :, cs], (KC, 256))
                gu.append((g, u))
            dn = []
            for half in range(2):
                parts = []
                for q in range(2):
                    src = wd_d[l].rearrange("(f p) d -> p f d", p=128)[:, q * 11:(q + 1) * 11, half * 512:(half + 1) * 512]
                    parts.append(WTile(src, (11, 512)))
                dn.append(parts)
            return gu, dn

        def mix_tiles():
            wi = []
            for g in range(6):
                wi.append(WTile(win_d.rearrange("(k p) c -> p k c", p=128)[:, :, g * 512:(g + 1) * 512], (KC, 512)))
            wo = []
            for half in range(2):
                wo.append(WTile(wout_d.rearrange("(k p) c -> p k c", p=128)[:, :, half * 512:(half + 1) * 512], (KC, 512)))
            return wi, wo

        plan = []
        for t in range(ntiles):
            plan.append((ffn_tiles(0), mix_tiles(), ffn_tiles(1)))

        def push_ffn(ft):
            gu, dn = ft
            for g, u in gu:
                wstream.extend([g, u])
            for parts in dn:
                wstream.extend(parts)

        def push_mix(mt):
            wi, wo = mt
            wstream.extend([wi[2], wi[1], wi[3], wi[5], wi[4], wi[0]])
            wstream.extend(wo)

        for pair in range(ntiles // 2):
            a, b = plan[2 * pair], plan[2 * pair + 1]
            push_ffn(a[0]); push_ffn(b[0])
            push_mix(a[1]); push_mix(b[1])
            push_ffn(a[2]); push_ffn(b[2])

        P.op("gpsimd", OPC("memset", IDF[:], 1.0), writes=[cB])
        P.op("gpsimd", OPC("affine_select", out=IDF[:], in_=IDF[:], pattern=[[-1, 128]],
                                                 compare_op=ALU.is_equal, fill=0.0, base=0,
                                                 channel_multiplier=1), reads=[cB], writes=[cB])
        P.op("gpsimd", OPC("memset", MASK[:], 1.0), writes=[cB])
        P.op("gpsimd", OPC("affine_select", out=MASK[:], in_=MASK[:], pattern=[[1, 128]],
                                                 compare_op=ALU.is_ge, fill=0.0, base=0,
                                                 channel_multiplier=-1), reads=[cB], writes=[cB])
        P.op("gpsimd", OPC("memset", MASK[0:64, 64:128], 0.0), reads=[cB], writes=[cB])
        P.op("gpsimd", OPC("memset", ONEB[:], 1.0), writes=[cB])
        P.op("gpsimd", OPC("memset", ONEF[:], 1.0), writes=[cB])
        P.op("vector", OPC("tensor_copy", out=IDB[:], in_=IDF[:]), reads=[cB], writes=[cB])
        rows = [(lb_d, 0, 2), (hg_d, 2, 1), (cb_d, 3, 1), (clg_d, 4, 1), (clb_d, 5, 1), (cw_d, 6, CW)]
        for (src, r0, n) in rows:
            P.op("sync", OPC("dma_start", out=PR[r0:r0 + n, :], in_=src),
                 writes=[cB], dma="c")
        pt_ps, pt_b = ps_alloc()
        for c in range(NH):
            MM(pt_b, pt_ps[:, c * 37:(c + 1) * 37], PR[0:37, c * 128:(c + 1) * 128], IDF[0:37, 0:37], [cB])
        P.op("vector", OPC("tensor_copy", out=PT[:].rearrange("p h r -> p (h r)"), in_=pt_ps[:, 0:NH * 37]),
             reads=[pt_b], writes=[cB])
        P.op("vector", OPC("tensor_tensor", out=PC[:, :, 3], in0=PT[:, :, 0], in1=PT[:, :, 1], op=ALU.subtract),
             reads=[cB], writes=[cB])
        P.op("scalar", OPC("activation", out=PC[:, :, 4], in_=PC[:, :, 3], func=AF.Tanh, scale=0.5),
             reads=[cB], writes=[cB])
        P.op("vector", OPC("tensor_scalar", out=PC[:, :, 0], in0=PC[:, :, 4], scalar1=0.25, scalar2=0.75,
                                                 op0=ALU.mult, op1=ALU.add), reads=[cB], writes=[cB])
        P.op("vector", OPC("tensor_scalar", out=PC[:, :, 1], in0=PC[:, :, 4], scalar1=-0.25, scalar2=0.25,
                                                 op0=ALU.mult, op1=ALU.add), reads=[cB], writes=[cB])
        P.op("vector", OPC("tensor_scalar", out=PC[:, :, 2], in0=PT[:, :, 2], scalar1=0.5, scalar2=None,
                                                 op0=ALU.mult), reads=[cB], writes=[cB])
        P.op("vector", OPC("tensor_scalar", out=W05[:], in0=PT[:, :, 6:6 + CW], scalar1=0.5, scalar2=None,
                                                 op0=ALU.mult), reads=[cB], writes=[cB])

        def newton_rsqrt(eng, y, v, tmp, bufs, n_iter=2):
            P.op(eng, OPC("tensor_scalar", out=y.bitcast(I32), in0=v.bitcast(I32), scalar1=1, scalar2=None,
                                                op0=ALU.arith_shift_right), reads=bufs, writes=bufs)
            P.op(eng, OPC("tensor_scalar", out=y.bitcast(I32), in0=y.bitcast(I32), scalar1=-1, scalar2=MAGIC,
                                                op0=ALU.mult, op1=ALU.add), reads=bufs, writes=bufs)
            for _ in range(n_iter):
                P.op(eng, OPC("tensor_tensor", out=tmp, in0=y, in1=y, op=ALU.mult), reads=bufs, writes=bufs)
                P.op(eng, OPC("tensor_tensor", out=tmp, in0=tmp, in1=v, op=ALU.mult), reads=bufs, writes=bufs)
                P.op(eng, OPC("tensor_scalar", out=tmp, in0=tmp, scalar1=-0.5, scalar2=1.5,
                                                    op0=ALU.mult, op1=ALU.add), reads=bufs, writes=bufs)
                P.op(eng, OPC("tensor_tensor", out=y, in0=y, in1=tmp, op=ALU.mult), reads=bufs, writes=bufs)

        def load_x(tile, xi):
            t0 = tile * NT
            for b in range(NB):
                P.op("sync", OPC("dma_start", out=X[xi][:, b, :], in_=x_d[t0 + b * 128:t0 + (b + 1) * 128, :]),
                     writes=[Xb[xi][b]], dma="x")

        def load_gb(li, slot):
            P.op("sync", OPC("dma_start", out=GB[:, slot, 0, :], in_=lng_d[li].partition_broadcast(128)),
                 writes=[GBb[slot]], dma="g")
            P.op("sync", OPC("dma_start", out=GB[:, slot, 1, :], in_=lnb_d[li].partition_broadcast(128)),
                 writes=[GBb[slot]], dma="g")

        def transposes(xi):
            for b in range(NB):
                xb_ap, xb_b = scr_alloc()
                xb16 = as_bf(xb_ap, D)
                P.op("scalar", OPC("copy", out=xb16, in_=X[xi][:, b, :]),
                     reads=[Xb[xi][b]], writes=[xb_b])
                ps, pb = ps_alloc()
                psT = ps[:].bitcast(BF16)
                for k in range(KC):
                    P.op("tensor", OPC("transpose", psT[:, k * 128:(k + 1) * 128],
                                                                                 xb16[:, k * 128:(k + 1) * 128], IDB[:]),
                         reads=[xb_b, cB], writes=[pb])
                P.op("vector", OPC("tensor_copy", out=XT[:, :, b * 128:(b + 1) * 128], in_=psT.rearrange("p (k t) -> p k t", k=KC)),
                     reads=[pb], writes=[XTb[b]])

        def ln_finish(xi, b, slot, eps, last, tile):
            sm = SM[:, xi, b, :]
            bufs = [SMb[xi][b]]
            P.op("vector", OPC("bn_aggr", out=sm[:, 0:2], in_=STT[:, xi, b, :, :].rearrange("p g s -> p (g s)")),
                 reads=[STTb[xi][b]], writes=bufs)
            P.op("vector", OPC("tensor_scalar", out=sm[:, 2:3], in0=sm[:, 1:2], scalar1=eps, scalar2=None,
                                                     op0=ALU.add), reads=bufs, writes=bufs)
            newton_rsqrt("vector", sm[:, 3:4], sm[:, 2:3], sm[:, 4:5], bufs)
            P.op("vector", OPC("scalar_tensor_tensor", out=sm[:, 5:6], in0=sm[:, 0:1], scalar=-1.0, in1=sm[:, 3:4],
                                                            op0=ALU.mult, op1=ALU.mult), reads=bufs, writes=bufs)
            xblk = X[xi][:, b, :]
            P.op("scalar", OPC("activation", out=xblk, in_=xblk, func=AF.Identity, scale=sm[:, 3:4], bias=sm[:, 5:6]),
                 reads=[Xb[xi][b]] + bufs, writes=[Xb[xi][b]])
            P.op("gpsimd", OPC("tensor_tensor", out=xblk, in0=xblk, in1=GB[:, slot, 0, :], op=ALU.mult),
                 reads=[Xb[xi][b], GBb[slot]], writes=[Xb[xi][b]])
            P.op("gpsimd", OPC("tensor_tensor", out=xblk, in0=xblk, in1=GB[:, slot, 1, :], op=ALU.add),
                 reads=[Xb[xi][b], GBb[slot]], writes=[Xb[xi][b]])
            if last:
                t0 = tile * NT
                fin.append(P.op("sync", OPC("dma_start", out=out_d[t0 + b * 128:t0 + (b + 1) * 128, :], in_=xblk),
                                reads=[Xb[xi][b]], dma="o"))

        def resid_ln(xi, pdict, res_scale, slot, eps, last, tile, wrel):
            for half in range(2):
                for b in range(NB):
                    ps, pb = pdict(half, b)
                    if b == NB - 1:
                        wrel(half)
                    xs = X[xi][:, b, half * 512:(half + 1) * 512]
                    P.op("vector", OPC("scalar_tensor_tensor", out=xs, in0=xs, scalar=res_scale, in1=ps[:], op0=ALU.mult, op1=ALU.add),
                         reads=[Xb[xi][b], pb], writes=[Xb[xi][b]])
                    P.op("vector", OPC("bn_stats", out=STT[:, xi, b, half, :], in_=xs), reads=[Xb[xi][b]], writes=[STTb[xi][b]])
                    if half == 1:
                        ln_finish(xi, b, slot, eps, last, tile)

        def ffn(xi, tiles, slot, last, tile):
            gu, dn = tiles
            for f in range(FC):
                HTb[f].aliases = [m for m in mix_alias]
            first = [True]
            for fg in range(FC // 2):
                g, u = gu[fg]
                wneed(g)
                wneed(u)
                for fi in range(2):
                    f = fg * 2 + fi
                    pg, pgb = ps_alloc()
                    pu, pub = ps_alloc()
                    for (wt, ps, pb) in ((g, pg, pgb), (u, pu, pub)):
                        for k in range(KC):
                            MM(pb, ps[:], wt.ap[:, k, fi * 128:(fi + 1) * 128], XT[:, k, :], [wt.buf] + XTb)
                    sg, sgb = scr_alloc()
                    P.op("scalar", OPC("activation", out=sg, in_=pg[:], func=AF.Silu),
                         reads=[pgb], writes=[sgb])
                    P.op("vector", OPC("tensor_tensor", out=HT[:, f, :], in0=sg, in1=pu[:], op=ALU.mult),
                         reads=[sgb, pub], writes=[HTb[f]])
                    if first[0]:
                        for m in mix_alias:
                            m.writer = None
                            m.readers = []
                        first[0] = False
                    HTb[f].aliases = []
                wrelease(g)
                wrelease(u)
            yield

            def pd(half, b):
                for part in dn[half]:
                    wneed(part)
                ps, pb = ps_alloc()
                for f in range(FC):
                    part = dn[half][f // 11]
                    MM(pb, ps[:], HT[:, f, b * 128:(b + 1) * 128], part.ap[:, f % 11, :], [HTb[f], part.buf])
                return ps, pb

            def wrel(half):
                for part in dn[half]:
                    wrelease(part)

            resid_ln(xi, pd, 2.0 * ALPHA, slot, 4.0 * LN_EPS, last, tile, wrel)

        def mixer(xi, tiles, slot, seq_start, par):
            wi, wo = tiles
            for m in mix_alias:
                m.aliases = [h for h in HTb]
            wf, wq, wv, wgo, wcv, wcg = wi[1], wi[0], wi[2], wi[3], wi[4], wi[5]
            NCH = NT // 64

            def rslot(ci):
                return ci if ci > 0 else (0 if par == 0 else NCH)

            def wslot(ci):
                return ci + 1 if ci < NCH - 1 else (NCH if par == 0 else 0)

            def proj_fm(wt, c):
                ps, pb = ps_alloc()
                for k in range(KC):
                    MM(pb, ps[:], wt.ap[:, k, c * 128:(c + 1) * 128], XT[:, k, :], [wt.buf] + XTb)
                return ps, pb

            if seq_start:
                P.op("gpsimd", OPC("memset", S[:], 0.0), writes=[Sb_])
                P.op("gpsimd", OPC("memset", SBS[:, rslot(0), :, :], 0.0), writes=[SBSb[rslot(0)]])
                for c in range(NH):
                    P.op("gpsimd", OPC("memset", UT[:, c, 0:HALO], 0.0), writes=[UTb[c]])

            for b in range(NB):
                Vb[b].aliases = list(CBb)
                KTOKb[b].aliases = list(CQb)
            wneed(wv)
            for b in range(NB):
                ps, pb = ps_alloc()
                for k in range(KC):
                    MM(pb, ps[:], XT[:, k, b * 128:(b + 1) * 128], wv.ap[:, k, :], [wv.buf, XTb[b]])
                P.op("scalar", OPC("copy", out=V[:, b, :], in_=ps[:]), reads=[pb], writes=[Vb[b]])
            wrelease(wv)
            wneed(wf)
            KH = []
            for h in range(NH):
                ps, pb = proj_fm(wf, h)
                fa, fb = scr_alloc()
                P.op("scalar", OPC("activation", out=fa, in_=ps[:], func=AF.Tanh, scale=0.5), reads=[pb], writes=[fb])
                P.op("vector", OPC("tensor_scalar", out=fa, in0=fa, scalar1=PC[:, h, 1:2], scalar2=PC[:, h, 0:1],
                                   op0=ALU.mult, op1=ALU.add), reads=[fb, cB], writes=[fb])
                for c in range(NCH):
                    cs = slice(c * 64, (c + 1) * 64)
                    P.op("vector", OPC("tensor_tensor_scan", out=BC[:, h, cs], data0=fa[:, cs], data1=ONEF[:, 0:64], initial=1.0,
                                       op0=ALU.mult, op1=ALU.mult), reads=[fb, cB], writes=[BCb[h]])
                rb, rbb = scr_alloc()
                P.op("vector", OPC("reciprocal", out=rb, in_=BC[:, h, :]), reads=[BCb[h]], writes=[rbb])
                P.op("vector", OPC("tensor_scalar", out=fa, in0=fa, scalar1=-1.0, scalar2=1.0, op0=ALU.mult, op1=ALU.add),
                     reads=[fb], writes=[fb])
                P.op("vector", OPC("tensor_tensor", out=KT[:, h, :], in0=fa, in1=rb, op=ALU.mult), reads=[fb, rbb], writes=[KTb[h]])
                P.op("gpsimd", OPC("tensor_tensor", out=rb.rearrange("p (c t) -> p c t", t=64), in0=rb.rearrange("p (c t) -> p c t", t=64),
                                   in1=BC[:, h, :].rearrange("p (c t) -> p c t", t=64)[:, :, 63:64].to_broadcast([128, NCH, 64]),
                                   op=ALU.mult), reads=[rbb, BCb[h]], writes=[rbb])
                kh, khb = scr_alloc()
                kh16 = as_bf(kh, NT)
                P.op("gpsimd", OPC("tensor_tensor", out=kh16, in0=fa, in1=rb, op=ALU.mult), reads=[fb, rbb], writes=[khb])
                KH.append((kh16, khb))
            wrelease(wf)
            wneed(wgo)
            for h in range(NH):
                ps, pb = proj_fm(wgo, h)
                P.op("scalar", OPC("activation", out=THG[:, h, :], in_=ps[:], func=AF.Tanh, scale=0.5), reads=[pb], writes=[THGb[h]])
            wrelease(wgo)
            THC = []
            wneed(wcg)
            for c in range(NH):
                ps, pb = proj_fm(wcg, c)
                ta, tb = scr_alloc()
                P.op("scalar", OPC("activation", out=ta, in_=ps[:], func=AF.Tanh, scale=0.5), reads=[pb], writes=[tb])
                THC.append((ta, tb))
            wrelease(wcg)
            wneed(wcv)
            for c in range(NH):
                ps, pb = proj_fm(wcv, c)
                ta, tb = THC[c]
                P.op("vector", OPC("scalar_tensor_tensor", out=UT[:, c, HALO:HALO + NT], in0=ta, scalar=1.0, in1=ps[:],
                                   op0=ALU.add, op1=ALU.mult), reads=[tb, pb], writes=[UTb[c]])
            wrelease(wcv)
            wneed(wq)
            for h in range(NH):
                ps, pb = proj_fm(wq, h)
                P.op("vector", OPC("tensor_tensor", out=QT[:, h, :], in0=ps[:], in1=BC[:, h, :], op=ALU.mult),
                     reads=[pb, BCb[h]], writes=[QTb[h]])
            wrelease(wq)
            for b in range(NB):
                ps, pb = ps_alloc()
                psT = ps[:].bitcast(BF16)
                for h in range(NH):
                    kh16, khb = KH[h]
                    P.op("tensor", OPC("transpose", psT[:, h * 128:(h + 1) * 128], kh16[:, b * 128:(b + 1) * 128], IDB[:]),
                         reads=[khb, cB], writes=[pb])
                P.op("scalar", OPC("copy", out=KTOK[:, b, :], in_=psT[:, 0:512]), reads=[pb], writes=[KTOKb[b]])
            yield

            def conv_mm(c):
                cps, cpb = ps_alloc()
                dg16 = dgb = None
                for j in range(CW):
                    if j % 8 == 0:
                        dg, dgb = scr_alloc()
                        dg16 = as_bf(dg, 1024)
                        n = min(8, CW - j)
                        P.op("gpsimd", OPC("tensor_tensor", out=dg16[:, 0:n * 128].rearrange("p (j t) -> p j t", j=n),
                                           in0=IDF[:].rearrange("p (o t) -> p o t", o=1).to_broadcast([128, n, 128]),
                                           in1=W05[:, c, j:j + n].rearrange("p (j o) -> p j o", o=1).to_broadcast([128, n, 128]),
                                           op=ALU.mult), reads=[cB], writes=[dgb])
                    jj = j % 8
                    MM(cpb, cps[:], dg16[:, jj * 128:(jj + 1) * 128], UT[:, c, j:j + NT], [dgb, UTb[c]])
                P.op("scalar", OPC("activation", out=CS[:, c, :], in_=cps[:], func=AF.Identity, bias=PT[:, c, 3:4]),
                     reads=[cpb, cB], writes=[CSb[c]])

            def conv_fin(c):
                P.op("scalar", OPC("copy", out=CB[:, c, :], in_=CS[:, c, :]), reads=[CSb[c]], writes=[CBb[c]])
                P.op("scalar", OPC("activation", out=CQ[:, c, :], in_=CS[:, c, :], func=AF.Square), reads=[CSb[c]], writes=[CQb[c]])
                P.op("gpsimd", OPC("tensor_copy", out=UT[:, c, 0:HALO], in_=UT[:, c, NT:NT + HALO]), reads=[UTb[c]], writes=[UTb[c]])

            def stage_a(b):
                bs = slice(b * 128, (b + 1) * 128)
                sc, scb = ps_alloc()
                for h in range(NH):
                    MM(scb, sc[:, h * 128:(h + 1) * 128], KT[:, h, bs], QT[:, h, bs], [KTb[h], QTb[h]])
                P.op("vector", OPC("tensor_tensor", out=AT[b][:], in0=sc[:].rearrange("p (h t) -> p h t", h=NH),
                                   in1=MASK[:].rearrange("p (o t) -> p o t", o=1).to_broadcast([128, NH, 128]), op=ALU.mult),
                     reads=[scb, cB], writes=[ATb[b]])
                for c2 in range(2):
                    p_, pb_ = ps_alloc()
                    rs = slice(c2 * 64, (c2 + 1) * 64)
                    for h in range(NH):
                        MM(pb_, p_[:, h * 128:(h + 1) * 128], KTOK[rs, b, h * 128:(h + 1) * 128], V[rs, b, h * 128:(h + 1) * 128],
                           [KTOKb[b], Vb[b]])
                    ci = b * 2 + c2
                    for h in range(NH):
                        P.op("vector", OPC("scalar_tensor_tensor", out=S[:, h, :], in0=S[:, h, :], scalar=BC[:, h, ci * 64 + 63:ci * 64 + 64],
                                           in1=p_[:, h * 128:(h + 1) * 128], op0=ALU.mult, op1=ALU.add),
                             reads=[Sb_, BCb[h], pb_], writes=[Sb_])
                    nxt = wslot(ci)
                    P.op("scalar", OPC("copy", out=SBS[:, nxt, :, :], in_=S[:]), reads=[Sb_], writes=[SBSb[nxt]])

            for b in range(NB):
                stage_a(b)
            conv_mm(0)
            conv_mm(1)

            OQ = {}

            def rms_stats(b):
                oq16, oqb = OQ[b]
                ss, ssb = ps_alloc()
                for h in range(NH):
                    MM(ssb, ss[:, h:h + 1], oq16[:, h * 128:(h + 1) * 128], ONEB[:, 0:1], [oqb, cB])
                sm = SM2[:, b, :]
                smb = [SM2b[b]]
                P.op("vector", OPC("tensor_scalar", out=sm[:, 0:4], in0=ss[:, 0:4], scalar1=1.0 / 128.0, scalar2=RMS_EPS,
                                   op0=ALU.mult, op1=ALU.add), reads=[ssb], writes=smb)
                newton_rsqrt("vector", sm[:, 4:8], sm[:, 0:4], sm[:, 8:12], smb)

            for b in range(NB):
                o_ps, o_b = ps_alloc()
                for h in range(NH):
                    MM(o_b, o_ps[:, h * 128:(h + 1) * 128], V[:, b, h * 128:(h + 1) * 128], AT[b][:, h, :], [Vb[b], ATb[b]])
                for c2 in range(2):
                    ci = b * 2 + c2
                    ts_ = slice(b * 128 + c2 * 64, b * 128 + (c2 + 1) * 64)
                    for h in range(NH):
                        MM(o_b, o_ps[:, h * 128 + c2 * 64:h * 128 + (c2 + 1) * 64], SBS[:, rslot(ci), h, :], QT[:, h, ts_],
                           [SBSb[rslot(ci)], QTb[h]])
                P.op("scalar", OPC("copy", out=OS[:, b, :], in_=o_ps[:]), reads=[o_b], writes=[OSb[b]])
                oq, oqb = scr_alloc()
                oq16 = as_bf(oq, 512)
                P.op("scalar", OPC("activation", out=oq16, in_=o_ps[:], func=AF.Square), reads=[o_b], writes=[oqb])
                OQ[b] = (oq16, oqb)
                if b > 0:
                    rms_stats(b - 1)
            rms_stats(NB - 1)

            def rms_apply(b):
                bs = slice(b * 128, (b + 1) * 128)
                sm = SM2[:, b, :]
                lt, ltb = scr_alloc()
                ltv = lt.rearrange("p (h t) -> p h t", h=NH)
                P.op("vector", OPC("tensor_copy", out=ltv, in_=sm[:, 4:8].rearrange("p (h o) -> p h o", o=1).to_broadcast([128, NH, 128])),
                     reads=[SM2b[b]], writes=[ltb])
                rb_ps, rb_b = ps_alloc()
                for h in range(NH):
                    MM(rb_b, rb_ps[:, h * 128:(h + 1) * 128], ltv[:, h, :], IDF[:], [ltb, cB])
                os_ = OS[:, b, :]
                for h in range(NH):
                    P.op("vector", OPC("scalar_tensor_tensor", out=os_[:, h * 128:(h + 1) * 128], in0=os_[:, h * 128:(h + 1) * 128],
                                       scalar=PC[:, h, 2:3], in1=rb_ps[:, h * 128:(h + 1) * 128], op0=ALU.mult, op1=ALU.mult),
                         reads=[OSb[b], rb_b, cB], writes=[OSb[b]])
                P.op("vector", OPC("scalar_tensor_tensor", out=CT[:, 0:NH, bs], in0=THG[:, :, bs], scalar=1.0,
                                   in1=os_.rearrange("p (h t) -> p h t", h=NH), op0=ALU.add, op1=ALU.mult),
                     reads=THGb + [OSb[b]], writes=[CTb[c][b] for c in range(NH)])

            for c in range(NH):
                CBb[c].aliases = list(Vb)
                CQb[c].aliases = list(KTOKb)
            conv_fin(0)
            conv_fin(1)
            for c in (2, 3):
                conv_mm(c)
                conv_fin(c)
                rms_apply(2 * (c - 2))
                rms_apply(2 * (c - 2) + 1)

            stp, stb = ps_alloc()
            for b in range(NB):
                bs = slice(b * 128, (b + 1) * 128)
                for (q, src, srcb) in ((0, CB, CBb), (1, CQ, CQb)):
                    for c in range(NH):
                        MM(stb, stp[:, b * 2 + q:b * 2 + q + 1], src[:, c, bs], ONEB[:, 0:1], [srcb[c], cB])
            cm, cmb = scr_alloc()
            P.op("vector", OPC("tensor_scalar", out=cm[:, 0:2 * NB], in0=stp[:, 0:2 * NB], scalar1=1.0 / 512.0, scalar2=None, op0=ALU.mult),
                 reads=[stb], writes=[cmb])
            cmv = cm[:, 0:2 * NB].rearrange("p (b q) -> p b q", q=2)
            mean = cmv[:, :, 0]
            ex2 = cmv[:, :, 1]
            P.op("vector", OPC("tensor_tensor", out=cm[:, 16:16 + NB], in0=mean, in1=mean, op=ALU.mult), reads=[cmb], writes=[cmb])
            P.op("vector", OPC("tensor_tensor", out=cm[:, 8:8 + NB], in0=ex2, in1=cm[:, 16:16 + NB], op=ALU.subtract), reads=[cmb], writes=[cmb])
            P.op("vector", OPC("tensor_scalar", out=cm[:, 8:8 + NB], in0=cm[:, 8:8 + NB], scalar1=LN_EPS, scalar2=None, op0=ALU.add),
                 reads=[cmb], writes=[cmb])
            newton_rsqrt("vector", cm[:, 12:12 + NB], cm[:, 8:8 + NB], cm[:, 16:16 + NB], [cmb])
            P.op("vector", OPC("scalar_tensor_tensor", out=cm[:, 20:20 + NB], in0=mean, scalar=-1.0, in1=cm[:, 12:12 + NB],
                               op0=ALU.mult, op1=ALU.mult), reads=[cmb], writes=[cmb])
            ra_ps, ra_b = ps_alloc()
            rn_ps, rn_b = ps_alloc()
            for (col0, dst, dstb) in ((12, ra_ps, ra_b), (20, rn_ps, rn_b)):
                lt, ltb = scr_alloc()
                ltv = lt.rearrange("p (b t) -> p b t", b=NB)
                P.op("vector", OPC("tensor_copy", out=ltv, in_=cm[:, col0:col0 + NB].rearrange("p (b o) -> p b o", o=1).to_broadcast([128, NB, 128])),
                     reads=[cmb], writes=[ltb])
                for b in range(NB):
                    MM(dstb, dst[:, b * 128:(b + 1) * 128], ltv[:, b, :], IDF[:], [ltb, cB])
            for c in range(NH):
                P.op("vector", OPC("tensor_tensor", out=CS[:, c, :], in0=CS[:, c, :], in1=ra_ps[:], op=ALU.mult),
                     reads=[CSb[c], ra_b], writes=[CSb[c]])
                P.op("vector", OPC("tensor_tensor", out=CS[:, c, :], in0=CS[:, c, :], in1=rn_ps[:], op=ALU.add),
                     reads=[CSb[c], rn_b], writes=[CSb[c]])
                P.op("scalar", OPC("activation", out=CT[:, NH + c, :], in_=CS[:, c, :], func=AF.Silu, scale=PT[:, c, 4:5], bias=PT[:, c, 5:6]),
                     reads=[CSb[c], cB], writes=[CTb[NH + c][b] for b in range(NB)])

            def pd(half, b):
                wneed(wo[half])
                ps, pb = ps_alloc()
                for k in range(KC):
                    MM(pb, ps[:], CT[:, k, b * 128:(b + 1) * 128], wo[half].ap[:, k, :], [CTb[k][b], wo[half].buf])
                return ps, pb

            def wrel(half):
                wrelease(wo[half])

            resid_ln(xi, pd, ALPHA, slot, LN_EPS, False, 0, wrel)

        assert ntiles % 2 == 0
        wfill()
        load_x(0, 0)
        load_x(1, 1)
        sa, sb_ = 0, 1
        load_gb(0, sa)
        transposes(0)
        for pair in range(ntiles // 2):
            tA, tB = 2 * pair, 2 * pair + 1
            fA, mA, gA = plan[tA]
            fB, mB, gB = plan[tB]
            load_gb(1, sb_)
            g = ffn(0, fA, sa, False, tA); next(g)
            transposes(1)
            next(g, None)
            g = ffn(1, fB, sa, False, tB); next(g)
            transposes(0)
            next(g, None)
            load_gb(2, sa)
            g = mixer(0, mA, sb_, (tA % tiles_per_seq) == 0, 0); next(g)
            transposes(1)
            next(g, None)
            g = mixer(1, mB, sb_, (tB % tiles_per_seq) == 0, 1); next(g)
            transposes(0)
            next(g, None)
            load_gb(0, sb_)
            g = ffn(0, gA, sa, True, tA); next(g)
            transposes(1)
            next(g, None)
            if tA + 2 < ntiles:
                load_x(tA + 2, 0)
            g = ffn(1, gB, sa, True, tB); next(g)
            if tA + 2 < ntiles:
                transposes(0)
            next(g, None)
            if tB + 2 < ntiles:
                load_x(tB + 2, 1)
            sa, sb_ = sb_, sa
        print('sbuf bytes remaining/partition:', nc.sbuf_bytes_remaining)
        P.emit(final_waits=fin)
    return nc


_NC_CACHE = {}


def _get_nc(nseq, T):
    key = (nseq, T)
    if key not in _NC_CACHE:
        _NC_CACHE[key] = build_nc(nseq, T)
    return _NC_CACHE[key]


def kernel(**inputs):
    x = np.asarray(inputs["x"], dtype=np.float32)
    B, T, _ = x.shape
    n = 8
    nseq = B // n
    nc = _get_nc(nseq, T)
    shared = {}
    for k in ("ffn1_w_gate", "ffn1_w_up", "ffn1_w_down", "ffn2_w_gate", "ffn2_w_up", "ffn2_w_down", "w_in", "w_out"):
        a = np.asarray(inputs[k], dtype=np.float32)
        shared[k] = np.ascontiguousarray(a.reshape(a.shape[-2], a.shape[-1]))
    for k in ("ln1_g", "ln1_b", "ln2_g", "ln2_b", "ln3_g", "ln3_b", "hgrn_norm_g", "conv_b", "conv_ln_g", "conv_ln_b"):
        a = np.asarray(inputs[k], dtype=np.float32)
        shared[k] = np.ascontiguousarray(a.reshape(1, a.shape[-1]))
    shared["lb_logits"] = np.ascontiguousarray(np.asarray(inputs["lb_logits"], dtype=np.float32).reshape(2, 512))
    shared["conv_w"] = np.ascontiguousarray(np.asarray(inputs["conv_w"], dtype=np.float32).reshape(CW, 512))
    in_maps = []
    for c in range(n):
        m = dict(shared)
        m["x"] = np.ascontiguousarray(x[c * nseq:(c + 1) * nseq].reshape(nseq * T, D))
        in_maps.append(m)
    res = run_bass_kernel_spmd(nc, in_maps, core_ids=list(range(n)))
    out = np.concatenate([np.asarray(r["out"]).reshape(nseq, T, D) for r in res.results], axis=0)
    return out.astype(np.float32, copy=False)
```

```python
import numpy as np
from contextlib import ExitStack
import concourse.bass as bass
import concourse.mybir as mybir
from concourse.bass_utils import run_bass_kernel_spmd

F32 = mybir.dt.float32
BF16 = mybir.dt.bfloat16
I32 = mybir.dt.int32
AF = mybir.ActivationFunctionType
ALU = mybir.AluOpType

D = 1024
KC = 8
DFF = 2816
FC = 22
DIN = 3072
NH = 4
CW = 31
HALO = CW - 1
ALPHA = 2.0 ** 0.25
LN_EPS = 1e-5
RMS_EPS = 1e-6
NT = 512
NB = NT // 128
RING_ELEMS = 22 * 1024
NSCR = 14
MAGIC = 0x5F3759DF

ENGINES = ("tensor", "vector", "scalar", "gpsimd", "sync")


class Buf:
    __slots__ = ("name", "writer", "readers", "aliases", "dead")

    def __init__(self, name, aliases=()):
        self.name = name
        self.writer = None
        self.readers = []
        self.aliases = list(aliases)
        self.dead = False


class Op:
    __slots__ = ("eng", "fn", "dma", "deps", "signal", "sem", "val", "waits", "pre")

    def __init__(self, eng, fn, dma):
        self.eng = eng
        self.fn = fn
        self.dma = dma
        self.deps = []
        self.signal = False
        self.sem = None
        self.val = 0
        self.waits = []


def OPC(name, *args, **kwargs):
    return lambda e: getattr(e, name)(*args, **kwargs)


class Prog:
    def __init__(self, nc):
        self.nc = nc
        self.ops = []

    def op(self, eng, fn, reads=(), writes=(), dma=None):
        o = Op(eng, fn, dma)
        deps = {}
        for b in reads:
            assert not b.dead, "read of dead buffer " + b.name
            for bb in [b] + b.aliases:
                if bb.writer is not None:
                    deps[id(bb.writer)] = bb.writer
        for b in writes:
            assert not b.dead, "write of dead buffer " + b.name
            for bb in [b] + b.aliases:
                if bb.writer is not None:
                    deps[id(bb.writer)] = bb.writer
                last = {}
                for r in bb.readers:
                    if r.dma is not None:
                        deps[id(r)] = r
                    else:
                        last[r.eng] = r
                for r in last.values():
                    deps[id(r)] = r
        for d in deps.values():
            if d is o:
                continue
            if d.eng == "tensor" and eng == "tensor" and d.dma is None and dma is None:
                continue
            o.deps.append(d)
            d.signal = True
        for b in reads:
            b.readers.append(o)
        for b in writes:
            b.readers = []
            for bb in b.aliases:
                bb.readers = []
                bb.writer = None
            b.aliases = []
            b.writer = o
        self.ops.append(o)
        return o

    def emit(self, final_waits=()):
        nc = self.nc
        with ExitStack() as es:
            sems = {}

            def get_sem(key):
                if key not in sems:
                    sems[key] = es.enter_context(nc.semaphore("s_" + key))
                return sems[key]

            counts = {}
            KDMA = 8
            for o in final_waits:
                o.signal = True
            for o in self.ops:
                o.pre = None
                if o.dma:
                    n = counts.get("d_" + o.dma, 0)
                    counts["d_" + o.dma] = n + 1
                    o.signal = True
                    o.sem = get_sem("d_%s_%d" % (o.dma, n % KDMA))
                    o.val = 16 * (n // KDMA + 1)
                    if n >= KDMA:
                        o.pre = (o.sem, 16 * (n // KDMA))
                    continue
                if not o.signal:
                    continue
                key = "e_" + o.eng
                counts[key] = counts.get(key, 0) + 1
                o.sem = get_sem(key)
                o.val = counts[key]
            self.counts = counts
            known = {e: {} for e in ENGINES}
            per_eng = {e: [] for e in ENGINES}
            for o in self.ops:
                need = {}
                if o.pre is not None:
                    need[id(o.pre[0])] = o.pre
                for d in o.deps:
                    k = id(d.sem)
                    if k not in need or need[k][1] < d.val:
                        need[k] = (d.sem, d.val)
                kn = known[o.eng]
                for k, (s, v) in need.items():
                    if kn.get(k, 0) >= v:
                        continue
                    kn[k] = v
                    o.waits.append((s, v))
                per_eng[o.eng].append(o)
            with nc.Block() as block:
                def mk(engname):
                    def body(eng):
                        for o in per_eng[engname]:
                            for (s, v) in o.waits:
                                eng.wait_ge(s, v)
                            ins = o.fn(eng)
                            if o.signal:
                                ins.then_inc(o.sem, 16 if o.dma else 1)
                        if engname == "sync":
                            for o in final_waits:
                                eng.wait_ge(o.sem, o.val)
                    return body
                for e in ENGINES:
                    if per_eng[e] or e == "sync":
                        getattr(block, e)(mk(e))


def build_nc(nseq, T):
    ntok = nseq * T
    tiles_per_seq = T // NT
    ntiles = nseq * tiles_per_seq
    nc = bass.Bass("TRN2", target_bir_lowering=False)

    def din(name, shape):
        return nc.dram_tensor(name, list(shape), F32, kind="ExternalInput").ap()

    x_d = din("x", [ntok, D])
    wg_d = [din("ffn1_w_gate", [D, DFF]), din("ffn2_w_gate", [D, DFF])]
    wu_d = [din("ffn1_w_up", [D, DFF]), din("ffn2_w_up", [D, DFF])]
    wd_d = [din("ffn1_w_down", [DFF, D]), din("ffn2_w_down", [DFF, D])]
    win_d = din("w_in", [D, DIN])
    wout_d = din("w_out", [D, D])
    lng_d = [din("ln1_g", [1, D]), din("ln2_g", [1, D]), din("ln3_g", [1, D])]
    lnb_d = [din("ln1_b", [1, D]), din("ln2_b", [1, D]), din("ln3_b", [1, D])]
    lb_d = din("lb_logits", [2, 512])
    hg_d = din("hgrn_norm_g", [1, 512])
    cw_d = din("conv_w", [CW, 512])
    cb_d = din("conv_b", [1, 512])
    clg_d = din("conv_ln_g", [1, 512])
    clb_d = din("conv_ln_b", [1, 512])
    out_d = nc.dram_tensor("out", [ntok, D], F32, kind="ExternalOutput").ap()

    P = Prog(nc)
    fin = []
    with ExitStack() as es:
        def sb(name, shape, dt):
            return es.enter_context(nc.sbuf_tensor(name, list(shape), dt))

        X = [sb("X%d" % i, [128, NB, D], F32) for i in range(2)]
        Xb = [[Buf("X%d_%d" % (i, b)) for b in range(NB)] for i in range(2)]
        XT = sb("XT", [128, KC, NT], BF16)
        XTb = [Buf("XT%d" % b) for b in range(NB)]
        HT = sb("HT", [128, FC, NT], BF16)
        HTb = [Buf("HT%d" % f) for f in range(FC)]
        HTf = HT[:].rearrange("p f t -> p (f t)")
        BC = HTf[:, 0:4096].bitcast(F32).rearrange("p (h t) -> p h t", h=NH)
        CS = HTf[:, 4096:8192].bitcast(F32).rearrange("p (h t) -> p h t", h=NH)
        THG = HTf[:, 8192:10240].rearrange("p (h t) -> p h t", h=NH)
        BCb = [Buf("BC%d" % h, aliases=HTb) for h in range(NH)]
        CSb = [Buf("CS%d" % h, aliases=HTb) for h in range(NH)]
        THGb = [Buf("THG%d" % h, aliases=HTb) for h in range(NH)]
        mix_alias = BCb + CSb + THGb

        KT = sb("KT", [128, NH, NT], BF16)
        KTb = [Buf("KT%d" % h) for h in range(NH)]
        QT = sb("QT", [128, NH, NT], BF16)
        QTb = [Buf("QT%d" % h) for h in range(NH)]
        V = sb("V", [128, NB, 512], BF16)
        Vb = [Buf("V%d" % b) for b in range(NB)]
        KTOK = sb("KTOK", [128, NB, 512], BF16)
        KTOKb = [Buf("KTOK%d" % b) for b in range(NB)]
        AT = [sb("AT%d" % i, [128, NH, 128], BF16) for i in range(NB)]
        ATb = [Buf("AT%d" % i) for i in range(NB)]
        S = sb("S", [128, NH, 128], F32)
        Sb_ = [Buf("S%d" % h) for h in range(NH)]
        SBS = sb("SBS", [128, NT // 64 + 1, NH, 128], BF16)
        SBSb = [[Buf("SBS%d_%d" % (i, h)) for h in range(NH)] for i in range(NT // 64 + 1)]
        UT = sb("UT", [128, NH, HALO + NT], BF16)
        UTb = [Buf("UT%d" % c) for c in range(NH)]
        CB = V
        CBb = [Buf("CB%d" % c) for c in range(NH)]
        CQ = KTOK
        CQb = [Buf("CQ%d" % c) for c in range(NH)]
        OS = sb("OS", [128, NB, 512], F32)
        OSb = [Buf("OS%d" % b) for b in range(NB)]
        CT = sb("CT", [128, KC, NT], BF16)
        CTb = [[Buf("CT%d_%d" % (c, b)) for b in range(NB)] for c in range(KC)]
        GB = sb("GB", [128, 2, 2, D], F32)
        GBb = [Buf("GB%d" % i) for i in range(2)]
        SCR = sb("SCR", [128, NSCR, 512], F32)
        SCRb = [Buf("SCR%d" % i) for i in range(NSCR)]
        RING = sb("RING", [128, RING_ELEMS], BF16)
        IDF = sb("IDF", [128, 128], F32)
        IDB = sb("IDB", [128, 128], BF16)
        ONEB = sb("ONEB", [128, 128], BF16)
        ONEF = sb("ONEF", [128, 64], F32)
        MASK = sb("MASK", [128, 128], F32)
        PR = sb("PR", [37, 512], F32)
        PT = sb("PT", [128, NH, 37], F32)
        PC = sb("PC", [128, NH, 8], F32)
        W05 = sb("W05", [128, NH, CW], F32)
        STT = sb("STT", [128, 2, NB, 2, 6], F32)
        SM = sb("SM", [128, 2, NB, 8], F32)
        SM2 = sb("SM2", [128, 4, 16], F32)
        cB = Buf("consts")
        STTb = [[Buf("STT%d_%d" % (i, b)) for b in range(NB)] for i in range(2)]
        SMb = [[Buf("SM%d_%d" % (i, b)) for b in range(NB)] for i in range(2)]
        SM2b = [Buf("SM2_%d" % i) for i in range(4)]

        psum = [es.enter_context(nc.psum_tensor("PS%d" % i, [128, 512], F32)) for i in range(8)]
        psb = [Buf("PS%d" % i) for i in range(8)]
        st = {"ps": 0, "scr": 0, "sm2": 0, "at": 0}

        ps_first = {}

        def ps_alloc():
            i = st["ps"] % 8
            st["ps"] += 1
            ps_first[id(psb[i])] = True
            return psum[i], psb[i]

        def MM(pb, out, lhsT, rhs, reads):
            first = ps_first[id(pb)]
            ps_first[id(pb)] = False
            P.op("tensor", OPC("matmul", out, lhsT=lhsT, rhs=rhs, start=first, stop=True, skip_group_check=True),
                 reads=reads, writes=[pb])

        class Scr:
            pass

        def scr_alloc():
            i = st["scr"] % NSCR
            st["scr"] += 1
            old = SCRb[i]
            nb = Buf("SCR%d_%d" % (i, st["scr"]), aliases=[old] + old.aliases)
            old.dead = True
            SCRb[i] = nb
            return SCR[:, i, :], nb

        def as_bf(ap, n):
            return ap.bitcast(BF16)[:, 0:n]

        ring = {"head": 0, "live": []}

        def ring_alloc(n):
            start = ring["head"]
            if start + n > RING_ELEMS:
                start = 0
            end = start + n
            aliases = []
            keep = []
            for (s0, e0, b0, rel) in ring["live"]:
                if s0 < end and start < e0:
                    if not rel[0]:
                        return None
                    aliases.append(b0)
                else:
                    keep.append((s0, e0, b0, rel))
            for b0 in aliases:
                b0.dead = True
            al = []
            for b0 in aliases:
                al.append(b0)
                al.extend(b0.aliases)
            nb = Buf("W@%d" % start, aliases=al)
            rel = [False]
            keep.append((start, end, nb, rel))
            ring["live"] = keep
            ring["head"] = end
            return start, nb, rel

        class WTile:
            def __init__(self, src_ap, shape):
                self.src = src_ap
                self.shape = shape
                self.ap = None
                self.buf = None
                self.rel = None

        wstream = []
        wcur = {"i": 0}

        def wfill():
            while wcur["i"] < len(wstream):
                wt = wstream[wcur["i"]]
                a, b = wt.shape
                r = ring_alloc(a * b)
                if r is None:
                    return
                start, nb, rel = r
                wt.ap = RING[:, start:start + a * b].rearrange("p (a b) -> p a b", a=a)
                wt.buf = nb
                wt.rel = rel
                P.op("gpsimd", OPC("dma_start", out=wt.ap, in_=wt.src),
                     writes=[nb], dma="w")
                wcur["i"] += 1

        def wneed(wt):
            if wt.buf is None:
                wfill()
            assert wt.buf is not None, "ring too small / stream order"
            return wt

        def wrelease(wt):
            wt.rel[0] = True
            wfill()

        def ffn_tiles(l):
            gu = []
            for fg in range(FC // 2):
                cs = slice(fg * 256, (fg + 1) * 256)
                g = WTile(wg_d[l].rearrange("(k p) f -> p k f", p=128)[:, :, cs], (KC, 256))
                u = WTile(wu_d[l].rearrange("(k p) f -> p k f", p=128)[:, :, cs], (KC, 256))
                gu.append((g, u))
            dn = []
            for half in range(2):
                parts = []
                for q in range(2):
                    src = wd_d[l].rearrange("(f p) d -> p f d", p=128)[:, q * 11:(q + 1) * 11, half * 512:(half + 1) * 512]
                    parts.append(WTile(src, (11, 512)))
                dn.append(parts)
            return gu, dn

        def mix_tiles():
            wi = []
            for g in range(6):
                wi.append(WTile(win_d.rearrange("(k p) c -> p k c", p=128)[:, :, g * 512:(g + 1) * 512], (KC, 512)))
            wo = []
            for half in range(2):
                wo.append(WTile(wout_d.rearrange("(k p) c -> p k c", p=128)[:, :, half * 512:(half + 1) * 512], (KC, 512)))
            return wi, wo

        plan = []
        for t in range(ntiles):
            plan.append((ffn_tiles(0), mix_tiles(), ffn_tiles(1)))

        def push_ffn(ft):
            gu, dn = ft
            for g, u in gu:
                wstream.extend([g, u])
            for parts in dn:
                wstream.extend(parts)

        def push_mix(mt):
            wi, wo = mt
            wstream.extend([wi[2], wi[1], wi[3], wi[5], wi[4], wi[0]])
            wstream.extend(wo)

        for pair in range(ntiles // 2):
            a, b = plan[2 * pair], plan[2 * pair + 1]
            push_ffn(a[0]); push_ffn(b[0])
            push_mix(a[1]); push_mix(b[1])
            push_ffn(a[2]); push_ffn(b[2])

        P.op("gpsimd", OPC("memset", IDF[:], 1.0), writes=[cB])
        P.op("gpsimd", OPC("affine_select", out=IDF[:], in_=IDF[:], pattern=[[-1, 128]],
                                                 compare_op=ALU.is_equal, fill=0.0, base=0,
                                                 channel_multiplier=1), reads=[cB], writes=[cB])
        P.op("gpsimd", OPC("memset", MASK[:], 1.0), writes=[cB])
        P.op("gpsimd", OPC("affine_select", out=MASK[:], in_=MASK[:], pattern=[[1, 128]],
                                                 compare_op=ALU.is_ge, fill=0.0, base=0,
                                                 channel_multiplier=-1), reads=[cB], writes=[cB])
        P.op("gpsimd", OPC("memset", MASK[0:64, 64:128], 0.0), reads=[cB], writes=[cB])
        P.op("gpsimd", OPC("memset", ONEB[:], 1.0), writes=[cB])
        P.op("gpsimd", OPC("memset", ONEF[:], 1.0), writes=[cB])
        P.op("vector", OPC("tensor_copy", out=IDB[:], in_=IDF[:]), reads=[cB], writes=[cB])
        rows = [(lb_d, 0, 2), (hg_d, 2, 1), (cb_d, 3, 1), (clg_d, 4, 1), (clb_d, 5, 1), (cw_d, 6, CW)]
        for (src, r0, n) in rows:
            P.op("sync", OPC("dma_start", out=PR[r0:r0 + n, :], in_=src),
                 writes=[cB], dma="c")
        pt_ps, pt_b = ps_alloc()
        for c in range(NH):
            MM(pt_b, pt_ps[:, c * 37:(c + 1) * 37], PR[0:37, c * 128:(c + 1) * 128], IDF[0:37, 0:37], [cB])
        P.op("vector", OPC("tensor_copy", out=PT[:].rearrange("p h r -> p (h r)"), in_=pt_ps[:, 0:NH * 37]),
             reads=[pt_b], writes=[cB])
        P.op("vector", OPC("tensor_tensor", out=PC[:, :, 3], in0=PT[:, :, 0], in1=PT[:, :, 1], op=ALU.subtract),
             reads=[cB], writes=[cB])
        P.op("scalar", OPC("activation", out=PC[:, :, 4], in_=PC[:, :, 3], func=AF.Tanh, scale=0.5),
             reads=[cB], writes=[cB])
        P.op("vector", OPC("tensor_scalar", out=PC[:, :, 0], in0=PC[:, :, 4], scalar1=0.25, scalar2=0.75,
                                                 op0=ALU.mult, op1=ALU.add), reads=[cB], writes=[cB])
        P.op("vector", OPC("tensor_scalar", out=PC[:, :, 1], in0=PC[:, :, 4], scalar1=-0.25, scalar2=0.25,
                                                 op0=ALU.mult, op1=ALU.add), reads=[cB], writes=[cB])
        P.op("vector", OPC("tensor_scalar", out=PC[:, :, 2], in0=PT[:, :, 2], scalar1=0.5, scalar2=None,
                                                 op0=ALU.mult), reads=[cB], writes=[cB])
        P.op("vector", OPC("tensor_scalar", out=W05[:], in0=PT[:, :, 6:6 + CW], scalar1=0.5, scalar2=None,
                                                 op0=ALU.mult), reads=[cB], writes=[cB])

        def newton_rsqrt(eng, y, v, tmp, bufs, n_iter=2):
            P.op(eng, OPC("tensor_scalar", out=y.bitcast(I32), in0=v.bitcast(I32), scalar1=1, scalar2=None,
                                                op0=ALU.arith_shift_right), reads=bufs, writes=bufs)
            P.op(eng, OPC("tensor_scalar", out=y.bitcast(I32), in0=y.bitcast(I32), scalar1=-1, scalar2=MAGIC,
                                                op0=ALU.mult, op1=ALU.add), reads=bufs, writes=bufs)
            for _ in range(n_iter):
                P.op(eng, OPC("tensor_tensor", out=tmp, in0=y, in1=y, op=ALU.mult), reads=bufs, writes=bufs)
                P.op(eng, OPC("tensor_tensor", out=tmp, in0=tmp, in1=v, op=ALU.mult), reads=bufs, writes=bufs)
                P.op(eng, OPC("tensor_scalar", out=tmp, in0=tmp, scalar1=-0.5, scalar2=1.5,
                                                    op0=ALU.mult, op1=ALU.add), reads=bufs, writes=bufs)
                P.op(eng, OPC("tensor_tensor", out=y, in0=y, in1=tmp, op=ALU.mult), reads=bufs, writes=bufs)

        def load_x(tile, xi):
            t0 = tile * NT
            for b in range(NB):
                P.op("sync", OPC("dma_start", out=X[xi][:, b, :], in_=x_d[t0 + b * 128:t0 + (b + 1) * 128, :]),
                     writes=[Xb[xi][b]], dma="x")

        def load_gb(li, slot):
            P.op("sync", OPC("dma_start", out=GB[:, slot, 0, :], in_=lng_d[li].partition_broadcast(128)),
                 writes=[GBb[slot]], dma="g")
            P.op("sync", OPC("dma_start", out=GB[:, slot, 1, :], in_=lnb_d[li].partition_broadcast(128)),
                 writes=[GBb[slot]], dma="g")

        def transposes(xi):
            for b in range(NB):
                xb_ap, xb_b = scr_alloc()
                xb16 = as_bf(xb_ap, D)
                P.op("scalar", OPC("copy", out=xb16, in_=X[xi][:, b, :]),
                     reads=[Xb[xi][b]], writes=[xb_b])
                ps, pb = ps_alloc()
                psT = ps[:].bitcast(BF16)
                for k in range(KC):
                    P.op("tensor", OPC("transpose", psT[:, k * 128:(k + 1) * 128],
                                                                                 xb16[:, k * 128:(k + 1) * 128], IDB[:]),
                         reads=[xb_b, cB], writes=[pb])
                P.op("vector", OPC("tensor_copy", out=XT[:, :, b * 128:(b + 1) * 128], in_=psT.rearrange("p (k t) -> p k t", k=KC)),
                     reads=[pb], writes=[XTb[b]])

        def ln_finish(xi, b, slot, eps, last, tile):
            sm = SM[:, xi, b, :]
            bufs = [SMb[xi][b]]
            P.op("vector", OPC("bn_aggr", out=sm[:, 0:2], in_=STT[:, xi, b, :, :].rearrange("p g s -> p (g s)")),
                 reads=[STTb[xi][b]], writes=bufs)
            P.op("vector", OPC("tensor_scalar", out=sm[:, 2:3], in0=sm[:, 1:2], scalar1=eps, scalar2=None,
                                                     op0=ALU.add), reads=bufs, writes=bufs)
            newton_rsqrt("vector", sm[:, 3:4], sm[:, 2:3], sm[:, 4:5], bufs)
            P.op("vector", OPC("scalar_tensor_tensor", out=sm[:, 5:6], in0=sm[:, 0:1], scalar=-1.0, in1=sm[:, 3:4],
                                                            op0=ALU.mult, op1=ALU.mult), reads=bufs, writes=bufs)
            xblk = X[xi][:, b, :]
            P.op("scalar", OPC("activation", out=xblk, in_=xblk, func=AF.Identity, scale=sm[:, 3:4], bias=sm[:, 5:6]),
                 reads=[Xb[xi][b]] + bufs, writes=[Xb[xi][b]])
            P.op("gpsimd", OPC("tensor_tensor", out=xblk, in0=xblk, in1=GB[:, slot, 0, :], op=ALU.mult),
                 reads=[Xb[xi][b], GBb[slot]], writes=[Xb[xi][b]])
            P.op("gpsimd", OPC("tensor_tensor", out=xblk, in0=xblk, in1=GB[:, slot, 1, :], op=ALU.add),
                 reads=[Xb[xi][b], GBb[slot]], writes=[Xb[xi][b]])
            if last:
                t0 = tile * NT
                fin.append(P.op("sync", OPC("dma_start", out=out_d[t0 + b * 128:t0 + (b + 1) * 128, :], in_=xblk),
                                reads=[Xb[xi][b]], dma="o"))

        def resid_ln(xi, pdict, res_scale, slot, eps, last, tile, wrel):
            for half in range(2):
                for b in range(NB):
                    ps, pb = pdict(half, b)
                    if b == NB - 1:
                        wrel(half)
                    xs = X[xi][:, b, half * 512:(half + 1) * 512]
                    P.op("vector", OPC("scalar_tensor_tensor", out=xs, in0=xs, scalar=res_scale, in1=ps[:], op0=ALU.mult, op1=ALU.add),
                         reads=[Xb[xi][b], pb], writes=[Xb[xi][b]])
                    P.op("vector", OPC("bn_stats", out=STT[:, xi, b, half, :], in_=xs), reads=[Xb[xi][b]], writes=[STTb[xi][b]])
                    if half == 1:
                        ln_finish(xi, b, slot, eps, last, tile)

        def ffn(xi, tiles, slot, last, tile):
            gu, dn = tiles
            for f in range(FC):
                HTb[f].aliases = [m for m in mix_alias]
            first = [True]
            for fg in range(FC // 2):
                g, u = gu[fg]
                wneed(g)
                wneed(u)
                for fi in range(2):
                    f = fg * 2 + fi
                    pg, pgb = ps_alloc()
                    pu, pub = ps_alloc()
                    for (wt, ps, pb) in ((g, pg, pgb), (u, pu, pub)):
                        for k in range(KC):
                            MM(pb, ps[:], wt.ap[:, k, fi * 128:(fi + 1) * 128], XT[:, k, :], [wt.buf] + XTb)
                    sg, sgb = scr_alloc()
                    P.op("scalar", OPC("activation", out=sg, in_=pg[:], func=AF.Silu),
                         reads=[pgb], writes=[sgb])
                    P.op("vector", OPC("tensor_tensor", out=HT[:, f, :], in0=sg, in1=pu[:], op=ALU.mult),
                         reads=[sgb, pub], writes=[HTb[f]])
                    if first[0]:
                        for m in mix_alias:
                            m.writer = None
                            m.readers = []
                        first[0] = False
                    HTb[f].aliases = []
                wrelease(g)
                wrelease(u)
            yield

            def pd(half, b):
                for part in dn[half]:
                    wneed(part)
                ps, pb = ps_alloc()
                for f in range(FC):
                    part = dn[half][f // 11]
                    MM(pb, ps[:], HT[:, f, b * 128:(b + 1) * 128], part.ap[:, f % 11, :], [HTb[f], part.buf])
                return ps, pb

            def wrel(half):
                for part in dn[half]:
                    wrelease(part)

            resid_ln(xi, pd, 2.0 * ALPHA, slot, 4.0 * LN_EPS, last, tile, wrel)

        def mixer(xi, tiles, slot, seq_start, par):
            wi, wo = tiles
            for m in mix_alias:
                m.aliases = [h for h in HTb]
            wf, wq, wv, wgo, wcv, wcg = wi[1], wi[0], wi[2], wi[3], wi[4], wi[5]
            NCH = NT // 64

            def rslot(ci):
                return ci if ci > 0 else (0 if par == 0 else NCH)

            def wslot(ci):
                return ci + 1 if ci < NCH - 1 else (NCH if par == 0 else 0)

            def proj_fm(wt, c):
                ps, pb = ps_alloc()
                for k in range(KC):
                    MM(pb, ps[:], wt.ap[:, k, c * 128:(c + 1) * 128], XT[:, k, :], [wt.buf] + XTb)
                return ps, pb

            if seq_start:
                P.op("gpsimd", OPC("memset", S[:], 0.0), writes=Sb_)
                P.op("gpsimd", OPC("memset", SBS[:, rslot(0), :, :], 0.0), writes=SBSb[rslot(0)])
                for c in range(NH):
                    P.op("gpsimd", OPC("memset", UT[:, c, 0:HALO], 0.0), writes=[UTb[c]])

            for b in range(NB):
                Vb[b].aliases = list(CBb)
                KTOKb[b].aliases = list(CQb)
            wneed(wv)
            for b in range(NB):
                ps, pb = ps_alloc()
                for k in range(KC):
                    MM(pb, ps[:], XT[:, k, b * 128:(b + 1) * 128], wv.ap[:, k, :], [wv.buf, XTb[b]])
                P.op("scalar", OPC("copy", out=V[:, b, :], in_=ps[:]), reads=[pb], writes=[Vb[b]])
            wrelease(wv)
            wneed(wf)
            KH = []
            for h in range(NH):
                ps, pb = proj_fm(wf, h)
                fa, fb = scr_alloc()
                P.op("scalar", OPC("activation", out=fa, in_=ps[:], func=AF.Tanh, scale=0.5), reads=[pb], writes=[fb])
                P.op("vector", OPC("tensor_scalar", out=fa, in0=fa, scalar1=PC[:, h, 1:2], scalar2=PC[:, h, 0:1],
                                   op0=ALU.mult, op1=ALU.add), reads=[fb, cB], writes=[fb])
                for c in range(NCH):
                    cs = slice(c * 64, (c + 1) * 64)
                    P.op("vector", OPC("tensor_tensor_scan", out=BC[:, h, cs], data0=fa[:, cs], data1=ONEF[:, 0:64], initial=1.0,
                                       op0=ALU.mult, op1=ALU.mult), reads=[fb, cB], writes=[BCb[h]])
                rb, rbb = scr_alloc()
                P.op("vector", OPC("reciprocal", out=rb, in_=BC[:, h, :]), reads=[BCb[h]], writes=[rbb])
                P.op("vector", OPC("tensor_scalar", out=fa, in0=fa, scalar1=-1.0, scalar2=1.0, op0=ALU.mult, op1=ALU.add),
                     reads=[fb], writes=[fb])
                P.op("vector", OPC("tensor_tensor", out=KT[:, h, :], in0=fa, in1=rb, op=ALU.mult), reads=[fb, rbb], writes=[KTb[h]])
                P.op("gpsimd", OPC("tensor_tensor", out=rb.rearrange("p (c t) -> p c t", t=64), in0=rb.rearrange("p (c t) -> p c t", t=64),
                                   in1=BC[:, h, :].rearrange("p (c t) -> p c t", t=64)[:, :, 63:64].to_broadcast([128, NCH, 64]),
                                   op=ALU.mult), reads=[rbb, BCb[h]], writes=[rbb])
                kh, khb = scr_alloc()
                kh16 = as_bf(kh, NT)
                P.op("gpsimd", OPC("tensor_tensor", out=kh16, in0=fa, in1=rb, op=ALU.mult), reads=[fb, rbb], writes=[khb])
                KH.append((kh16, khb))
            wrelease(wf)
            wneed(wgo)
            for h in range(NH):
                ps, pb = proj_fm(wgo, h)
                P.op("scalar", OPC("activation", out=THG[:, h, :], in_=ps[:], func=AF.Tanh, scale=0.5), reads=[pb], writes=[THGb[h]])
            wrelease(wgo)
            THC = []
            wneed(wcg)
            for c in range(NH):
                ps, pb = proj_fm(wcg, c)
                ta, tb = scr_alloc()
                P.op("scalar", OPC("activation", out=ta, in_=ps[:], func=AF.Tanh, scale=0.5), reads=[pb], writes=[tb])
                THC.append((ta, tb))
            wrelease(wcg)
            wneed(wcv)
            for c in range(NH):
                ps, pb = proj_fm(wcv, c)
                ta, tb = THC[c]
                P.op("vector", OPC("scalar_tensor_tensor", out=UT[:, c, HALO:HALO + NT], in0=ta, scalar=1.0, in1=ps[:],
                                   op0=ALU.add, op1=ALU.mult), reads=[tb, pb], writes=[UTb[c]])
            wrelease(wcv)
            wneed(wq)
            for h in range(NH):
                ps, pb = proj_fm(wq, h)
                P.op("vector", OPC("tensor_tensor", out=QT[:, h, :], in0=ps[:], in1=BC[:, h, :], op=ALU.mult),
                     reads=[pb, BCb[h]], writes=[QTb[h]])
            wrelease(wq)
            for b in range(NB):
                ps, pb = ps_alloc()
                psT = ps[:].bitcast(BF16)
                for h in range(NH):
                    kh16, khb = KH[h]
                    P.op("tensor", OPC("transpose", psT[:, h * 128:(h + 1) * 128], kh16[:, b * 128:(b + 1) * 128], IDB[:]),
                         reads=[khb, cB], writes=[pb])
                P.op("scalar", OPC("copy", out=KTOK[:, b, :], in_=psT[:, 0:512]), reads=[pb], writes=[KTOKb[b]])
            yield

            def conv_mm(c):
                cps, cpb = ps_alloc()
                dg16 = dgb = None
                for j in range(CW):
                    if j % 8 == 0:
                        dg, dgb = scr_alloc()
                        dg16 = as_bf(dg, 1024)
                        n = min(8, CW - j)
                        P.op("gpsimd", OPC("tensor_tensor", out=dg16[:, 0:n * 128].rearrange("p (j t) -> p j t", j=n),
                                           in0=IDF[:].rearrange("p (o t) -> p o t", o=1).to_broadcast([128, n, 128]),
                                           in1=W05[:, c, j:j + n].rearrange("p (j o) -> p j o", o=1).to_broadcast([128, n, 128]),
                                           op=ALU.mult), reads=[cB], writes=[dgb])
                    jj = j % 8
                    MM(cpb, cps[:], dg16[:, jj * 128:(jj + 1) * 128], UT[:, c, j:j + NT], [dgb, UTb[c]])
                P.op("scalar", OPC("activation", out=CS[:, c, :], in_=cps[:], func=AF.Identity, bias=PT[:, c, 3:4]),
                     reads=[cpb, cB], writes=[CSb[c]])

            def conv_fin(c):
                P.op("scalar", OPC("copy", out=CB[:, c, :], in_=CS[:, c, :]), reads=[CSb[c]], writes=[CBb[c]])
                P.op("scalar", OPC("activation", out=CQ[:, c, :], in_=CS[:, c, :], func=AF.Square), reads=[CSb[c]], writes=[CQb[c]])
                P.op("gpsimd", OPC("tensor_copy", out=UT[:, c, 0:HALO], in_=UT[:, c, NT:NT + HALO]), reads=[UTb[c]], writes=[UTb[c]])

            def stage_a(b):
                bs = slice(b * 128, (b + 1) * 128)
                sc, scb = ps_alloc()
                for h in range(NH):
                    MM(scb, sc[:, h * 128:(h + 1) * 128], KT[:, h, bs], QT[:, h, bs], [KTb[h], QTb[h]])
                P.op("vector", OPC("tensor_tensor", out=AT[b][:], in0=sc[:].rearrange("p (h t) -> p h t", h=NH),
                                   in1=MASK[:].rearrange("p (o t) -> p o t", o=1).to_broadcast([128, NH, 128]), op=ALU.mult),
                     reads=[scb, cB], writes=[ATb[b]])
                for c2 in range(2):
                    p_, pb_ = ps_alloc()
                    rs = slice(c2 * 64, (c2 + 1) * 64)
                    for h in range(NH):
                        MM(pb_, p_[:, h * 128:(h + 1) * 128], KTOK[rs, b, h * 128:(h + 1) * 128], V[rs, b, h * 128:(h + 1) * 128],
                           [KTOKb[b], Vb[b]])
                    ci = b * 2 + c2
                    nxt = wslot(ci)
                    for h in range(NH):
                        P.op("vector", OPC("scalar_tensor_tensor", out=S[:, h, :], in0=S[:, h, :], scalar=BC[:, h, ci * 64 + 63:ci * 64 + 64],
                                           in1=p_[:, h * 128:(h + 1) * 128], op0=ALU.mult, op1=ALU.add),
                             reads=[Sb_[h], BCb[h], pb_], writes=[Sb_[h]])
                        P.op("scalar", OPC("copy", out=SBS[:, nxt, h, :], in_=S[:, h, :]), reads=[Sb_[h]], writes=[SBSb[nxt][h]])

            for b in range(NB):
                stage_a(b)
            conv_mm(0)
            conv_mm(1)

            OQ = {}

            for b in range(NB):
                o_ps, o_b = ps_alloc()
                for h in range(NH):
                    MM(o_b, o_ps[:, h * 128:(h + 1) * 128], V[:, b, h * 128:(h + 1) * 128], AT[b][:, h, :], [Vb[b], ATb[b]])
                for c2 in range(2):
                    ci = b * 2 + c2
                    ts_ = slice(b * 128 + c2 * 64, b * 128 + (c2 + 1) * 64)
                    for h in range(NH):
                        MM(o_b, o_ps[:, h * 128 + c2 * 64:h * 128 + (c2 + 1) * 64], SBS[:, rslot(ci), h, :], QT[:, h, ts_],
                           [SBSb[rslot(ci)][h], QTb[h]])
                P.op("scalar", OPC("copy", out=OS[:, b, :], in_=o_ps[:]), reads=[o_b], writes=[OSb[b]])
                oq, oqb = scr_alloc()
                oq16 = as_bf(oq, 512)
                P.op("scalar", OPC("activation", out=oq16, in_=o_ps[:], func=AF.Square), reads=[o_b], writes=[oqb])
                OQ[b] = (oq16, oqb)
            ss, ssb = ps_alloc()
            for b in range(NB):
                oq16, oqb = OQ[b]
                for h in range(NH):
                    MM(ssb, ss[:, b * NH + h:b * NH + h + 1], oq16[:, h * 128:(h + 1) * 128], ONEB[:, 0:1], [oqb, cB])
            smf = SM2[:].rearrange("p a b -> p (a b)")
            NS = NB * NH
            P.op("vector", OPC("tensor_scalar", out=smf[:, 0:NS], in0=ss[:, 0:NS], scalar1=1.0 / 128.0, scalar2=RMS_EPS,
                               op0=ALU.mult, op1=ALU.add), reads=[ssb], writes=SM2b)
            newton_rsqrt("vector", smf[:, NS:2 * NS], smf[:, 0:NS], smf[:, 2 * NS:3 * NS], SM2b)

            def rms_apply(b):
                bs = slice(b * 128, (b + 1) * 128)
                smf = SM2[:].rearrange("p a b -> p (a b)")
                r0 = NB * NH + b * NH
                lt, ltb = scr_alloc()
                ltv = lt.rearrange("p (h t) -> p h t", h=NH)
                P.op("vector", OPC("tensor_copy", out=ltv, in_=smf[:, r0:r0 + NH].rearrange("p (h o) -> p h o", o=1).to_broadcast([128, NH, 128])),
                     reads=SM2b, writes=[ltb])
                rb_ps, rb_b = ps_alloc()
                for h in range(NH):
                    MM(rb_b, rb_ps[:, h * 128:(h + 1) * 128], ltv[:, h, :], IDF[:], [ltb, cB])
                os_ = OS[:, b, :]
                for h in range(NH):
                    P.op("vector", OPC("scalar_tensor_tensor", out=os_[:, h * 128:(h + 1) * 128], in0=os_[:, h * 128:(h + 1) * 128],
                                       scalar=PC[:, h, 2:3], in1=rb_ps[:, h * 128:(h + 1) * 128], op0=ALU.mult, op1=ALU.mult),
                         reads=[OSb[b], rb_b, cB], writes=[OSb[b]])
                P.op("vector", OPC("scalar_tensor_tensor", out=CT[:, 0:NH, bs], in0=THG[:, :, bs], scalar=1.0,
                                   in1=os_.rearrange("p (h t) -> p h t", h=NH), op0=ALU.add, op1=ALU.mult),
                     reads=THGb + [OSb[b]], writes=[CTb[c][b] for c in range(NH)])

            for c in range(NH):
                CBb[c].aliases = list(Vb)
                CQb[c].aliases = list(KTOKb)
            conv_fin(0)
            conv_fin(1)
            for c in (2, 3):
                conv_mm(c)
                conv_fin(c)
                rms_apply(2 * (c - 2))
                rms_apply(2 * (c - 2) + 1)

            stp, stb = ps_alloc()
            for b in range(NB):
                bs = slice(b * 128, (b + 1) * 128)
                for (q, src, srcb) in ((0, CB, CBb), (1, CQ, CQb)):
                    for c in range(NH):
                        MM(stb, stp[:, b * 2 + q:b * 2 + q + 1], src[:, c, bs], ONEB[:, 0:1], [srcb[c], cB])
            cm, cmb = scr_alloc()
            P.op("vector", OPC("tensor_scalar", out=cm[:, 0:2 * NB], in0=stp[:, 0:2 * NB], scalar1=1.0 / 512.0, scalar2=None, op0=ALU.mult),
                 reads=[stb], writes=[cmb])
            cmv = cm[:, 0:2 * NB].rearrange("p (b q) -> p b q", q=2)
            mean = cmv[:, :, 0]
            ex2 = cmv[:, :, 1]
            P.op("vector", OPC("tensor_tensor", out=cm[:, 16:16 + NB], in0=mean, in1=mean, op=ALU.mult), reads=[cmb], writes=[cmb])
            P.op("vector", OPC("tensor_tensor", out=cm[:, 8:8 + NB], in0=ex2, in1=cm[:, 16:16 + NB], op=ALU.subtract), reads=[cmb], writes=[cmb])
            P.op("vector", OPC("tensor_scalar", out=cm[:, 8:8 + NB], in0=cm[:, 8:8 + NB], scalar1=LN_EPS, scalar2=None, op0=ALU.add),
                 reads=[cmb], writes=[cmb])
            newton_rsqrt("vector", cm[:, 12:12 + NB], cm[:, 8:8 + NB], cm[:, 16:16 + NB], [cmb])
            P.op("vector", OPC("scalar_tensor_tensor", out=cm[:, 20:20 + NB], in0=mean, scalar=-1.0, in1=cm[:, 12:12 + NB],
                               op0=ALU.mult, op1=ALU.mult), reads=[cmb], writes=[cmb])
            ra_ps, ra_b = ps_alloc()
            rn_ps, rn_b = ps_alloc()
            for (col0, dst, dstb) in ((12, ra_ps, ra_b), (20, rn_ps, rn_b)):
                lt, ltb = scr_alloc()
                ltv = lt.rearrange("p (b t) -> p b t", b=NB)
                P.op("vector", OPC("tensor_copy", out=ltv, in_=cm[:, col0:col0 + NB].rearrange("p (b o) -> p b o", o=1).to_broadcast([128, NB, 128])),
                     reads=[cmb], writes=[ltb])
                for b in range(NB):
                    MM(dstb, dst[:, b * 128:(b + 1) * 128], ltv[:, b, :], IDF[:], [ltb, cB])
            for c in range(NH):
                P.op("vector", OPC("tensor_tensor", out=CS[:, c, :], in0=CS[:, c, :], in1=ra_ps[:], op=ALU.mult),
                     reads=[CSb[c], ra_b], writes=[CSb[c]])
                P.op("vector", OPC("tensor_tensor", out=CS[:, c, :], in0=CS[:, c, :], in1=rn_ps[:], op=ALU.add),
                     reads=[CSb[c], rn_b], writes=[CSb[c]])
                P.op("scalar", OPC("activation", out=CT[:, NH + c, :], in_=CS[:, c, :], func=AF.Silu, scale=PT[:, c, 4:5], bias=PT[:, c, 5:6]),
                     reads=[CSb[c], cB], writes=[CTb[NH + c][b] for b in range(NB)])

            def pd(half, b):
                wneed(wo[half])
                ps, pb = ps_alloc()
                for k in range(KC):
                    MM(pb, ps[:], CT[:, k, b * 128:(b + 1) * 128], wo[half].ap[:, k, :], [CTb[k][b], wo[half].buf])
                return ps, pb

            def wrel(half):
                wrelease(wo[half])

            resid_ln(xi, pd, ALPHA, slot, LN_EPS, False, 0, wrel)

        assert ntiles % 2 == 0
        wfill()
        load_x(0, 0)
        load_x(1, 1)
        sa, sb_ = 0, 1
        load_gb(0, sa)
        transposes(0)
        for pair in range(ntiles // 2):
            tA, tB = 2 * pair, 2 * pair + 1
            fA, mA, gA = plan[tA]
            fB, mB, gB = plan[tB]
            load_gb(1, sb_)
            g = ffn(0, fA, sa, False, tA); next(g)
            transposes(1)
            next(g, None)
            g = ffn(1, fB, sa, False, tB); next(g)
            transposes(0)
            next(g, None)
            load_gb(2, sa)
            g = mixer(0, mA, sb_, (tA % tiles_per_seq) == 0, 0); next(g)
            transposes(1)
            next(g, None)
            g = mixer(1, mB, sb_, (tB % tiles_per_seq) == 0, 1); next(g)
            transposes(0)
            next(g, None)
            load_gb(0, sb_)
            g = ffn(0, gA, sa, True, tA); next(g)
            transposes(1)
            next(g, None)
            if tA + 2 < ntiles:
                load_x(tA + 2, 0)
            g = ffn(1, gB, sa, True, tB); next(g)
            if tA + 2 < ntiles:
                transposes(0)
            next(g, None)
            if tB + 2 < ntiles:
                load_x(tB + 2, 1)
            sa, sb_ = sb_, sa
        print('sbuf bytes remaining/partition:', nc.sbuf_bytes_remaining)
        P.emit(final_waits=fin)
    return nc


_NC_CACHE = {}


def _get_nc(nseq, T):
    key = (nseq, T)
    if key not in _NC_CACHE:
        _NC_CACHE[key] = build_nc(nseq, T)
    return _NC_CACHE[key]


def kernel(**inputs):
    x = np.asarray(inputs["x"], dtype=np.float32)
    B, T, _ = x.shape
    n = 8
    nseq = B // n
    nc = _get_nc(nseq, T)
    shared = {}
    for k in ("ffn1_w_gate", "ffn1_w_up", "ffn1_w_down", "ffn2_w_gate", "ffn2_w_up", "ffn2_w_down", "w_in", "w_out"):
        a = np.asarray(inputs[k], dtype=np.float32)
        shared[k] = np.ascontiguousarray(a.reshape(a.shape[-2], a.shape[-1]))
    for k in ("ln1_g", "ln1_b", "ln2_g", "ln2_b", "ln3_g", "ln3_b", "hgrn_norm_g", "conv_b", "conv_ln_g", "conv_ln_b"):
        a = np.asarray(inputs[k], dtype=np.float32)
        shared[k] = np.ascontiguousarray(a.reshape(1, a.shape[-1]))
    shared["lb_logits"] = np.ascontiguousarray(np.asarray(inputs["lb_logits"], dtype=np.float32).reshape(2, 512))
    shared["conv_w"] = np.ascontiguousarray(np.asarray(inputs["conv_w"], dtype=np.float32).reshape(CW, 512))
    in_maps = []
    for c in range(n):
        m = dict(shared)
        m["x"] = np.ascontiguousarray(x[c * nseq:(c + 1) * nseq].reshape(nseq * T, D))
        in_maps.append(m)
    res = run_bass_kernel_spmd(nc, in_maps, core_ids=list(range(n)))
    out = np.concatenate([np.asarray(r["out"]).reshape(nseq, T, D) for r in res.results], axis=0)
    return out.astype(np.float32, copy=False)
```

```python
import numpy as np
from contextlib import ExitStack
import concourse.bass as bass
import concourse.mybir as mybir
from concourse.bass_utils import run_bass_kernel_spmd

F32 = mybir.dt.float32
BF16 = mybir.dt.bfloat16
I32 = mybir.dt.int32
AF = mybir.ActivationFunctionType
ALU = mybir.AluOpType

D = 1024
KC = 8
DFF = 2816
FC = 22
DIN = 3072
NH = 4
CW = 31
HALO = CW - 1
ALPHA = 2.0 ** 0.25
LN_EPS = 1e-5
RMS_EPS = 1e-6
NT = 512
NB = NT // 128
RING_ELEMS = 22 * 1024
NSCR = 14
MAGIC = 0x5F3759DF

ENGINES = ("tensor", "vector", "scalar", "gpsimd", "sync")


class Buf:
    __slots__ = ("name", "writer", "readers", "aliases", "dead")

    def __init__(self, name, aliases=()):
        self.name = name
        self.writer = None
        self.readers = []
        self.aliases = list(aliases)
        self.dead = False


class Op:
    __slots__ = ("eng", "fn", "dma", "deps", "signal", "sem", "val", "waits", "pre")

    def __init__(self, eng, fn, dma):
        self.eng = eng
        self.fn = fn
        self.dma = dma
        self.deps = []
        self.signal = False
        self.sem = None
        self.val = 0
        self.waits = []


def OPC(name, *args, **kwargs):
    return lambda e: getattr(e, name)(*args, **kwargs)


class Prog:
    def __init__(self, nc):
        self.nc = nc
        self.ops = []

    def op(self, eng, fn, reads=(), writes=(), dma=None):
        o = Op(eng, fn, dma)
        deps = {}
        for b in reads:
            assert not b.dead, "read of dead buffer " + b.name
            for bb in [b] + b.aliases:
                if bb.writer is not None:
                    deps[id(bb.writer)] = bb.writer
        for b in writes:
            assert not b.dead, "write of dead buffer " + b.name
            for bb in [b] + b.aliases:
                if bb.writer is not None:
                    deps[id(bb.writer)] = bb.writer
                last = {}
                for r in bb.readers:
                    if r.dma is not None:
                        deps[id(r)] = r
                    else:
                        last[r.eng] = r
                for r in last.values():
                    deps[id(r)] = r
        for d in deps.values():
            if d is o:
                continue
            if d.eng == "tensor" and eng == "tensor" and d.dma is None and dma is None:
                continue
            o.deps.append(d)
            d.signal = True
        for b in reads:
            b.readers.append(o)
        for b in writes:
            b.readers = []
            for bb in b.aliases:
                bb.readers = []
                bb.writer = None
            b.aliases = []
            b.writer = o
        self.ops.append(o)
        return o

    def emit(self, final_waits=()):
        nc = self.nc
        with ExitStack() as es:
            sems = {}

            def get_sem(key):
                if key not in sems:
                    sems[key] = es.enter_context(nc.semaphore("s_" + key))
                return sems[key]

            counts = {}
            KDMA = 8
            for o in final_waits:
                o.signal = True
            for o in self.ops:
                o.pre = None
                if o.dma:
                    n = counts.get("d_" + o.dma, 0)
                    counts["d_" + o.dma] = n + 1
                    o.signal = True
                    o.sem = get_sem("d_%s_%d" % (o.dma, n % KDMA))
                    o.val = 16 * (n // KDMA + 1)
                    if n >= KDMA:
                        o.pre = (o.sem, 16 * (n // KDMA))
                    continue
                if not o.signal:
                    continue
                key = "e_" + o.eng
                counts[key] = counts.get(key, 0) + 1
                o.sem = get_sem(key)
                o.val = counts[key]
            self.counts = counts
            known = {e: {} for e in ENGINES}
            per_eng = {e: [] for e in ENGINES}
            for o in self.ops:
                need = {}
                if o.pre is not None:
                    need[id(o.pre[0])] = o.pre
                for d in o.deps:
                    k = id(d.sem)
                    if k not in need or need[k][1] < d.val:
                        need[k] = (d.sem, d.val)
                kn = known[o.eng]
                for k, (s, v) in need.items():
                    if kn.get(k, 0) >= v:
                        continue
                    kn[k] = v
                    o.waits.append((s, v))
                per_eng[o.eng].append(o)
            with nc.Block() as block:
                def mk(engname):
                    def body(eng):
                        for o in per_eng[engname]:
                            for (s, v) in o.waits:
                                eng.wait_ge(s, v)
                            ins = o.fn(eng)
                            if o.signal:
                                ins.then_inc(o.sem, 16 if o.dma else 1)
                        if engname == "sync":
                            for o in final_waits:
                                eng.wait_ge(o.sem, o.val)
                    return body
                for e in ENGINES:
                    if per_eng[e] or e == "sync":
                        getattr(block, e)(mk(e))


def build_nc(nseq, T):
    ntok = nseq * T
    tiles_per_seq = T // NT
    ntiles = nseq * tiles_per_seq
    nc = bass.Bass("TRN2", target_bir_lowering=False)

    def din(name, shape):
        return nc.dram_tensor(name, list(shape), F32, kind="ExternalInput").ap()

    x_d = din("x", [ntok, D])
    wg_d = [din("ffn1_w_gate", [D, DFF]), din("ffn2_w_gate", [D, DFF])]
    wu_d = [din("ffn1_w_up", [D, DFF]), din("ffn2_w_up", [D, DFF])]
    wd_d = [din("ffn1_w_down", [DFF, D]), din("ffn2_w_down", [DFF, D])]
    win_d = din("w_in", [D, DIN])
    wout_d = din("w_out", [D, D])
    lng_d = [din("ln1_g", [1, D]), din("ln2_g", [1, D]), din("ln3_g", [1, D])]
    lnb_d = [din("ln1_b", [1, D]), din("ln2_b", [1, D]), din("ln3_b", [1, D])]
    lb_d = din("lb_logits", [2, 512])
    hg_d = din("hgrn_norm_g", [1, 512])
    cw_d = din("conv_w", [CW, 512])
    cb_d = din("conv_b", [1, 512])
    clg_d = din("conv_ln_g", [1, 512])
    clb_d = din("conv_ln_b", [1, 512])
    out_d = nc.dram_tensor("out", [ntok, D], F32, kind="ExternalOutput").ap()

    def dscr(name, shape):
        return nc.dram_tensor(name, list(shape), BF16, kind="Internal").ap()

    wg_s = [dscr("wg1_bf", [D, DFF]), dscr("wg2_bf", [D, DFF])]
    wu_s = [dscr("wu1_bf", [D, DFF]), dscr("wu2_bf", [D, DFF])]
    wd_s = [dscr("wd1_bf", [DFF, D]), dscr("wd2_bf", [DFF, D])]
    win_s = dscr("win_bf", [D, DIN])
    wout_s = dscr("wout_bf", [D, D])

    P = Prog(nc)
    fin = []
    with ExitStack() as es:
        def sb(name, shape, dt):
            return es.enter_context(nc.sbuf_tensor(name, list(shape), dt))

        X = [sb("X%d" % i, [128, NB, D], F32) for i in range(2)]
        Xb = [[Buf("X%d_%d" % (i, b)) for b in range(NB)] for i in range(2)]
        XT = sb("XT", [128, KC, NT], BF16)
        XTb = [Buf("XT%d" % b) for b in range(NB)]
        HT = sb("HT", [128, FC, NT], BF16)
        HTb = [Buf("HT%d" % f) for f in range(FC)]
        HTf = HT[:].rearrange("p f t -> p (f t)")
        BC = HTf[:, 0:4096].bitcast(F32).rearrange("p (h t) -> p h t", h=NH)
        CS = HTf[:, 4096:8192].bitcast(F32).rearrange("p (h t) -> p h t", h=NH)
        THG = HTf[:, 8192:10240].rearrange("p (h t) -> p h t", h=NH)
        BCb = [Buf("BC%d" % h, aliases=HTb) for h in range(NH)]
        CSb = [Buf("CS%d" % h, aliases=HTb) for h in range(NH)]
        THGb = [Buf("THG%d" % h, aliases=HTb) for h in range(NH)]
        mix_alias = BCb + CSb + THGb

        KT = sb("KT", [128, NH, NT], BF16)
        KTb = [Buf("KT%d" % h) for h in range(NH)]
        QT = sb("QT", [128, NH, NT], BF16)
        QTb = [Buf("QT%d" % h) for h in range(NH)]
        V = sb("V", [128, NB, 512], BF16)
        Vb = [Buf("V%d" % b) for b in range(NB)]
        KTOK = sb("KTOK", [128, NB, 512], BF16)
        KTOKb = [Buf("KTOK%d" % b) for b in range(NB)]
        AT = [sb("AT%d" % i, [128, NH, 128], BF16) for i in range(NB)]
        ATb = [Buf("AT%d" % i) for i in range(NB)]
        S = sb("S", [128, NH, 128], F32)
        Sb_ = [Buf("S%d" % h) for h in range(NH)]
        SBS = sb("SBS", [128, NT // 64 + 1, NH, 128], BF16)
        SBSb = [[Buf("SBS%d_%d" % (i, h)) for h in range(NH)] for i in range(NT // 64 + 1)]
        UT = sb("UT", [128, NH, HALO + NT], BF16)
        UTb = [Buf("UT%d" % c) for c in range(NH)]
        CB = V
        CBb = [Buf("CB%d" % c) for c in range(NH)]
        CQ = KTOK
        CQb = [Buf("CQ%d" % c) for c in range(NH)]
        OS = sb("OS", [128, NB, 512], F32)
        OSb = [Buf("OS%d" % b) for b in range(NB)]
        CT = sb("CT", [128, KC, NT], BF16)
        CTb = [[Buf("CT%d_%d" % (c, b)) for b in range(NB)] for c in range(KC)]
        GB = sb("GB", [128, 2, 2, D], F32)
        GBb = [Buf("GB%d" % i) for i in range(2)]
        SCR = sb("SCR", [128, NSCR, 512], F32)
        SCRb = [Buf("SCR%d" % i) for i in range(NSCR)]
        RING = sb("RING", [128, RING_ELEMS], BF16)
        IDF = sb("IDF", [128, 128], F32)
        IDB = sb("IDB", [128, 128], BF16)
        ONEB = sb("ONEB", [128, 128], BF16)
        ONEF = sb("ONEF", [128, 64], F32)
        MASK = sb("MASK", [128, 128], F32)
        PR = sb("PR", [37, 512], F32)
        PT = sb("PT", [128, NH, 37], F32)
        PC = sb("PC", [128, NH, 8], F32)
        W05 = sb("W05", [128, NH, CW], F32)
        STT = sb("STT", [128, 2, NB, 2, 6], F32)
        SM = sb("SM", [128, 2, NB, 8], F32)
        SM2 = sb("SM2", [128, 4, 16], F32)
        cB = Buf("consts")
        STTb = [[Buf("STT%d_%d" % (i, b)) for b in range(NB)] for i in range(2)]
        SMb = [[Buf("SM%d_%d" % (i, b)) for b in range(NB)] for i in range(2)]
        SM2b = [Buf("SM2_%d" % i) for i in range(4)]

        psum = [es.enter_context(nc.psum_tensor("PS%d" % i, [128, 512], F32)) for i in range(8)]
        psb = [Buf("PS%d" % i) for i in range(8)]
        st = {"ps": 0, "scr": 0, "sm2": 0, "at": 0}

        ps_first = {}

        def ps_alloc():
            i = st["ps"] % 8
            st["ps"] += 1
            ps_first[id(psb[i])] = True
            return psum[i], psb[i]

        def MM(pb, out, lhsT, rhs, reads):
            first = ps_first[id(pb)]
            ps_first[id(pb)] = False
            P.op("tensor", OPC("matmul", out, lhsT=lhsT, rhs=rhs, start=first, stop=True, skip_group_check=True),
                 reads=reads, writes=[pb])

        class Scr:
            pass

        def scr_alloc():
            i = st["scr"] % NSCR
            st["scr"] += 1
            old = SCRb[i]
            nb = Buf("SCR%d_%d" % (i, st["scr"]), aliases=[old] + old.aliases)
            old.dead = True
            SCRb[i] = nb
            return SCR[:, i, :], nb

        def as_bf(ap, n):
            return ap.bitcast(BF16)[:, 0:n]

        ring = {"head": 0, "live": []}

        def ring_alloc(n):
            start = ring["head"]
            if start + n > RING_ELEMS:
                start = 0
            end = start + n
            aliases = []
            keep = []
            for (s0, e0, b0, rel) in ring["live"]:
                if s0 < end and start < e0:
                    if not rel[0]:
                        return None
                    aliases.append(b0)
                else:
                    keep.append((s0, e0, b0, rel))
            for b0 in aliases:
                b0.dead = True
            al = []
            for b0 in aliases:
                al.append(b0)
                al.extend(b0.aliases)
            nb = Buf("W@%d" % start, aliases=al)
            rel = [False]
            keep.append((start, end, nb, rel))
            ring["live"] = keep
            ring["head"] = end
            return start, nb, rel

        scr_bufs = {}

        class WTile:
            def __init__(self, src_ap, shape, scr_ap=None, key=None):
                self.src = src_ap
                self.scr = scr_ap
                self.key = key
                self.shape = shape
                self.ap = None
                self.buf = None
                self.rel = None

        wstream = []
        wcur = {"i": 0}

        def wfill():
            while wcur["i"] < len(wstream):
                wt = wstream[wcur["i"]]
                a, b = wt.shape
                r = ring_alloc(a * b)
                if r is None:
                    return
                start, nb, rel = r
                wt.ap = RING[:, start:start + a * b].rearrange("p (a b) -> p a b", a=a)
                wt.buf = nb
                wt.rel = rel
                if wt.key not in scr_bufs:
                    P.op("gpsimd", OPC("dma_start", out=wt.ap, in_=wt.src), writes=[nb], dma="w")
                    sbuf_ = Buf("scr_%s" % (wt.key,))
                    scr_bufs[wt.key] = sbuf_
                    P.op("sync", OPC("dma_start", out=wt.scr, in_=wt.ap), reads=[nb], writes=[sbuf_], dma="wb")
                else:
                    P.op("sync", OPC("dma_start", out=wt.ap, in_=wt.scr), reads=[scr_bufs[wt.key]], writes=[nb], dma="w2")
                wcur["i"] += 1

        def wneed(wt):
            if wt.buf is None:
                wfill()
            assert wt.buf is not None, "ring too small / stream order"
            return wt

        def wrelease(wt):
            wt.rel[0] = True
            wfill()

        def ffn_tiles(l):
            gu = []
            for fg in range(FC // 2):
                cs = slice(fg * 256, (fg + 1) * 256)
                g = WTile(wg_d[l].rearrange("(k p) f -> p k f", p=128)[:, :, cs], (KC, 256),
                          wg_s[l].rearrange("(k p) f -> p k f", p=128)[:, :, cs], ("g", l, fg))
                u = WTile(wu_d[l].rearrange("(k p) f -> p k f", p=128)[:, :, cs], (KC, 256),
                          wu_s[l].rearrange("(k p) f -> p k f", p=128)[:, :, cs], ("u", l, fg))
                gu.append((g, u))
            dn = []
            for half in range(2):
                parts = []
                for q in range(2):
                    src = wd_d[l].rearrange("(f p) d -> p f d", p=128)[:, q * 11:(q + 1) * 11, half * 512:(half + 1) * 512]
                    scr = wd_s[l].rearrange("(f p) d -> p f d", p=128)[:, q * 11:(q + 1) * 11, half * 512:(half + 1) * 512]
                    parts.append(WTile(src, (11, 512), scr, ("d", l, half, q)))
                dn.append(parts)
            return gu, dn

        def mix_tiles():
            wi = []
            for g in range(6):
                wi.append(WTile(win_d.rearrange("(k p) c -> p k c", p=128)[:, :, g * 512:(g + 1) * 512], (KC, 512),
                                win_s.rearrange("(k p) c -> p k c", p=128)[:, :, g * 512:(g + 1) * 512], ("i", g)))
            wo = []
            for half in range(2):
                wo.append(WTile(wout_d.rearrange("(k p) c -> p k c", p=128)[:, :, half * 512:(half + 1) * 512], (KC, 512),
                                wout_s.rearrange("(k p) c -> p k c", p=128)[:, :, half * 512:(half + 1) * 512], ("o", half)))
            return wi, wo

        plan = []
        for t in range(ntiles):
            plan.append((ffn_tiles(0), mix_tiles(), ffn_tiles(1)))

        def push_ffn(ft):
            gu, dn = ft
            for g, u in gu:
                wstream.extend([g, u])
            for parts in dn:
                wstream.extend(parts)

        def push_mix(mt):
            wi, wo = mt
            wstream.extend([wi[2], wi[1], wi[3], wi[5], wi[4], wi[0]])
            wstream.extend(wo)

        for pair in range(ntiles // 2):
            a, b = plan[2 * pair], plan[2 * pair + 1]
            push_ffn(a[0]); push_ffn(b[0])
            push_mix(a[1]); push_mix(b[1])
            push_ffn(a[2]); push_ffn(b[2])

        P.op("gpsimd", OPC("memset", IDF[:], 1.0), writes=[cB])
        P.op("gpsimd", OPC("affine_select", out=IDF[:], in_=IDF[:], pattern=[[-1, 128]],
                                                 compare_op=ALU.is_equal, fill=0.0, base=0,
                                                 channel_multiplier=1), reads=[cB], writes=[cB])
        P.op("gpsimd", OPC("memset", MASK[:], 1.0), writes=[cB])
        P.op("gpsimd", OPC("affine_select", out=MASK[:], in_=MASK[:], pattern=[[1, 128]],
                                                 compare_op=ALU.is_ge, fill=0.0, base=0,
                                                 channel_multiplier=-1), reads=[cB], writes=[cB])
        P.op("gpsimd", OPC("memset", MASK[0:64, 64:128], 0.0), reads=[cB], writes=[cB])
        P.op("gpsimd", OPC("memset", ONEB[:], 1.0), writes=[cB])
        P.op("gpsimd", OPC("memset", ONEF[:], 1.0), writes=[cB])
        P.op("vector", OPC("tensor_copy", out=IDB[:], in_=IDF[:]), reads=[cB], writes=[cB])
        rows = [(lb_d, 0, 2), (hg_d, 2, 1), (cb_d, 3, 1), (clg_d, 4, 1), (clb_d, 5, 1), (cw_d, 6, CW)]
        for (src, r0, n) in rows:
            P.op("sync", OPC("dma_start", out=PR[r0:r0 + n, :], in_=src),
                 writes=[cB], dma="c")
        pt_ps, pt_b = ps_alloc()
        for c in range(NH):
            MM(pt_b, pt_ps[:, c * 37:(c + 1) * 37], PR[0:37, c * 128:(c + 1) * 128], IDF[0:37, 0:37], [cB])
        P.op("vector", OPC("tensor_copy", out=PT[:].rearrange("p h r -> p (h r)"), in_=pt_ps[:, 0:NH * 37]),
             reads=[pt_b], writes=[cB])
        P.op("vector", OPC("tensor_tensor", out=PC[:, :, 3], in0=PT[:, :, 0], in1=PT[:, :, 1], op=ALU.subtract),
             reads=[cB], writes=[cB])
        P.op("scalar", OPC("activation", out=PC[:, :, 4], in_=PC[:, :, 3], func=AF.Tanh, scale=0.5),
             reads=[cB], writes=[cB])
        P.op("vector", OPC("tensor_scalar", out=PC[:, :, 0], in0=PC[:, :, 4], scalar1=0.25, scalar2=0.75,
                                                 op0=ALU.mult, op1=ALU.add), reads=[cB], writes=[cB])
        P.op("vector", OPC("tensor_scalar", out=PC[:, :, 1], in0=PC[:, :, 4], scalar1=-0.25, scalar2=0.25,
                                                 op0=ALU.mult, op1=ALU.add), reads=[cB], writes=[cB])
        P.op("vector", OPC("tensor_scalar", out=PC[:, :, 2], in0=PT[:, :, 2], scalar1=0.5, scalar2=None,
                                                 op0=ALU.mult), reads=[cB], writes=[cB])
        P.op("vector", OPC("tensor_scalar", out=W05[:], in0=PT[:, :, 6:6 + CW], scalar1=0.5, scalar2=None,
                                                 op0=ALU.mult), reads=[cB], writes=[cB])

        def newton_rsqrt(eng, y, v, tmp, bufs, n_iter=2):
            P.op(eng, OPC("tensor_scalar", out=y.bitcast(I32), in0=v.bitcast(I32), scalar1=1, scalar2=None,
                                                op0=ALU.arith_shift_right), reads=bufs, writes=bufs)
            P.op(eng, OPC("tensor_scalar", out=y.bitcast(I32), in0=y.bitcast(I32), scalar1=-1, scalar2=MAGIC,
                                                op0=ALU.mult, op1=ALU.add), reads=bufs, writes=bufs)
            for _ in range(n_iter):
                P.op(eng, OPC("tensor_tensor", out=tmp, in0=y, in1=y, op=ALU.mult), reads=bufs, writes=bufs)
                P.op(eng, OPC("tensor_tensor", out=tmp, in0=tmp, in1=v, op=ALU.mult), reads=bufs, writes=bufs)
                P.op(eng, OPC("tensor_scalar", out=tmp, in0=tmp, scalar1=-0.5, scalar2=1.5,
                                                    op0=ALU.mult, op1=ALU.add), reads=bufs, writes=bufs)
                P.op(eng, OPC("tensor_tensor", out=y, in0=y, in1=tmp, op=ALU.mult), reads=bufs, writes=bufs)

        def load_x(tile, xi):
            t0 = tile * NT
            for b in range(NB):
                P.op("sync", OPC("dma_start", out=X[xi][:, b, :], in_=x_d[t0 + b * 128:t0 + (b + 1) * 128, :]),
                     writes=[Xb[xi][b]], dma="x")

        def load_gb(li, slot):
            P.op("sync", OPC("dma_start", out=GB[:, slot, 0, :], in_=lng_d[li].partition_broadcast(128)),
                 writes=[GBb[slot]], dma="g")
            P.op("sync", OPC("dma_start", out=GB[:, slot, 1, :], in_=lnb_d[li].partition_broadcast(128)),
                 writes=[GBb[slot]], dma="g")

        def transposes(xi):
            for b in range(NB):
                xb_ap, xb_b = scr_alloc()
                xb16 = as_bf(xb_ap, D)
                P.op("scalar", OPC("copy", out=xb16, in_=X[xi][:, b, :]),
                     reads=[Xb[xi][b]], writes=[xb_b])
                ps, pb = ps_alloc()
                psT = ps[:].bitcast(BF16)
                for k in range(KC):
                    P.op("tensor", OPC("transpose", psT[:, k * 128:(k + 1) * 128],
                                                                                 xb16[:, k * 128:(k + 1) * 128], IDB[:]),
                         reads=[xb_b, cB], writes=[pb])
                P.op("vector", OPC("tensor_copy", out=XT[:, :, b * 128:(b + 1) * 128], in_=psT.rearrange("p (k t) -> p k t", k=KC)),
                     reads=[pb], writes=[XTb[b]])

        def ln_finish(xi, b, slot, eps, last, tile):
            sm = SM[:, xi, b, :]
            bufs = [SMb[xi][b]]
            P.op("vector", OPC("bn_aggr", out=sm[:, 0:2], in_=STT[:, xi, b, :, :].rearrange("p g s -> p (g s)")),
                 reads=[STTb[xi][b]], writes=bufs)
            P.op("vector", OPC("tensor_scalar", out=sm[:, 2:3], in0=sm[:, 1:2], scalar1=eps, scalar2=None,
                                                     op0=ALU.add), reads=bufs, writes=bufs)
            newton_rsqrt("vector", sm[:, 3:4], sm[:, 2:3], sm[:, 4:5], bufs)
            P.op("vector", OPC("scalar_tensor_tensor", out=sm[:, 5:6], in0=sm[:, 0:1], scalar=-1.0, in1=sm[:, 3:4],
                                                            op0=ALU.mult, op1=ALU.mult), reads=bufs, writes=bufs)
            xblk = X[xi][:, b, :]
            P.op("scalar", OPC("activation", out=xblk, in_=xblk, func=AF.Identity, scale=sm[:, 3:4], bias=sm[:, 5:6]),
                 reads=[Xb[xi][b]] + bufs, writes=[Xb[xi][b]])
            P.op("gpsimd", OPC("tensor_tensor", out=xblk, in0=xblk, in1=GB[:, slot, 0, :], op=ALU.mult),
                 reads=[Xb[xi][b], GBb[slot]], writes=[Xb[xi][b]])
            P.op("gpsimd", OPC("tensor_tensor", out=xblk, in0=xblk, in1=GB[:, slot, 1, :], op=ALU.add),
                 reads=[Xb[xi][b], GBb[slot]], writes=[Xb[xi][b]])
            if last:
                t0 = tile * NT
                fin.append(P.op("sync", OPC("dma_start", out=out_d[t0 + b * 128:t0 + (b + 1) * 128, :], in_=xblk),
                                reads=[Xb[xi][b]], dma="o"))

        def resid_ln(xi, pdict, res_scale, slot, eps, last, tile, wrel):
            for half in range(2):
                for b in range(NB):
                    ps, pb = pdict(half, b)
                    if b == NB - 1:
                        wrel(half)
                    xs = X[xi][:, b, half * 512:(half + 1) * 512]
                    P.op("vector", OPC("scalar_tensor_tensor", out=xs, in0=xs, scalar=res_scale, in1=ps[:], op0=ALU.mult, op1=ALU.add),
                         reads=[Xb[xi][b], pb], writes=[Xb[xi][b]])
                    P.op("vector", OPC("bn_stats", out=STT[:, xi, b, half, :], in_=xs), reads=[Xb[xi][b]], writes=[STTb[xi][b]])
                    if half == 1:
                        ln_finish(xi, b, slot, eps, last, tile)

        def ffn(xi, tiles, slot, last, tile):
            gu, dn = tiles
            for f in range(FC):
                HTb[f].aliases = [m for m in mix_alias]
            first = [True]
            for fg in range(FC // 2):
                g, u = gu[fg]
                wneed(g)
                wneed(u)
                for fi in range(2):
                    f = fg * 2 + fi
                    pg, pgb = ps_alloc()
                    pu, pub = ps_alloc()
                    for (wt, ps, pb) in ((g, pg, pgb), (u, pu, pub)):
                        for k in range(KC):
                            MM(pb, ps[:], wt.ap[:, k, fi * 128:(fi + 1) * 128], XT[:, k, :], [wt.buf] + XTb)
                    sg, sgb = scr_alloc()
                    P.op("scalar", OPC("activation", out=sg, in_=pg[:], func=AF.Silu),
                         reads=[pgb], writes=[sgb])
                    P.op("vector", OPC("tensor_tensor", out=HT[:, f, :], in0=sg, in1=pu[:], op=ALU.mult),
                         reads=[sgb, pub], writes=[HTb[f]])
                    if first[0]:
                        for m in mix_alias:
                            m.writer = None
                            m.readers = []
                        first[0] = False
                    HTb[f].aliases = []
                wrelease(g)
                wrelease(u)
            yield

            def pd(half, b):
                for part in dn[half]:
                    wneed(part)
                ps, pb = ps_alloc()
                for f in range(FC):
                    part = dn[half][f // 11]
                    MM(pb, ps[:], HT[:, f, b * 128:(b + 1) * 128], part.ap[:, f % 11, :], [HTb[f], part.buf])
                return ps, pb

            def wrel(half):
                for part in dn[half]:
                    wrelease(part)

            resid_ln(xi, pd, 2.0 * ALPHA, slot, 4.0 * LN_EPS, last, tile, wrel)

        def mixer(xi, tiles, slot, seq_start, par):
            wi, wo = tiles
            for m in mix_alias:
                m.aliases = [h for h in HTb]
            wf, wq, wv, wgo, wcv, wcg = wi[1], wi[0], wi[2], wi[3], wi[4], wi[5]
            NCH = NT // 64

            def rslot(ci):
                return ci if ci > 0 else (0 if par == 0 else NCH)

            def wslot(ci):
                return ci + 1 if ci < NCH - 1 else (NCH if par == 0 else 0)

            def proj_fm(wt, c):
                ps, pb = ps_alloc()
                for k in range(KC):
                    MM(pb, ps[:], wt.ap[:, k, c * 128:(c + 1) * 128], XT[:, k, :], [wt.buf] + XTb)
                return ps, pb

            if seq_start:
                P.op("gpsimd", OPC("memset", S[:], 0.0), writes=Sb_)
                P.op("gpsimd", OPC("memset", SBS[:, rslot(0), :, :], 0.0), writes=SBSb[rslot(0)])
                for c in range(NH):
                    P.op("gpsimd", OPC("memset", UT[:, c, 0:HALO], 0.0), writes=[UTb[c]])

            for b in range(NB):
                Vb[b].aliases = list(CBb)
                KTOKb[b].aliases = list(CQb)
            wneed(wv)
            for b in range(NB):
                ps, pb = ps_alloc()
                for k in range(KC):
                    MM(pb, ps[:], XT[:, k, b * 128:(b + 1) * 128], wv.ap[:, k, :], [wv.buf, XTb[b]])
                P.op("scalar", OPC("copy", out=V[:, b, :], in_=ps[:]), reads=[pb], writes=[Vb[b]])
            wrelease(wv)
            wneed(wf)
            KH = []
            for h in range(NH):
                ps, pb = proj_fm(wf, h)
                fa, fb = scr_alloc()
                P.op("scalar", OPC("activation", out=fa, in_=ps[:], func=AF.Tanh, scale=0.5), reads=[pb], writes=[fb])
                P.op("vector", OPC("tensor_scalar", out=fa, in0=fa, scalar1=PC[:, h, 1:2], scalar2=PC[:, h, 0:1],
                                   op0=ALU.mult, op1=ALU.add), reads=[fb, cB], writes=[fb])
                for c in range(NCH):
                    cs = slice(c * 64, (c + 1) * 64)
                    P.op("vector", OPC("tensor_tensor_scan", out=BC[:, h, cs], data0=fa[:, cs], data1=ONEF[:, 0:64], initial=1.0,
                                       op0=ALU.mult, op1=ALU.mult), reads=[fb, cB], writes=[BCb[h]])
                rb, rbb = scr_alloc()
                P.op("vector", OPC("reciprocal", out=rb, in_=BC[:, h, :]), reads=[BCb[h]], writes=[rbb])
                P.op("vector", OPC("tensor_scalar", out=fa, in0=fa, scalar1=-1.0, scalar2=1.0, op0=ALU.mult, op1=ALU.add),
                     reads=[fb], writes=[fb])
                P.op("vector", OPC("tensor_tensor", out=KT[:, h, :], in0=fa, in1=rb, op=ALU.mult), reads=[fb, rbb], writes=[KTb[h]])
                P.op("gpsimd", OPC("tensor_tensor", out=rb.rearrange("p (c t) -> p c t", t=64), in0=rb.rearrange("p (c t) -> p c t", t=64),
                                   in1=BC[:, h, :].rearrange("p (c t) -> p c t", t=64)[:, :, 63:64].to_broadcast([128, NCH, 64]),
                                   op=ALU.mult), reads=[rbb, BCb[h]], writes=[rbb])
                kh, khb = scr_alloc()
                kh16 = as_bf(kh, NT)
                P.op("gpsimd", OPC("tensor_tensor", out=kh16, in0=fa, in1=rb, op=ALU.mult), reads=[fb, rbb], writes=[khb])
                KH.append((kh16, khb))
            wrelease(wf)
            wneed(wgo)
            for h in range(NH):
                ps, pb = proj_fm(wgo, h)
                P.op("scalar", OPC("activation", out=THG[:, h, :], in_=ps[:], func=AF.Tanh, scale=0.5), reads=[pb], writes=[THGb[h]])
            wrelease(wgo)
            THC = []
            wneed(wcg)
            for c in range(NH):
                ps, pb = proj_fm(wcg, c)
                ta, tb = scr_alloc()
                P.op("scalar", OPC("activation", out=ta, in_=ps[:], func=AF.Tanh, scale=0.5), reads=[pb], writes=[tb])
                THC.append((ta, tb))
            wrelease(wcg)
            wneed(wcv)
            for c in range(NH):
                ps, pb = proj_fm(wcv, c)
                ta, tb = THC[c]
                P.op("vector", OPC("scalar_tensor_tensor", out=UT[:, c, HALO:HALO + NT], in0=ta, scalar=1.0, in1=ps[:],
                                   op0=ALU.add, op1=ALU.mult), reads=[tb, pb], writes=[UTb[c]])
            wrelease(wcv)
            wneed(wq)
            for h in range(NH):
                ps, pb = proj_fm(wq, h)
                P.op("vector", OPC("tensor_tensor", out=QT[:, h, :], in0=ps[:], in1=BC[:, h, :], op=ALU.mult),
                     reads=[pb, BCb[h]], writes=[QTb[h]])
            wrelease(wq)
            for b in range(NB):
                ps, pb = ps_alloc()
                psT = ps[:].bitcast(BF16)
                for h in range(NH):
                    kh16, khb = KH[h]
                    P.op("tensor", OPC("transpose", psT[:, h * 128:(h + 1) * 128], kh16[:, b * 128:(b + 1) * 128], IDB[:]),
                         reads=[khb, cB], writes=[pb])
                P.op("scalar", OPC("copy", out=KTOK[:, b, :], in_=psT[:, 0:512]), reads=[pb], writes=[KTOKb[b]])
            yield

            def conv_mm(c):
                cps, cpb = ps_alloc()
                dg16 = dgb = None
                for j in range(CW):
                    if j % 8 == 0:
                        dg, dgb = scr_alloc()
                        dg16 = as_bf(dg, 1024)
                        n = min(8, CW - j)
                        P.op("gpsimd", OPC("tensor_tensor", out=dg16[:, 0:n * 128].rearrange("p (j t) -> p j t", j=n),
                                           in0=IDF[:].rearrange("p (o t) -> p o t", o=1).to_broadcast([128, n, 128]),
                                           in1=W05[:, c, j:j + n].rearrange("p (j o) -> p j o", o=1).to_broadcast([128, n, 128]),
                                           op=ALU.mult), reads=[cB], writes=[dgb])
                    jj = j % 8
                    MM(cpb, cps[:], dg16[:, jj * 128:(jj + 1) * 128], UT[:, c, j:j + NT], [dgb, UTb[c]])
                P.op("scalar", OPC("activation", out=CS[:, c, :], in_=cps[:], func=AF.Identity, bias=PT[:, c, 3:4]),
                     reads=[cpb, cB], writes=[CSb[c]])

            def conv_fin(c):
                P.op("scalar", OPC("copy", out=CB[:, c, :], in_=CS[:, c, :]), reads=[CSb[c]], writes=[CBb[c]])
                P.op("scalar", OPC("activation", out=CQ[:, c, :], in_=CS[:, c, :], func=AF.Square), reads=[CSb[c]], writes=[CQb[c]])
                P.op("gpsimd", OPC("tensor_copy", out=UT[:, c, 0:HALO], in_=UT[:, c, NT:NT + HALO]), reads=[UTb[c]], writes=[UTb[c]])

            def stage_a(b):
                bs = slice(b * 128, (b + 1) * 128)
                sc, scb = ps_alloc()
                for h in range(NH):
                    MM(scb, sc[:, h * 128:(h + 1) * 128], KT[:, h, bs], QT[:, h, bs], [KTb[h], QTb[h]])
                P.op("vector", OPC("tensor_tensor", out=AT[b][:], in0=sc[:].rearrange("p (h t) -> p h t", h=NH),
                                   in1=MASK[:].rearrange("p (o t) -> p o t", o=1).to_broadcast([128, NH, 128]), op=ALU.mult),
                     reads=[scb, cB], writes=[ATb[b]])
                for c2 in range(2):
                    p_, pb_ = ps_alloc()
                    rs = slice(c2 * 64, (c2 + 1) * 64)
                    for h in range(NH):
                        MM(pb_, p_[:, h * 128:(h + 1) * 128], KTOK[rs, b, h * 128:(h + 1) * 128], V[rs, b, h * 128:(h + 1) * 128],
                           [KTOKb[b], Vb[b]])
                    ci = b * 2 + c2
                    nxt = wslot(ci)
                    for h in range(NH):
                        P.op("vector", OPC("scalar_tensor_tensor", out=S[:, h, :], in0=S[:, h, :], scalar=BC[:, h, ci * 64 + 63:ci * 64 + 64],
                                           in1=p_[:, h * 128:(h + 1) * 128], op0=ALU.mult, op1=ALU.add),
                             reads=[Sb_[h], BCb[h], pb_], writes=[Sb_[h]])
                        P.op("scalar", OPC("copy", out=SBS[:, nxt, h, :], in_=S[:, h, :]), reads=[Sb_[h]], writes=[SBSb[nxt][h]])

            for b in range(NB):
                stage_a(b)
            conv_mm(0)
            conv_mm(1)

            OQ = {}

            for b in range(NB):
                o_ps, o_b = ps_alloc()
                for h in range(NH):
                    MM(o_b, o_ps[:, h * 128:(h + 1) * 128], V[:, b, h * 128:(h + 1) * 128], AT[b][:, h, :], [Vb[b], ATb[b]])
                for c2 in range(2):
                    ci = b * 2 + c2
                    ts_ = slice(b * 128 + c2 * 64, b * 128 + (c2 + 1) * 64)
                    for h in range(NH):
                        MM(o_b, o_ps[:, h * 128 + c2 * 64:h * 128 + (c2 + 1) * 64], SBS[:, rslot(ci), h, :], QT[:, h, ts_],
                           [SBSb[rslot(ci)][h], QTb[h]])
                P.op("scalar", OPC("copy", out=OS[:, b, :], in_=o_ps[:]), reads=[o_b], writes=[OSb[b]])
                oq, oqb = scr_alloc()
                oq16 = as_bf(oq, 512)
                P.op("scalar", OPC("activation", out=oq16, in_=o_ps[:], func=AF.Square), reads=[o_b], writes=[oqb])
                OQ[b] = (oq16, oqb)
            ss, ssb = ps_alloc()
            for b in range(NB):
                oq16, oqb = OQ[b]
                for h in range(NH):
                    MM(ssb, ss[:, b * NH + h:b * NH + h + 1], oq16[:, h * 128:(h + 1) * 128], ONEB[:, 0:1], [oqb, cB])
            smf = SM2[:].rearrange("p a b -> p (a b)")
            NS = NB * NH
            P.op("vector", OPC("tensor_scalar", out=smf[:, 0:NS], in0=ss[:, 0:NS], scalar1=1.0 / 128.0, scalar2=RMS_EPS,
                               op0=ALU.mult, op1=ALU.add), reads=[ssb], writes=SM2b)
            newton_rsqrt("vector", smf[:, NS:2 * NS], smf[:, 0:NS], smf[:, 2 * NS:3 * NS], SM2b)

            def rms_apply(b):
                bs = slice(b * 128, (b + 1) * 128)
                smf = SM2[:].rearrange("p a b -> p (a b)")
                r0 = NB * NH + b * NH
                lt, ltb = scr_alloc()
                ltv = lt.rearrange("p (h t) -> p h t", h=NH)
                P.op("vector", OPC("tensor_copy", out=ltv, in_=smf[:, r0:r0 + NH].rearrange("p (h o) -> p h o", o=1).to_broadcast([128, NH, 128])),
                     reads=SM2b, writes=[ltb])
                rb_ps, rb_b = ps_alloc()
                for h in range(NH):
                    MM(rb_b, rb_ps[:, h * 128:(h + 1) * 128], ltv[:, h, :], IDF[:], [ltb, cB])
                os_ = OS[:, b, :]
                for h in range(NH):
                    P.op("vector", OPC("scalar_tensor_tensor", out=os_[:, h * 128:(h + 1) * 128], in0=os_[:, h * 128:(h + 1) * 128],
                                       scalar=PC[:, h, 2:3], in1=rb_ps[:, h * 128:(h + 1) * 128], op0=ALU.mult, op1=ALU.mult),
                         reads=[OSb[b], rb_b, cB], writes=[OSb[b]])
                P.op("vector", OPC("scalar_tensor_tensor", out=CT[:, 0:NH, bs], in0=THG[:, :, bs], scalar=1.0,
                                   in1=os_.rearrange("p (h t) -> p h t", h=NH), op0=ALU.add, op1=ALU.mult),
                     reads=THGb + [OSb[b]], writes=[CTb[c][b] for c in range(NH)])

            for c in range(NH):
                CBb[c].aliases = list(Vb)
                CQb[c].aliases = list(KTOKb)
            conv_fin(0)
            conv_fin(1)
            for c in (2, 3):
                conv_mm(c)
                conv_fin(c)
                rms_apply(2 * (c - 2))
                rms_apply(2 * (c - 2) + 1)

            stp, stb = ps_alloc()
            for b in range(NB):
                bs = slice(b * 128, (b + 1) * 128)
                for (q, src, srcb) in ((0, CB, CBb), (1, CQ, CQb)):
                    for c in range(NH):
                        MM(stb, stp[:, b * 2 + q:b * 2 + q + 1], src[:, c, bs], ONEB[:, 0:1], [srcb[c], cB])
            cm, cmb = scr_alloc()
            P.op("vector", OPC("tensor_scalar", out=cm[:, 0:2 * NB], in0=stp[:, 0:2 * NB], scalar1=1.0 / 512.0, scalar2=None, op0=ALU.mult),
                 reads=[stb], writes=[cmb])
            cmv = cm[:, 0:2 * NB].rearrange("p (b q) -> p b q", q=2)
            mean = cmv[:, :, 0]
            ex2 = cmv[:, :, 1]
            P.op("vector", OPC("tensor_tensor", out=cm[:, 16:16 + NB], in0=mean, in1=mean, op=ALU.mult), reads=[cmb], writes=[cmb])
            P.op("vector", OPC("tensor_tensor", out=cm[:, 8:8 + NB], in0=ex2, in1=cm[:, 16:16 + NB], op=ALU.subtract), reads=[cmb], writes=[cmb])
            P.op("vector", OPC("tensor_scalar", out=cm[:, 8:8 + NB], in0=cm[:, 8:8 + NB], scalar1=LN_EPS, scalar2=None, op0=ALU.add),
                 reads=[cmb], writes=[cmb])
            newton_rsqrt("vector", cm[:, 12:12 + NB], cm[:, 8:8 + NB], cm[:, 16:16 + NB], [cmb])
            P.op("vector", OPC("scalar_tensor_tensor", out=cm[:, 20:20 + NB], in0=mean, scalar=-1.0, in1=cm[:, 12:12 + NB],
                               op0=ALU.mult, op1=ALU.mult), reads=[cmb], writes=[cmb])
            ra_ps, ra_b = ps_alloc()
            rn_ps, rn_b = ps_alloc()
            for (col0, dst, dstb) in ((12, ra_ps, ra_b), (20, rn_ps, rn_b)):
                lt, ltb = scr_alloc()
                ltv = lt.rearrange("p (b t) -> p b t", b=NB)
                P.op("vector", OPC("tensor_copy", out=ltv, in_=cm[:, col0:col0 + NB].rearrange("p (b o) -> p b o", o=1).to_broadcast([128, NB, 128])),
                     reads=[cmb], writes=[ltb])
                for b in range(NB):
                    MM(dstb, dst[:, b * 128:(b + 1) * 128], ltv[:, b, :], IDF[:], [ltb, cB])
            for c in range(NH):
                P.op("vector", OPC("tensor_tensor", out=CS[:, c, :], in0=CS[:, c, :], in1=ra_ps[:], op=ALU.mult),
                     reads=[CSb[c], ra_b], writes=[CSb[c]])
                P.op("vector", OPC("tensor_tensor", out=CS[:, c, :], in0=CS[:, c, :], in1=rn_ps[:], op=ALU.add),
                     reads=[CSb[c], rn_b], writes=[CSb[c]])
                P.op("scalar", OPC("activation", out=CT[:, NH + c, :], in_=CS[:, c, :], func=AF.Silu, scale=PT[:, c, 4:5], bias=PT[:, c, 5:6]),
                     reads=[CSb[c], cB], writes=[CTb[NH + c][b] for b in range(NB)])

            def pd(half, b):
                wneed(wo[half])
                ps, pb = ps_alloc()
                for k in range(KC):
                    MM(pb, ps[:], CT[:, k, b * 128:(b + 1) * 128], wo[half].ap[:, k, :], [CTb[k][b], wo[half].buf])
                return ps, pb

            def wrel(half):
                wrelease(wo[half])

            resid_ln(xi, pd, ALPHA, slot, LN_EPS, False, 0, wrel)

        assert ntiles % 2 == 0
        wfill()
        load_x(0, 0)
        load_x(1, 1)
        sa, sb_ = 0, 1
        load_gb(0, sa)
        transposes(0)
        for pair in range(ntiles // 2):
            tA, tB = 2 * pair, 2 * pair + 1
            fA, mA, gA = plan[tA]
            fB, mB, gB = plan[tB]
            load_gb(1, sb_)
            g = ffn(0, fA, sa, False, tA); next(g)
            transposes(1)
            next(g, None)
            g = ffn(1, fB, sa, False, tB); next(g)
            transposes(0)
            next(g, None)
            load_gb(2, sa)
            g = mixer(0, mA, sb_, (tA % tiles_per_seq) == 0, 0); next(g)
            transposes(1)
            next(g, None)
            g = mixer(1, mB, sb_, (tB % tiles_per_seq) == 0, 1); next(g)
            transposes(0)
            next(g, None)
            load_gb(0, sb_)
            g = ffn(0, gA, sa, True, tA); next(g)
            transposes(1)
            next(g, None)
            if tA + 2 < ntiles:
                load_x(tA + 2, 0)
            g = ffn(1, gB, sa, True, tB); next(g)
            if tA + 2 < ntiles:
                transposes(0)
            next(g, None)
            if tB + 2 < ntiles:
                load_x(tB + 2, 1)
            sa, sb_ = sb_, sa
        print('sbuf bytes remaining/partition:', nc.sbuf_bytes_remaining)
        P.emit(final_waits=fin)
    return nc


_NC_CACHE = {}


def _get_nc(nseq, T):
    key = (nseq, T)
    if key not in _NC_CACHE:
        _NC_CACHE[key] = build_nc(nseq, T)
    return _NC_CACHE[key]


def kernel(**inputs):
    x = np.asarray(inputs["x"], dtype=np.float32)
    B, T, _ = x.shape
    n = 8
    nseq = B // n
    nc = _get_nc(nseq, T)
    shared = {}
    for k in ("ffn1_w_gate", "ffn1_w_up", "ffn1_w_down", "ffn2_w_gate", "ffn2_w_up", "ffn2_w_down", "w_in", "w_out"):
        a = np.asarray(inputs[k], dtype=np.float32)
        shared[k] = np.ascontiguousarray(a.reshape(a.shape[-2], a.shape[-1]))
    for k in ("ln1_g", "ln1_b", "ln2_g", "ln2_b", "ln3_g", "ln3_b", "hgrn_norm_g", "conv_b", "conv_ln_g", "conv_ln_b"):
        a = np.asarray(inputs[k], dtype=np.float32)
        shared[k] = np.ascontiguousarray(a.reshape(1, a.shape[-1]))
    shared["lb_logits"] = np.ascontiguousarray(np.asarray(inputs["lb_logits"], dtype=np.float32).reshape(2, 512))
    shared["conv_w"] = np.ascontiguousarray(np.asarray(inputs["conv_w"], dtype=np.float32).reshape(CW, 512))
    in_maps = []
    for c in range(n):
        m = dict(shared)
        m["x"] = np.ascontiguousarray(x[c * nseq:(c + 1) * nseq].reshape(nseq * T, D))
        in_maps.append(m)
    res = run_bass_kernel_spmd(nc, in_maps, core_ids=list(range(n)))
    out = np.concatenate([np.asarray(r["out"]).reshape(nseq, T, D) for r in res.results], axis=0)
    return out.astype(np.float32, copy=False)
```

```python
import numpy as np
from contextlib import ExitStack
import concourse.bass as bass
import concourse.mybir as mybir
from concourse.bass_utils import run_bass_kernel_spmd

F32 = mybir.dt.float32
BF16 = mybir.dt.bfloat16
I32 = mybir.dt.int32
AF = mybir.ActivationFunctionType
ALU = mybir.AluOpType

D = 1024
KC = 8
DFF = 2816
FC = 22
DIN = 3072
NH = 4
CW = 31
HALO = CW - 1
ALPHA = 2.0 ** 0.25
LN_EPS = 1e-5
RMS_EPS = 1e-6
NT = 512
NB = NT // 128
RING_ELEMS = 22 * 1024
NSCR = 14
MAGIC = 0x5F3759DF

ENGINES = ("tensor", "vector", "scalar", "gpsimd", "sync")


class Buf:
    __slots__ = ("name", "writer", "readers", "aliases", "dead")

    def __init__(self, name, aliases=()):
        self.name = name
        self.writer = None
        self.readers = []
        self.aliases = list(aliases)
        self.dead = False


class Op:
    __slots__ = ("eng", "fn", "dma", "deps", "signal", "sem", "val", "waits", "pre")

    def __init__(self, eng, fn, dma):
        self.eng = eng
        self.fn = fn
        self.dma = dma
        self.deps = []
        self.signal = False
        self.sem = None
        self.val = 0
        self.waits = []


def OPC(name, *args, **kwargs):
    return lambda e: getattr(e, name)(*args, **kwargs)


class Prog:
    def __init__(self, nc):
        self.nc = nc
        self.ops = []

    def op(self, eng, fn, reads=(), writes=(), dma=None):
        o = Op(eng, fn, dma)
        deps = {}
        for b in reads:
            assert not b.dead, "read of dead buffer " + b.name
            for bb in [b] + b.aliases:
                if bb.writer is not None:
                    deps[id(bb.writer)] = bb.writer
        for b in writes:
            assert not b.dead, "write of dead buffer " + b.name
            for bb in [b] + b.aliases:
                if bb.writer is not None:
                    deps[id(bb.writer)] = bb.writer
                last = {}
                for r in bb.readers:
                    if r.dma is not None:
                        deps[id(r)] = r
                    else:
                        last[r.eng] = r
                for r in last.values():
                    deps[id(r)] = r
        for d in deps.values():
            if d is o:
                continue
            if d.eng == "tensor" and eng == "tensor" and d.dma is None and dma is None:
                continue
            o.deps.append(d)
            d.signal = True
        for b in reads:
            b.readers.append(o)
        for b in writes:
            b.readers = []
            for bb in b.aliases:
                bb.readers = []
                bb.writer = None
            b.aliases = []
            b.writer = o
        self.ops.append(o)
        return o

    def emit(self, final_waits=()):
        nc = self.nc
        with ExitStack() as es:
            sems = {}

            def get_sem(key):
                if key not in sems:
                    sems[key] = es.enter_context(nc.semaphore("s_" + key))
                return sems[key]

            counts = {}
            KDMA = 8
            for o in final_waits:
                o.signal = True
            for o in self.ops:
                o.pre = None
                if o.dma:
                    n = counts.get("d_" + o.dma, 0)
                    counts["d_" + o.dma] = n + 1
                    o.signal = True
                    o.sem = get_sem("d_%s_%d" % (o.dma, n % KDMA))
                    o.val = 16 * (n // KDMA + 1)
                    if n >= KDMA:
                        o.pre = (o.sem, 16 * (n // KDMA))
                    continue
                if not o.signal:
                    continue
                key = "e_" + o.eng
                counts[key] = counts.get(key, 0) + 1
                o.sem = get_sem(key)
                o.val = counts[key]
            self.counts = counts
            known = {e: {} for e in ENGINES}
            per_eng = {e: [] for e in ENGINES}
            for o in self.ops:
                need = {}
                if o.pre is not None:
                    need[id(o.pre[0])] = o.pre
                for d in o.deps:
                    k = id(d.sem)
                    if k not in need or need[k][1] < d.val:
                        need[k] = (d.sem, d.val)
                kn = known[o.eng]
                for k, (s, v) in need.items():
                    if kn.get(k, 0) >= v:
                        continue
                    kn[k] = v
                    o.waits.append((s, v))
                per_eng[o.eng].append(o)
            with nc.Block() as block:
                def mk(engname):
                    def body(eng):
                        for o in per_eng[engname]:
                            for (s, v) in o.waits:
                                eng.wait_ge(s, v)
                            ins = o.fn(eng)
                            if o.signal:
                                ins.then_inc(o.sem, 16 if o.dma else 1)
                        if engname == "sync":
                            for o in final_waits:
                                eng.wait_ge(o.sem, o.val)
                    return body
                for e in ENGINES:
                    if per_eng[e] or e == "sync":
                        getattr(block, e)(mk(e))


def build_nc(nseq, T):
    ntok = nseq * T
    tiles_per_seq = T // NT
    ntiles = nseq * tiles_per_seq
    nc = bass.Bass("TRN2", target_bir_lowering=False)

    def din(name, shape):
        return nc.dram_tensor(name, list(shape), F32, kind="ExternalInput").ap()

    x_d = din("x", [ntok, D])
    wg_d = [din("ffn1_w_gate", [D, DFF]), din("ffn2_w_gate", [D, DFF])]
    wu_d = [din("ffn1_w_up", [D, DFF]), din("ffn2_w_up", [D, DFF])]
    wd_d = [din("ffn1_w_down", [DFF, D]), din("ffn2_w_down", [DFF, D])]
    win_d = din("w_in", [D, DIN])
    wout_d = din("w_out", [D, D])
    lng_d = [din("ln1_g", [1, D]), din("ln2_g", [1, D]), din("ln3_g", [1, D])]
    lnb_d = [din("ln1_b", [1, D]), din("ln2_b", [1, D]), din("ln3_b", [1, D])]
    lb_d = din("lb_logits", [2, 512])
    hg_d = din("hgrn_norm_g", [1, 512])
    cw_d = din("conv_w", [CW, 512])
    cb_d = din("conv_b", [1, 512])
    clg_d = din("conv_ln_g", [1, 512])
    clb_d = din("conv_ln_b", [1, 512])
    out_d = nc.dram_tensor("out", [ntok, D], F32, kind="ExternalOutput").ap()

    def dscr(name, shape):
        return nc.dram_tensor(name, list(shape), BF16, kind="Internal").ap()

    wg_s = [dscr("wg1_bf", [D, DFF]), dscr("wg2_bf", [D, DFF])]
    wu_s = [dscr("wu1_bf", [D, DFF]), dscr("wu2_bf", [D, DFF])]
    wd_s = [dscr("wd1_bf", [DFF, D]), dscr("wd2_bf", [DFF, D])]
    win_s = dscr("win_bf", [D, DIN])
    wout_s = dscr("wout_bf", [D, D])

    P = Prog(nc)
    fin = []
    with ExitStack() as es:
        def sb(name, shape, dt):
            return es.enter_context(nc.sbuf_tensor(name, list(shape), dt))

        X = [sb("X%d" % i, [128, NB, D], F32) for i in range(2)]
        Xb = [[Buf("X%d_%d" % (i, b)) for b in range(NB)] for i in range(2)]
        XT = sb("XT", [128, KC, NT], BF16)
        XTb = [Buf("XT%d" % b) for b in range(NB)]
        HT = sb("HT", [128, FC, NT], BF16)
        HTb = [Buf("HT%d" % f) for f in range(FC)]
        HTf = HT[:].rearrange("p f t -> p (f t)")
        BC = HTf[:, 0:4096].bitcast(F32).rearrange("p (h t) -> p h t", h=NH)
        CS = HTf[:, 4096:8192].bitcast(F32).rearrange("p (h t) -> p h t", h=NH)
        THG = HTf[:, 8192:10240].rearrange("p (h t) -> p h t", h=NH)
        BCb = [Buf("BC%d" % h, aliases=HTb) for h in range(NH)]
        CSb = [Buf("CS%d" % h, aliases=HTb) for h in range(NH)]
        THGb = [Buf("THG%d" % h, aliases=HTb) for h in range(NH)]
        mix_alias = BCb + CSb + THGb

        KT = sb("KT", [128, NH, NT], BF16)
        KTb = [Buf("KT%d" % h) for h in range(NH)]
        QT = sb("QT", [128, NH, NT], BF16)
        QTb = [Buf("QT%d" % h) for h in range(NH)]
        V = sb("V", [128, NB, 512], BF16)
        Vb = [Buf("V%d" % b) for b in range(NB)]
        KTOK = sb("KTOK", [128, NB, 512], BF16)
        KTOKb = [Buf("KTOK%d" % b) for b in range(NB)]
        AT = [sb("AT%d" % i, [128, NH, 128], BF16) for i in range(NB)]
        ATb = [Buf("AT%d" % i) for i in range(NB)]
        S = sb("S", [128, NH, 128], F32)
        Sb_ = [Buf("S%d" % h) for h in range(NH)]
        SBS = sb("SBS", [128, NT // 64 + 1, NH, 128], BF16)
        SBSb = [[Buf("SBS%d_%d" % (i, h)) for h in range(NH)] for i in range(NT // 64 + 1)]
        UT = sb("UT", [128, NH, HALO + NT], BF16)
        UTb = [Buf("UT%d" % c) for c in range(NH)]
        CB = V
        CBb = [Buf("CB%d" % c) for c in range(NH)]
        CQ = KTOK
        CQb = [Buf("CQ%d" % c) for c in range(NH)]
        OS = sb("OS", [128, NB, 512], F32)
        OSb = [Buf("OS%d" % b) for b in range(NB)]
        CT = sb("CT", [128, KC, NT], BF16)
        CTb = [[Buf("CT%d_%d" % (c, b)) for b in range(NB)] for c in range(KC)]
        GB = sb("GB", [128, 2, 2, D], F32)
        GBb = [Buf("GB%d" % i) for i in range(2)]
        SCR = sb("SCR", [128, NSCR, 512], F32)
        SCRb = [Buf("SCR%d" % i) for i in range(NSCR)]
        RING = sb("RING", [128, RING_ELEMS], BF16)
        IDF = sb("IDF", [128, 128], F32)
        IDB = sb("IDB", [128, 128], BF16)
        ONEB = sb("ONEB", [128, 128], BF16)
        ONEF = sb("ONEF", [128, 64], F32)
        NEGC = sb("NEGC", [128, 2], F32)
        MASK = sb("MASK", [128, 128], F32)
        PR = sb("PR", [37, 512], F32)
        PT = sb("PT", [128, NH, 37], F32)
        PC = sb("PC", [128, NH, 8], F32)
        W05 = sb("W05", [128, NH, CW], F32)
        STT = sb("STT", [128, 2, NB, 2, 6], F32)
        SM = sb("SM", [128, 2, NB, 8], F32)
        SM2 = sb("SM2", [128, 4, 16], F32)
        cB = Buf("consts")
        STTb = [[Buf("STT%d_%d" % (i, b)) for b in range(NB)] for i in range(2)]
        SMb = [[Buf("SM%d_%d" % (i, b)) for b in range(NB)] for i in range(2)]
        SM2b = [Buf("SM2_%d" % i) for i in range(4)]

        psum = [es.enter_context(nc.psum_tensor("PS%d" % i, [128, 512], F32)) for i in range(8)]
        psb = [Buf("PS%d" % i) for i in range(8)]
        st = {"ps": 0, "scr": 0, "sm2": 0, "at": 0}

        ps_first = {}

        def ps_alloc():
            i = st["ps"] % 8
            st["ps"] += 1
            ps_first[id(psb[i])] = True
            return psum[i], psb[i]

        def MM(pb, out, lhsT, rhs, reads):
            first = ps_first[id(pb)]
            ps_first[id(pb)] = False
            P.op("tensor", OPC("matmul", out, lhsT=lhsT, rhs=rhs, start=first, stop=True, skip_group_check=True),
                 reads=reads, writes=[pb])

        class Scr:
            pass

        def scr_alloc():
            i = st["scr"] % NSCR
            st["scr"] += 1
            old = SCRb[i]
            nb = Buf("SCR%d_%d" % (i, st["scr"]), aliases=[old] + old.aliases)
            old.dead = True
            SCRb[i] = nb
            return SCR[:, i, :], nb

        def as_bf(ap, n):
            return ap.bitcast(BF16)[:, 0:n]

        ring = {"head": 0, "live": []}

        def ring_alloc(n):
            start = ring["head"]
            if start + n > RING_ELEMS:
                start = 0
            end = start + n
            aliases = []
            keep = []
            for (s0, e0, b0, rel) in ring["live"]:
                if s0 < end and start < e0:
                    if not rel[0]:
                        return None
                    aliases.append(b0)
                else:
                    keep.append((s0, e0, b0, rel))
            for b0 in aliases:
                b0.dead = True
            al = []
            for b0 in aliases:
                al.append(b0)
                al.extend(b0.aliases)
            nb = Buf("W@%d" % start, aliases=al)
            rel = [False]
            keep.append((start, end, nb, rel))
            ring["live"] = keep
            ring["head"] = end
            return start, nb, rel

        scr_bufs = {}

        class WTile:
            def __init__(self, src_ap, shape, scr_ap=None, key=None):
                self.src = src_ap
                self.scr = scr_ap
                self.key = key
                self.shape = shape
                self.ap = None
                self.buf = None
                self.rel = None

        wstream = []
        wcur = {"i": 0}

        def wfill():
            while wcur["i"] < len(wstream):
                wt = wstream[wcur["i"]]
                a, b = wt.shape
                r = ring_alloc(a * b)
                if r is None:
                    return
                start, nb, rel = r
                wt.ap = RING[:, start:start + a * b].rearrange("p (a b) -> p a b", a=a)
                wt.buf = nb
                wt.rel = rel
                if wt.key not in scr_bufs:
                    P.op("gpsimd", OPC("dma_start", out=wt.ap, in_=wt.src), writes=[nb], dma="w")
                    sbuf_ = Buf("scr_%s" % (wt.key,))
                    scr_bufs[wt.key] = sbuf_
                    P.op("sync", OPC("dma_start", out=wt.scr, in_=wt.ap), reads=[nb], writes=[sbuf_], dma="wb")
                else:
                    P.op("sync", OPC("dma_start", out=wt.ap, in_=wt.scr), reads=[scr_bufs[wt.key]], writes=[nb], dma="w2")
                wcur["i"] += 1

        def wneed(wt):
            if wt.buf is None:
                wfill()
            assert wt.buf is not None, "ring too small / stream order"
            return wt

        def wrelease(wt):
            wt.rel[0] = True
            wfill()

        def ffn_tiles(l):
            gu = []
            for fg in range(FC // 2):
                cs = slice(fg * 256, (fg + 1) * 256)
                g = WTile(wg_d[l].rearrange("(k p) f -> p k f", p=128)[:, :, cs], (KC, 256),
                          wg_s[l].rearrange("(k p) f -> p k f", p=128)[:, :, cs], ("g", l, fg))
                u = WTile(wu_d[l].rearrange("(k p) f -> p k f", p=128)[:, :, cs], (KC, 256),
                          wu_s[l].rearrange("(k p) f -> p k f", p=128)[:, :, cs], ("u", l, fg))
                gu.append((g, u))
            dn = []
            for half in range(2):
                parts = []
                for q in range(2):
                    src = wd_d[l].rearrange("(f p) d -> p f d", p=128)[:, q * 11:(q + 1) * 11, half * 512:(half + 1) * 512]
                    scr = wd_s[l].rearrange("(f p) d -> p f d", p=128)[:, q * 11:(q + 1) * 11, half * 512:(half + 1) * 512]
                    parts.append(WTile(src, (11, 512), scr, ("d", l, half, q)))
                dn.append(parts)
            return gu, dn

        def mix_tiles():
            wi = []
            for g in range(6):
                wi.append(WTile(win_d.rearrange("(k p) c -> p k c", p=128)[:, :, g * 512:(g + 1) * 512], (KC, 512),
                                win_s.rearrange("(k p) c -> p k c", p=128)[:, :, g * 512:(g + 1) * 512], ("i", g)))
            wo = []
            for half in range(2):
                wo.append(WTile(wout_d.rearrange("(k p) c -> p k c", p=128)[:, :, half * 512:(half + 1) * 512], (KC, 512),
                                wout_s.rearrange("(k p) c -> p k c", p=128)[:, :, half * 512:(half + 1) * 512], ("o", half)))
            return wi, wo

        plan = []
        for t in range(ntiles):
            plan.append((ffn_tiles(0), mix_tiles(), ffn_tiles(1)))

        def push_ffn(ft):
            gu, dn = ft
            for g, u in gu:
                wstream.extend([g, u])
            for parts in dn:
                wstream.extend(parts)

        def push_mix(mt):
            wi, wo = mt
            wstream.extend([wi[2], wi[1], wi[3], wi[5], wi[4], wi[0]])
            wstream.extend(wo)

        for pair in range(ntiles // 2):
            a, b = plan[2 * pair], plan[2 * pair + 1]
            push_ffn(a[0]); push_ffn(b[0])
            push_mix(a[1]); push_mix(b[1])
            push_ffn(a[2]); push_ffn(b[2])

        P.op("gpsimd", OPC("memset", IDF[:], 1.0), writes=[cB])
        P.op("gpsimd", OPC("affine_select", out=IDF[:], in_=IDF[:], pattern=[[-1, 128]],
                                                 compare_op=ALU.is_equal, fill=0.0, base=0,
                                                 channel_multiplier=1), reads=[cB], writes=[cB])
        P.op("gpsimd", OPC("memset", MASK[:], 1.0), writes=[cB])
        P.op("gpsimd", OPC("affine_select", out=MASK[:], in_=MASK[:], pattern=[[1, 128]],
                                                 compare_op=ALU.is_ge, fill=0.0, base=0,
                                                 channel_multiplier=-1), reads=[cB], writes=[cB])
        P.op("gpsimd", OPC("memset", MASK[0:64, 64:128], 0.0), reads=[cB], writes=[cB])
        P.op("gpsimd", OPC("memset", ONEB[:], 1.0), writes=[cB])
        P.op("gpsimd", OPC("memset", ONEF[:], 1.0), writes=[cB])
        P.op("gpsimd", OPC("memset", NEGC[:, 0:1], -1.0), writes=[cB])
        P.op("gpsimd", OPC("memset", NEGC[:, 1:2], -0.5), writes=[cB])
        P.op("vector", OPC("tensor_copy", out=IDB[:], in_=IDF[:]), reads=[cB], writes=[cB])
        rows = [(lb_d, 0, 2), (hg_d, 2, 1), (cb_d, 3, 1), (clg_d, 4, 1), (clb_d, 5, 1), (cw_d, 6, CW)]
        for (src, r0, n) in rows:
            P.op("sync", OPC("dma_start", out=PR[r0:r0 + n, :], in_=src),
                 writes=[cB], dma="c")
        pt_ps, pt_b = ps_alloc()
        for c in range(NH):
            MM(pt_b, pt_ps[:, c * 37:(c + 1) * 37], PR[0:37, c * 128:(c + 1) * 128], IDF[0:37, 0:37], [cB])
        P.op("vector", OPC("tensor_copy", out=PT[:].rearrange("p h r -> p (h r)"), in_=pt_ps[:, 0:NH * 37]),
             reads=[pt_b], writes=[cB])
        P.op("vector", OPC("tensor_tensor", out=PC[:, :, 3], in0=PT[:, :, 0], in1=PT[:, :, 1], op=ALU.subtract),
             reads=[cB], writes=[cB])
        P.op("scalar", OPC("activation", out=PC[:, :, 4], in_=PC[:, :, 3], func=AF.Tanh, scale=0.5),
             reads=[cB], writes=[cB])
        P.op("vector", OPC("tensor_scalar", out=PC[:, :, 0], in0=PC[:, :, 4], scalar1=0.25, scalar2=0.75,
                                                 op0=ALU.mult, op1=ALU.add), reads=[cB], writes=[cB])
        P.op("vector", OPC("tensor_scalar", out=PC[:, :, 1], in0=PC[:, :, 4], scalar1=-0.25, scalar2=0.25,
                                                 op0=ALU.mult, op1=ALU.add), reads=[cB], writes=[cB])
        P.op("vector", OPC("tensor_scalar", out=PC[:, :, 2], in0=PT[:, :, 2], scalar1=0.5, scalar2=None,
                                                 op0=ALU.mult), reads=[cB], writes=[cB])
        P.op("vector", OPC("tensor_scalar", out=W05[:], in0=PT[:, :, 6:6 + CW], scalar1=0.5, scalar2=None,
                                                 op0=ALU.mult), reads=[cB], writes=[cB])

        def newton_rsqrt(eng, y, v, tmp, bufs, n_iter=2):
            shp = list(v.shape)
            P.op("gpsimd", OPC("tensor_tensor", out=y, in0=v, in1=NEGC[:, 1:2].to_broadcast(shp), op=ALU.pow),
                 reads=bufs + [cB], writes=bufs)

        def load_x(tile, xi):
            t0 = tile * NT
            for b in range(NB):
                P.op("sync", OPC("dma_start", out=X[xi][:, b, :], in_=x_d[t0 + b * 128:t0 + (b + 1) * 128, :]),
                     writes=[Xb[xi][b]], dma="x")

        def load_gb(li, slot):
            P.op("sync", OPC("dma_start", out=GB[:, slot, 0, :], in_=lng_d[li].partition_broadcast(128)),
                 writes=[GBb[slot]], dma="g")
            P.op("sync", OPC("dma_start", out=GB[:, slot, 1, :], in_=lnb_d[li].partition_broadcast(128)),
                 writes=[GBb[slot]], dma="g")

        def transposes(xi):
            for b in range(NB):
                xb_ap, xb_b = scr_alloc()
                xb16 = as_bf(xb_ap, D)
                P.op("scalar", OPC("copy", out=xb16, in_=X[xi][:, b, :]),
                     reads=[Xb[xi][b]], writes=[xb_b])
                ps, pb = ps_alloc()
                psT = ps[:].bitcast(BF16)
                for k in range(KC):
                    P.op("tensor", OPC("transpose", psT[:, k * 128:(k + 1) * 128],
                                                                                 xb16[:, k * 128:(k + 1) * 128], IDB[:]),
                         reads=[xb_b, cB], writes=[pb])
                P.op("vector", OPC("tensor_copy", out=XT[:, :, b * 128:(b + 1) * 128], in_=psT.rearrange("p (k t) -> p k t", k=KC)),
                     reads=[pb], writes=[XTb[b]])

        def ln_finish(xi, b, slot, eps, last, tile):
            sm = SM[:, xi, b, :]
            bufs = [SMb[xi][b]]
            P.op("vector", OPC("bn_aggr", out=sm[:, 0:2], in_=STT[:, xi, b, :, :].rearrange("p g s -> p (g s)")),
                 reads=[STTb[xi][b]], writes=bufs)
            P.op("vector", OPC("tensor_scalar", out=sm[:, 2:3], in0=sm[:, 1:2], scalar1=eps, scalar2=None,
                                                     op0=ALU.add), reads=bufs, writes=bufs)
            newton_rsqrt("vector", sm[:, 3:4], sm[:, 2:3], sm[:, 4:5], bufs)
            P.op("vector", OPC("scalar_tensor_tensor", out=sm[:, 5:6], in0=sm[:, 0:1], scalar=-1.0, in1=sm[:, 3:4],
                                                            op0=ALU.mult, op1=ALU.mult), reads=bufs, writes=bufs)
            xblk = X[xi][:, b, :]
            P.op("scalar", OPC("activation", out=xblk, in_=xblk, func=AF.Identity, scale=sm[:, 3:4], bias=sm[:, 5:6]),
                 reads=[Xb[xi][b]] + bufs, writes=[Xb[xi][b]])
            P.op("gpsimd", OPC("tensor_tensor", out=xblk, in0=xblk, in1=GB[:, slot, 0, :], op=ALU.mult),
                 reads=[Xb[xi][b], GBb[slot]], writes=[Xb[xi][b]])
            P.op("gpsimd", OPC("tensor_tensor", out=xblk, in0=xblk, in1=GB[:, slot, 1, :], op=ALU.add),
                 reads=[Xb[xi][b], GBb[slot]], writes=[Xb[xi][b]])
            if last:
                t0 = tile * NT
                fin.append(P.op("sync", OPC("dma_start", out=out_d[t0 + b * 128:t0 + (b + 1) * 128, :], in_=xblk),
                                reads=[Xb[xi][b]], dma="o"))

        def resid_ln(xi, pdict, res_scale, slot, eps, last, tile, wrel):
            for half in range(2):
                for b in range(NB):
                    ps, pb = pdict(half, b)
                    if b == NB - 1:
                        wrel(half)
                    xs = X[xi][:, b, half * 512:(half + 1) * 512]
                    P.op("vector", OPC("scalar_tensor_tensor", out=xs, in0=xs, scalar=res_scale, in1=ps[:], op0=ALU.mult, op1=ALU.add),
                         reads=[Xb[xi][b], pb], writes=[Xb[xi][b]])
                    P.op("vector", OPC("bn_stats", out=STT[:, xi, b, half, :], in_=xs), reads=[Xb[xi][b]], writes=[STTb[xi][b]])
                    if half == 1:
                        ln_finish(xi, b, slot, eps, last, tile)

        def ffn(xi, tiles, slot, last, tile):
            gu, dn = tiles
            for f in range(FC):
                HTb[f].aliases = [m for m in mix_alias]
            first = [True]
            for fg in range(FC // 2):
                g, u = gu[fg]
                wneed(g)
                wneed(u)
                for fi in range(2):
                    f = fg * 2 + fi
                    pg, pgb = ps_alloc()
                    pu, pub = ps_alloc()
                    for (wt, ps, pb) in ((g, pg, pgb), (u, pu, pub)):
                        for k in range(KC):
                            MM(pb, ps[:], wt.ap[:, k, fi * 128:(fi + 1) * 128], XT[:, k, :], [wt.buf] + XTb)
                    sg, sgb = scr_alloc()
                    P.op("scalar", OPC("activation", out=sg, in_=pg[:], func=AF.Silu),
                         reads=[pgb], writes=[sgb])
                    P.op("vector", OPC("tensor_tensor", out=HT[:, f, :], in0=sg, in1=pu[:], op=ALU.mult),
                         reads=[sgb, pub], writes=[HTb[f]])
                    if first[0]:
                        for m in mix_alias:
                            m.writer = None
                            m.readers = []
                        first[0] = False
                    HTb[f].aliases = []
                wrelease(g)
                wrelease(u)
            yield

            def pd(half, b):
                for part in dn[half]:
                    wneed(part)
                ps, pb = ps_alloc()
                for f in range(FC):
                    part = dn[half][f // 11]
                    MM(pb, ps[:], HT[:, f, b * 128:(b + 1) * 128], part.ap[:, f % 11, :], [HTb[f], part.buf])
                return ps, pb

            def wrel(half):
                for part in dn[half]:
                    wrelease(part)

            resid_ln(xi, pd, 2.0 * ALPHA, slot, 4.0 * LN_EPS, last, tile, wrel)

        def mixer(xi, tiles, slot, seq_start, par):
            wi, wo = tiles
            for m in mix_alias:
                m.aliases = [h for h in HTb]
            wf, wq, wv, wgo, wcv, wcg = wi[1], wi[0], wi[2], wi[3], wi[4], wi[5]
            NCH = NT // 64

            def rslot(ci):
                return ci if ci > 0 else (0 if par == 0 else NCH)

            def wslot(ci):
                return ci + 1 if ci < NCH - 1 else (NCH if par == 0 else 0)

            def proj_fm(wt, c):
                ps, pb = ps_alloc()
                for k in range(KC):
                    MM(pb, ps[:], wt.ap[:, k, c * 128:(c + 1) * 128], XT[:, k, :], [wt.buf] + XTb)
                return ps, pb

            if seq_start:
                P.op("gpsimd", OPC("memset", S[:], 0.0), writes=Sb_)
                P.op("gpsimd", OPC("memset", SBS[:, rslot(0), :, :], 0.0), writes=SBSb[rslot(0)])
                for c in range(NH):
                    P.op("gpsimd", OPC("memset", UT[:, c, 0:HALO], 0.0), writes=[UTb[c]])

            for b in range(NB):
                Vb[b].aliases = list(CBb)
                KTOKb[b].aliases = list(CQb)
            wneed(wv)
            for b in range(NB):
                ps, pb = ps_alloc()
                for k in range(KC):
                    MM(pb, ps[:], XT[:, k, b * 128:(b + 1) * 128], wv.ap[:, k, :], [wv.buf, XTb[b]])
                P.op("scalar", OPC("copy", out=V[:, b, :], in_=ps[:]), reads=[pb], writes=[Vb[b]])
            wrelease(wv)
            wneed(wf)
            KH = []
            for h in range(NH):
                ps, pb = proj_fm(wf, h)
                fa, fb = scr_alloc()
                P.op("scalar", OPC("activation", out=fa, in_=ps[:], func=AF.Tanh, scale=0.5), reads=[pb], writes=[fb])
                P.op("vector", OPC("tensor_scalar", out=fa, in0=fa, scalar1=PC[:, h, 1:2], scalar2=PC[:, h, 0:1],
                                   op0=ALU.mult, op1=ALU.add), reads=[fb, cB], writes=[fb])
                for c in range(NCH):
                    cs = slice(c * 64, (c + 1) * 64)
                    P.op("vector", OPC("tensor_tensor_scan", out=BC[:, h, cs], data0=fa[:, cs], data1=ONEF[:, 0:64], initial=1.0,
                                       op0=ALU.mult, op1=ALU.mult), reads=[fb, cB], writes=[BCb[h]])
                rb, rbb = scr_alloc()
                P.op("vector", OPC("reciprocal", out=rb, in_=BC[:, h, :]), reads=[BCb[h]], writes=[rbb])
                P.op("vector", OPC("tensor_scalar", out=fa, in0=fa, scalar1=-1.0, scalar2=1.0, op0=ALU.mult, op1=ALU.add),
                     reads=[fb], writes=[fb])
                P.op("vector", OPC("tensor_tensor", out=KT[:, h, :], in0=fa, in1=rb, op=ALU.mult), reads=[fb, rbb], writes=[KTb[h]])
                P.op("gpsimd", OPC("tensor_tensor", out=rb.rearrange("p (c t) -> p c t", t=64), in0=rb.rearrange("p (c t) -> p c t", t=64),
                                   in1=BC[:, h, :].rearrange("p (c t) -> p c t", t=64)[:, :, 63:64].to_broadcast([128, NCH, 64]),
                                   op=ALU.mult), reads=[rbb, BCb[h]], writes=[rbb])
                kh, khb = scr_alloc()
                kh16 = as_bf(kh, NT)
                P.op("gpsimd", OPC("tensor_tensor", out=kh16, in0=fa, in1=rb, op=ALU.mult), reads=[fb, rbb], writes=[khb])
                KH.append((kh16, khb))
            wrelease(wf)
            wneed(wgo)
            for h in range(NH):
                ps, pb = proj_fm(wgo, h)
                P.op("scalar", OPC("activation", out=THG[:, h, :], in_=ps[:], func=AF.Tanh, scale=0.5), reads=[pb], writes=[THGb[h]])
            wrelease(wgo)
            THC = []
            wneed(wcg)
            for c in range(NH):
                ps, pb = proj_fm(wcg, c)
                ta, tb = scr_alloc()
                P.op("scalar", OPC("activation", out=ta, in_=ps[:], func=AF.Tanh, scale=0.5), reads=[pb], writes=[tb])
                THC.append((ta, tb))
            wrelease(wcg)
            wneed(wcv)
            for c in range(NH):
                ps, pb = proj_fm(wcv, c)
                ta, tb = THC[c]
                P.op("vector", OPC("scalar_tensor_tensor", out=UT[:, c, HALO:HALO + NT], in0=ta, scalar=1.0, in1=ps[:],
                                   op0=ALU.add, op1=ALU.mult), reads=[tb, pb], writes=[UTb[c]])
            wrelease(wcv)
            wneed(wq)
            for h in range(NH):
                ps, pb = proj_fm(wq, h)
                P.op("vector", OPC("tensor_tensor", out=QT[:, h, :], in0=ps[:], in1=BC[:, h, :], op=ALU.mult),
                     reads=[pb, BCb[h]], writes=[QTb[h]])
            wrelease(wq)
            for b in range(NB):
                ps, pb = ps_alloc()
                psT = ps[:].bitcast(BF16)
                for h in range(NH):
                    kh16, khb = KH[h]
                    P.op("tensor", OPC("transpose", psT[:, h * 128:(h + 1) * 128], kh16[:, b * 128:(b + 1) * 128], IDB[:]),
                         reads=[khb, cB], writes=[pb])
                P.op("scalar", OPC("copy", out=KTOK[:, b, :], in_=psT[:, 0:512]), reads=[pb], writes=[KTOKb[b]])
            yield

            def conv_mm(c):
                cps, cpb = ps_alloc()
                dg16 = dgb = None
                for j in range(CW):
                    if j % 8 == 0:
                        dg, dgb = scr_alloc()
                        dg16 = as_bf(dg, 1024)
                        n = min(8, CW - j)
                        P.op("gpsimd", OPC("tensor_tensor", out=dg16[:, 0:n * 128].rearrange("p (j t) -> p j t", j=n),
                                           in0=IDF[:].rearrange("p (o t) -> p o t", o=1).to_broadcast([128, n, 128]),
                                           in1=W05[:, c, j:j + n].rearrange("p (j o) -> p j o", o=1).to_broadcast([128, n, 128]),
                                           op=ALU.mult), reads=[cB], writes=[dgb])
                    jj = j % 8
                    MM(cpb, cps[:], dg16[:, jj * 128:(jj + 1) * 128], UT[:, c, j:j + NT], [dgb, UTb[c]])
                P.op("scalar", OPC("activation", out=CS[:, c, :], in_=cps[:], func=AF.Identity, bias=PT[:, c, 3:4]),
                     reads=[cpb, cB], writes=[CSb[c]])

            def conv_fin(c):
                P.op("scalar", OPC("copy", out=CB[:, c, :], in_=CS[:, c, :]), reads=[CSb[c]], writes=[CBb[c]])
                P.op("scalar", OPC("activation", out=CQ[:, c, :], in_=CS[:, c, :], func=AF.Square), reads=[CSb[c]], writes=[CQb[c]])
                P.op("gpsimd", OPC("tensor_copy", out=UT[:, c, 0:HALO], in_=UT[:, c, NT:NT + HALO]), reads=[UTb[c]], writes=[UTb[c]])

            def stage_a(b):
                bs = slice(b * 128, (b + 1) * 128)
                sc, scb = ps_alloc()
                for h in range(NH):
                    MM(scb, sc[:, h * 128:(h + 1) * 128], KT[:, h, bs], QT[:, h, bs], [KTb[h], QTb[h]])
                P.op("vector", OPC("tensor_tensor", out=AT[b][:], in0=sc[:].rearrange("p (h t) -> p h t", h=NH),
                                   in1=MASK[:].rearrange("p (o t) -> p o t", o=1).to_broadcast([128, NH, 128]), op=ALU.mult),
                     reads=[scb, cB], writes=[ATb[b]])
                for c2 in range(2):
                    p_, pb_ = ps_alloc()
                    rs = slice(c2 * 64, (c2 + 1) * 64)
                    for h in range(NH):
                        MM(pb_, p_[:, h * 128:(h + 1) * 128], KTOK[rs, b, h * 128:(h + 1) * 128], V[rs, b, h * 128:(h + 1) * 128],
                           [KTOKb[b], Vb[b]])
                    ci = b * 2 + c2
                    nxt = wslot(ci)
                    for h in range(NH):
                        P.op("vector", OPC("scalar_tensor_tensor", out=S[:, h, :], in0=S[:, h, :], scalar=BC[:, h, ci * 64 + 63:ci * 64 + 64],
                                           in1=p_[:, h * 128:(h + 1) * 128], op0=ALU.mult, op1=ALU.add),
                             reads=[Sb_[h], BCb[h], pb_], writes=[Sb_[h]])
                        P.op("scalar", OPC("copy", out=SBS[:, nxt, h, :], in_=S[:, h, :]), reads=[Sb_[h]], writes=[SBSb[nxt][h]])

            for b in range(NB):
                stage_a(b)
            conv_mm(0)
            conv_mm(1)

            OQ = {}

            for b in range(NB):
                o_ps, o_b = ps_alloc()
                for h in range(NH):
                    MM(o_b, o_ps[:, h * 128:(h + 1) * 128], V[:, b, h * 128:(h + 1) * 128], AT[b][:, h, :], [Vb[b], ATb[b]])
                for c2 in range(2):
                    ci = b * 2 + c2
                    ts_ = slice(b * 128 + c2 * 64, b * 128 + (c2 + 1) * 64)
                    for h in range(NH):
                        MM(o_b, o_ps[:, h * 128 + c2 * 64:h * 128 + (c2 + 1) * 64], SBS[:, rslot(ci), h, :], QT[:, h, ts_],
                           [SBSb[rslot(ci)][h], QTb[h]])
                P.op("scalar", OPC("copy", out=OS[:, b, :], in_=o_ps[:]), reads=[o_b], writes=[OSb[b]])
                oq, oqb = scr_alloc()
                oq16 = as_bf(oq, 512)
                P.op("scalar", OPC("activation", out=oq16, in_=o_ps[:], func=AF.Square), reads=[o_b], writes=[oqb])
                OQ[b] = (oq16, oqb)
            ss, ssb = ps_alloc()
            for b in range(NB):
                oq16, oqb = OQ[b]
                for h in range(NH):
                    MM(ssb, ss[:, b * NH + h:b * NH + h + 1], oq16[:, h * 128:(h + 1) * 128], ONEB[:, 0:1], [oqb, cB])
            smf = SM2[:].rearrange("p a b -> p (a b)")
            NS = NB * NH
            P.op("vector", OPC("tensor_scalar", out=smf[:, 0:NS], in0=ss[:, 0:NS], scalar1=1.0 / 128.0, scalar2=RMS_EPS,
                               op0=ALU.mult, op1=ALU.add), reads=[ssb], writes=SM2b)
            newton_rsqrt("vector", smf[:, NS:2 * NS], smf[:, 0:NS], smf[:, 2 * NS:3 * NS], SM2b)

            def rms_apply(b):
                bs = slice(b * 128, (b + 1) * 128)
                smf = SM2[:].rearrange("p a b -> p (a b)")
                r0 = NB * NH + b * NH
                lt, ltb = scr_alloc()
                ltv = lt.rearrange("p (h t) -> p h t", h=NH)
                P.op("vector", OPC("tensor_copy", out=ltv, in_=smf[:, r0:r0 + NH].rearrange("p (h o) -> p h o", o=1).to_broadcast([128, NH, 128])),
                     reads=SM2b, writes=[ltb])
                rb_ps, rb_b = ps_alloc()
                for h in range(NH):
                    MM(rb_b, rb_ps[:, h * 128:(h + 1) * 128], ltv[:, h, :], IDF[:], [ltb, cB])
                os_ = OS[:, b, :]
                for h in range(NH):
                    P.op("vector", OPC("scalar_tensor_tensor", out=os_[:, h * 128:(h + 1) * 128], in0=os_[:, h * 128:(h + 1) * 128],
                                       scalar=PC[:, h, 2:3], in1=rb_ps[:, h * 128:(h + 1) * 128], op0=ALU.mult, op1=ALU.mult),
                         reads=[OSb[b], rb_b, cB], writes=[OSb[b]])
                P.op("vector", OPC("scalar_tensor_tensor", out=CT[:, 0:NH, bs], in0=THG[:, :, bs], scalar=1.0,
                                   in1=os_.rearrange("p (h t) -> p h t", h=NH), op0=ALU.add, op1=ALU.mult),
                     reads=THGb + [OSb[b]], writes=[CTb[c][b] for c in range(NH)])

            for c in range(NH):
                CBb[c].aliases = list(Vb)
                CQb[c].aliases = list(KTOKb)
            conv_fin(0)
            conv_fin(1)
            for c in (2, 3):
                conv_mm(c)
                conv_fin(c)
                rms_apply(2 * (c - 2))
                rms_apply(2 * (c - 2) + 1)

            stp, stb = ps_alloc()
            for b in range(NB):
                bs = slice(b * 128, (b + 1) * 128)
                for (q, src, srcb) in ((0, CB, CBb), (1, CQ, CQb)):
                    for c in range(NH):
                        MM(stb, stp[:, b * 2 + q:b * 2 + q + 1], src[:, c, bs], ONEB[:, 0:1], [srcb[c], cB])
            cm, cmb = scr_alloc()
            P.op("vector", OPC("tensor_scalar", out=cm[:, 0:2 * NB], in0=stp[:, 0:2 * NB], scalar1=1.0 / 512.0, scalar2=None, op0=ALU.mult),
                 reads=[stb], writes=[cmb])
            cmv = cm[:, 0:2 * NB].rearrange("p (b q) -> p b q", q=2)
            mean = cmv[:, :, 0]
            ex2 = cmv[:, :, 1]
            P.op("vector", OPC("tensor_tensor", out=cm[:, 16:16 + NB], in0=mean, in1=mean, op=ALU.mult), reads=[cmb], writes=[cmb])
            P.op("vector", OPC("tensor_tensor", out=cm[:, 8:8 + NB], in0=ex2, in1=cm[:, 16:16 + NB], op=ALU.subtract), reads=[cmb], writes=[cmb])
            P.op("vector", OPC("tensor_scalar", out=cm[:, 8:8 + NB], in0=cm[:, 8:8 + NB], scalar1=LN_EPS, scalar2=None, op0=ALU.add),
                 reads=[cmb], writes=[cmb])
            newton_rsqrt("vector", cm[:, 12:12 + NB], cm[:, 8:8 + NB], cm[:, 16:16 + NB], [cmb])
            P.op("vector", OPC("scalar_tensor_tensor", out=cm[:, 20:20 + NB], in0=mean, scalar=-1.0, in1=cm[:, 12:12 + NB],
                               op0=ALU.mult, op1=ALU.mult), reads=[cmb], writes=[cmb])
            ra_ps, ra_b = ps_alloc()
            rn_ps, rn_b = ps_alloc()
            for (col0, dst, dstb) in ((12, ra_ps, ra_b), (20, rn_ps, rn_b)):
                lt, ltb = scr_alloc()
                ltv = lt.rearrange("p (b t) -> p b t", b=NB)
                P.op("vector", OPC("tensor_copy", out=ltv, in_=cm[:, col0:col0 + NB].rearrange("p (b o) -> p b o", o=1).to_broadcast([128, NB, 128])),
                     reads=[cmb], writes=[ltb])
                for b in range(NB):
                    MM(dstb, dst[:, b * 128:(b + 1) * 128], ltv[:, b, :], IDF[:], [ltb, cB])
            for c in range(NH):
                P.op("vector", OPC("tensor_tensor", out=CS[:, c, :], in0=CS[:, c, :], in1=ra_ps[:], op=ALU.mult),
                     reads=[CSb[c], ra_b], writes=[CSb[c]])
                P.op("vector", OPC("tensor_tensor", out=CS[:, c, :], in0=CS[:, c, :], in1=rn_ps[:], op=ALU.add),
                     reads=[CSb[c], rn_b], writes=[CSb[c]])
                P.op("scalar", OPC("activation", out=CT[:, NH + c, :], in_=CS[:, c, :], func=AF.Silu, scale=PT[:, c, 4:5], bias=PT[:, c, 5:6]),
                     reads=[CSb[c], cB], writes=[CTb[NH + c][b] for b in range(NB)])

            def pd(half, b):
                wneed(wo[half])
                ps, pb = ps_alloc()
                for k in range(KC):
                    MM(pb, ps[:], CT[:, k, b * 128:(b + 1) * 128], wo[half].ap[:, k, :], [CTb[k][b], wo[half].buf])
                return ps, pb

            def wrel(half):
                wrelease(wo[half])

            resid_ln(xi, pd, ALPHA, slot, LN_EPS, False, 0, wrel)

        assert ntiles % 2 == 0
        wfill()
        load_x(0, 0)
        load_x(1, 1)
        sa, sb_ = 0, 1
        load_gb(0, sa)
        transposes(0)
        for pair in range(ntiles // 2):
            tA, tB = 2 * pair, 2 * pair + 1
            fA, mA, gA = plan[tA]
            fB, mB, gB = plan[tB]
            load_gb(1, sb_)
            g = ffn(0, fA, sa, False, tA); next(g)
            transposes(1)
            next(g, None)
            g = ffn(1, fB, sa, False, tB); next(g)
            transposes(0)
            next(g, None)
            load_gb(2, sa)
            g = mixer(0, mA, sb_, (tA % tiles_per_seq) == 0, 0); next(g)
            transposes(1)
            next(g, None)
            g = mixer(1, mB, sb_, (tB % tiles_per_seq) == 0, 1); next(g)
            transposes(0)
            next(g, None)
            load_gb(0, sb_)
            g = ffn(0, gA, sa, True, tA); next(g)
            transposes(1)
            next(g, None)
            if tA + 2 < ntiles:
                load_x(tA + 2, 0)
            g = ffn(1, gB, sa, True, tB); next(g)
            if tA + 2 < ntiles:
                transposes(0)
            next(g, None)
            if tB + 2 < ntiles:
                load_x(tB + 2, 1)
            sa, sb_ = sb_, sa
        print('sbuf bytes remaining/partition:', nc.sbuf_bytes_remaining)
        P.emit(final_waits=fin)
    return nc


_NC_CACHE = {}


def _get_nc(nseq, T):
    key = (nseq, T)
    if key not in _NC_CACHE:
        _NC_CACHE[key] = build_nc(nseq, T)
    return _NC_CACHE[key]


def kernel(**inputs):
    x = np.asarray(inputs["x"], dtype=np.float32)
    B, T, _ = x.shape
    n = 8
    nseq = B // n
    nc = _get_nc(nseq, T)
    shared = {}
    for k in ("ffn1_w_gate", "ffn1_w_up", "ffn1_w_down", "ffn2_w_gate", "ffn2_w_up", "ffn2_w_down", "w_in", "w_out"):
        a = np.asarray(inputs[k], dtype=np.float32)
        shared[k] = np.ascontiguousarray(a.reshape(a.shape[-2], a.shape[-1]))
    for k in ("ln1_g", "ln1_b", "ln2_g", "ln2_b", "ln3_g", "ln3_b", "hgrn_norm_g", "conv_b", "conv_ln_g", "conv_ln_b"):
        a = np.asarray(inputs[k], dtype=np.float32)
        shared[k] = np.ascontiguousarray(a.reshape(1, a.shape[-1]))
    shared["lb_logits"] = np.ascontiguousarray(np.asarray(inputs["lb_logits"], dtype=np.float32).reshape(2, 512))
    shared["conv_w"] = np.ascontiguousarray(np.asarray(inputs["conv_w"], dtype=np.float32).reshape(CW, 512))
    in_maps = []
    for c in range(n):
        m = dict(shared)
        m["x"] = np.ascontiguousarray(x[c * nseq:(c + 1) * nseq].reshape(nseq * T, D))
        in_maps.append(m)
    res = run_bass_kernel_spmd(nc, in_maps, core_ids=list(range(n)))
    out = np.concatenate([np.asarray(r["out"]).reshape(nseq, T, D) for r in res.results], axis=0)
    return out.astype(np.float32, copy=False)
```
